# Optimizing a Trainium2 kernel written in Bass

```python
import jax, jax.numpy as jnp
from jax import lax
import numpy as np

D_MODEL = 1024
BATCH = 32
SEQ = 2048
DEPTH = 1

D_PLE = 256
HEAD_DIM = 64
ROPE_DIM = HEAD_DIM // 4
ROPE_THETA = 500000.0
N_HEADS_A = 8
N_HEADS_B = 8
WIDTH_A = N_HEADS_A * HEAD_DIM
WIDTH_B = N_HEADS_B * HEAD_DIM
N_IDX_HEADS = 8
IDX_DIM = 64
TOPK_MAX = 256
Q_BLOCK = 128
EPS = 1e-6
NEG = -1e30

SPLITS = (WIDTH_A, HEAD_DIM, HEAD_DIM, WIDTH_A,
          N_IDX_HEADS * IDX_DIM, IDX_DIM, N_IDX_HEADS,
          WIDTH_B, WIDTH_B, WIDTH_B, N_HEADS_B, WIDTH_B)
D_IN = sum(SPLITS)

kernel_name = "hybrid_dsa_fox_gated_block"


def _offsets():
    offs, acc = [], 0
    for w in SPLITS[:-1]:
        acc += w
        offs.append(acc)
    return offs


def rmsnorm(x, g):
    xf = x.astype(jnp.float32)
    y = xf * lax.rsqrt(jnp.mean(xf * xf, axis=-1, keepdims=True) + EPS) * g.astype(jnp.float32)
    return y.astype(x.dtype)


def partial_rope(x, pos):
    half = ROPE_DIM // 2
    freqs = ROPE_THETA ** (-jnp.arange(half, dtype=jnp.float32) / half)
    ang = pos.astype(jnp.float32)[:, None] * freqs[None, :]
    cos = jnp.cos(ang)[None, :, None, :]
    sin = jnp.sin(ang)[None, :, None, :]
    x1 = x[..., :half].astype(jnp.float32)
    x2 = x[..., half:ROPE_DIM].astype(jnp.float32)
    rot = jnp.concatenate([x1 * cos - x2 * sin, x2 * cos + x1 * sin], axis=-1).astype(x.dtype)
    return jnp.concatenate([rot, x[..., ROPE_DIM:]], axis=-1)


def to_blocks(t):
    B, S = t.shape[:2]
    t = t.reshape((B, S // Q_BLOCK, Q_BLOCK) + t.shape[2:])
    return jnp.moveaxis(t, 1, 0)


def from_blocks(t):
    t = jnp.moveaxis(t, 0, 1)
    return t.reshape((t.shape[0], t.shape[1] * t.shape[2]) + t.shape[3:])


def dsa_branch(qa, ka, va, qi, ki, wi):
    B, S = ka.shape[:2]
    topk = min(TOPK_MAX, S // 4)
    nb = S // Q_BLOCK
    key_pos = jnp.arange(S)
    scale = HEAD_DIM ** -0.5

    def block(args):
        blk, q_blk, qi_blk, wi_blk = args
        q_pos = blk * Q_BLOCK + jnp.arange(Q_BLOCK)
        causal = key_pos[None, :] <= q_pos[:, None]
        dots = jnp.einsum('bqhd,bsd->bqhs', qi_blk, ki)
        score = jnp.einsum('bqhs,bqh->bqs', jax.nn.relu(dots).astype(jnp.float32),
                           wi_blk.astype(jnp.float32))
        score = jnp.where(causal[None], score, -jnp.inf)
        _, sel = lax.top_k(score, topk)
        k_sel = jax.vmap(lambda k, i: k[i])(ka, sel)
        v_sel = jax.vmap(lambda v, i: v[i])(va, sel)
        logits = jnp.einsum('bqhd,bqkd->bhqk', q_blk, k_sel).astype(jnp.float32) * scale
        valid = sel <= q_pos[None, :, None]
        logits = jnp.where(valid[:, None], logits, NEG)
        probs = jax.nn.softmax(logits, axis=-1).astype(v_sel.dtype)
        return jnp.einsum('bhqk,bqkd->bqhd', probs, v_sel)

    out = lax.map(block, (jnp.arange(nb), to_blocks(qa), to_blocks(qi), to_blocks(wi)))
    return from_blocks(out)


def fox_branch(qb, kb, vb, log_f):
    B, S = qb.shape[:2]
    nb = S // Q_BLOCK
    key_pos = jnp.arange(S)
    scale = HEAD_DIM ** -0.5
    c = jnp.cumsum(log_f, axis=1)
    c_k = jnp.transpose(c, (0, 2, 1))

    def block(args):
        blk, q_blk, cq_blk = args
        q_pos = blk * Q_BLOCK + jnp.arange(Q_BLOCK)
        causal = key_pos[None, :] <= q_pos[:, None]
        logits = jnp.einsum('bqhd,bshd->bhqs', q_blk, kb).astype(jnp.float32) * scale
        logits = logits + jnp.transpose(cq_blk, (0, 2, 1))[..., None] - c_k[:, :, None, :]
        logits = jnp.where(causal[None, None], logits, NEG)
        probs = jax.nn.softmax(logits, axis=-1).astype(vb.dtype)
        return jnp.einsum('bhqs,bshd->bqhd', probs, vb)

    out = lax.map(block, (jnp.arange(nb), to_blocks(qb), to_blocks(c)))
    return from_blocks(out)


def setup_inputs(seed: int = 0) -> dict:
    key = jax.random.key(seed)
    ks = jax.random.split(key, 14)
    f32 = jnp.float32
    nrm = lambda k, shape, fan_in: jax.random.normal(k, shape, f32) * (fan_in ** -0.5)
    x = jax.random.normal(ks[0], (BATCH, SEQ, D_MODEL), f32)
    p = jax.random.normal(ks[1], (DEPTH, BATCH, SEQ, D_PLE), f32)
    w_in = nrm(ks[2], (DEPTH, D_MODEL, D_IN), D_MODEL)
    b_forget = jax.random.uniform(ks[3], (DEPTH, N_HEADS_B), f32, 1.0, 5.0)
    w_branch_a = nrm(ks[4], (DEPTH, WIDTH_A, D_MODEL), WIDTH_A)
    w_branch_b = nrm(ks[5], (DEPTH, WIDTH_B, D_MODEL), WIDTH_B)
    w_merge = nrm(ks[6], (DEPTH, D_MODEL, 2 * D_MODEL), D_MODEL)
    w_out = nrm(ks[7], (DEPTH, D_MODEL, D_MODEL), D_MODEL)
    g_pre = 1.0 + 0.02 * jax.random.normal(ks[8], (DEPTH, D_MODEL), f32)
    g_post = 1.0 + 0.02 * jax.random.normal(ks[9], (DEPTH, D_MODEL), f32)
    w_ple = nrm(ks[10], (DEPTH, D_PLE, D_MODEL), D_PLE)
    w_ple_gate = nrm(ks[11], (DEPTH, D_MODEL, D_MODEL), D_MODEL)
    g_ple = 1.0 + 0.02 * jax.random.normal(ks[12], (DEPTH, D_MODEL), f32)
    return {"x": x, "p": p, "w_in": w_in, "b_forget": b_forget,
            "w_branch_a": w_branch_a, "w_branch_b": w_branch_b, "w_merge": w_merge,
            "w_out": w_out, "g_pre": g_pre, "g_post": g_post, "w_ple": w_ple,
            "w_ple_gate": w_ple_gate, "g_ple": g_ple}


def reference(x, p, w_in, b_forget, w_branch_a, w_branch_b, w_merge, w_out,
              g_pre, g_post, w_ple, w_ple_gate, g_ple):
    B, S, _ = x.shape
    pos = jnp.arange(S)
    offs = _offsets()
    idx_scale = (N_IDX_HEADS ** -0.5) * (IDX_DIM ** -0.5)
    for i in range(DEPTH):
        h = rmsnorm(x, g_pre[i])
        proj = h @ w_in[i]
        (qa, ka, va, gate_a, qi, ki, wi, qb, kb, vb, fb, gate_b) = jnp.split(proj, offs, axis=-1)

        qa = partial_rope(qa.reshape(B, S, N_HEADS_A, HEAD_DIM), pos)
        ka = partial_rope(ka.reshape(B, S, 1, HEAD_DIM), pos).reshape(B, S, HEAD_DIM)
        qi = partial_rope(qi.reshape(B, S, N_IDX_HEADS, IDX_DIM), pos)
        ki = partial_rope(ki.reshape(B, S, 1, IDX_DIM), pos).reshape(B, S, IDX_DIM)
        wi = wi * idx_scale
        att_a = dsa_branch(qa, ka, va, qi, ki, wi).reshape(B, S, WIDTH_A)
        y_a = (att_a * jax.nn.silu(gate_a)) @ w_branch_a[i]

        log_f = jax.nn.log_sigmoid(fb.astype(jnp.float32) + b_forget[i].astype(jnp.float32))
        att_b = fox_branch(qb.reshape(B, S, N_HEADS_B, HEAD_DIM),
                           kb.reshape(B, S, N_HEADS_B, HEAD_DIM),
                           vb.reshape(B, S, N_HEADS_B, HEAD_DIM), log_f).reshape(B, S, WIDTH_B)
        y_b = (att_b * jax.nn.silu(gate_b)) @ w_branch_b[i]

        m_a, m_b = jnp.split(jax.nn.sigmoid(h @ w_merge[i]), 2, axis=-1)
        out = (m_a * y_a + m_b * y_b) @ w_out[i]
        x = x + rmsnorm(out, g_post[i])

        e = p[i] @ w_ple[i]
        gate = jax.nn.sigmoid(x @ w_ple_gate[i])
        x = x + rmsnorm(gate * e, g_ple[i])
    return x
```

```python
import contextlib
import os
import numpy as np
import concourse.bass as bass
import concourse.mybir as mybir
from concourse.bass_utils import run_bass_kernel_spmd

F32 = mybir.dt.float32
BF16 = mybir.dt.bfloat16
ALU = mybir.AluOpType
AF = mybir.ActivationFunctionType
AX = mybir.AxisListType

NCORES = 8
S = 2048
D = 1024
NT = 16
DIN = 3792
OFF = dict(qa=0, ka=512, va=576, ga=640, qi=1152, ki=1664, wi=1728, qb=1736, kb=2248, vb=2760, fb=3272, gb=3280)
EPS = 1e-6
NIT = 20
STAGE = float(os.environ.get('MK_STAGE', '9'))
ATTACH = int(os.environ.get('MK_ATTACH', '0'))
TOPK = 256
MASKNEG = -30000.0
IDX_SCALE = (8 ** -0.5) * (64 ** -0.5)

ENGS = ("pe", "act", "dve", "pool", "sp")


class Trk:
    def __init__(self, nc):
        self.nc = nc
        self.h = {"pe": nc.tensor, "act": nc.scalar, "dve": nc.vector, "pool": nc.gpsimd, "sp": nc.sync}
        self.stack = contextlib.ExitStack()
        self.sems = {e: self.stack.enter_context(nc.semaphore("s_" + e)) for e in ENGS}
        self.cnt = {e: 0 for e in ENGS}
        self.lanecnt = {}
        self.clock = {e: {} for e in ENGS}
        self.snap = {}
        self.res = {}
        self.nwaits = 0
        self.nops = 0
        self.enabled = True
        self.pending = {e: None for e in ENGS}

    def _sem(self, sk):
        s = self.sems.get(sk)
        if s is None:
            s = self.stack.enter_context(self.nc.semaphore("l_%d" % len(self.sems)))
            self.sems[sk] = s
        return s

    def op(self, eng, fn, reads=(), writes=(), lane=None):
        if not self.enabled:
            return None
        deps = []
        for r in reads:
            st = self.res.get(r)
            if st and st[0]:
                deps.append(st[0])
        for w in writes:
            st = self.res.get(w)
            if st:
                if st[0]:
                    deps.append(st[0])
                deps.extend(st[1])
        clk = self.clock[eng]
        waits = {}
        if self.pending[eng]:
            deps.extend(self.pending[eng].items())
            self.pending[eng] = None
        for (sk, v) in deps:
            if sk == eng and eng == "pe":
                continue
            if clk.get(sk, 0) >= v:
                continue
            if waits.get(sk, 0) < v:
                waits[sk] = v
        if waits:
            clk = dict(clk)
            for sk, v in waits.items():
                sn = self.snap.get((sk, v))
                if sn:
                    for k2, v2 in sn.items():
                        if clk.get(k2, 0) < v2:
                            clk[k2] = v2
                if clk.get(sk, 0) < v:
                    clk[sk] = v
            self.clock[eng] = clk
        if lane is None:
            self.cnt[eng] += 1
            me = (eng, self.cnt[eng])
        else:
            sk = ("lane", lane)
            self.lanecnt[lane] = self.lanecnt.get(lane, 0) + 16
            me = (sk, self.lanecnt[lane])
        sn = dict(clk)
        sn[me[0]] = me[1]
        self.snap[me] = sn
        for r in reads:
            st = self.res.setdefault(r, [None, []])
            st[1].append(me)
        for w in writes:
            self.res[w] = [me, []]
        h = self.h[eng]
        wl = list(waits.items())
        self.nwaits += len(wl)
        self.nops += 1
        if wl and ATTACH:
            for sk, v in wl[:-1]:
                h.wait_ge(self._sem(sk), v)
            ins = fn(h)
            sk, v = wl[-1]
            ins = ins._wait_ge(self._sem(sk), v)
        else:
            for sk, v in wl:
                h.wait_ge(self._sem(sk), v)
            ins = fn(h)
        ins.then_inc(self._sem(me[0]), 16 if lane is not None else 1)
        return me

    def fence(self):
        marks = {e: c for e, c in self.cnt.items() if c}
        for ln, c in self.lanecnt.items():
            marks[("lane", ln)] = c
        for e in ENGS:
            self.pending[e] = dict(marks)

    def wait_marks(self, eng, marks):
        h = self.h[eng]
        for sk, v in marks:
            h.wait_ge(self._sem(sk), v)

    def close(self):
        self.stack.close()


def build_program(nseq, dbg=False):
    nc = bass.Bass("TRN2", target_bir_lowering=False)

    def din(name, shape):
        return nc.dram_tensor(name, shape, F32, kind="ExternalInput").ap()

    x_d = din("x", [nseq, S, D])
    p_d = din("p", [nseq, S, 256])
    w_in_d = din("w_in", [D, DIN])
    w_ba_d = din("w_ba", [512, D])
    w_bb_d = din("w_bb", [512, D])
    w_m_d = din("w_m", [D, 2 * D])
    w_o_d = din("w_o", [D, D])
    w_p_d = din("w_p", [256, D])
    w_pg_d = din("w_pg", [D, D])
    gpre_d = din("gpre", [128, D])
    gpost_d = din("gpost", [128, D])
    gple_d = din("gple", [128, D])
    bfb_d = din("bfb", [128, 8])
    cos_d = din("cosT", [128, S])
    sin_d = din("sinT", [128, S])
    cst_d = din("cst", [128, 7, 128])
    pw2_d = din("pw2", [128, NIT])
    out_d = nc.dram_tensor("out", [nseq, S, D], F32, kind="ExternalOutput").ap()
    dbg_d = {}

    T = Trk(nc)
    op = T.op
    uid = [0]

    def dma_in(eng, out_ap, in_ap, key, lane):
        return op(eng, lambda h: h.dma_start(out=out_ap, in_=in_ap), writes=[key], lane=lane)

    with contextlib.ExitStack() as g:
        def sb(name, shape, dt, es=g):
            uid[0] += 1
            return es.enter_context(nc.sbuf_tensor("%s_u%d" % (name, uid[0]), shape, dt))

        def ps(name, shape, dt, es=g):
            uid[0] += 1
            return es.enter_context(nc.psum_tensor("%s_u%d" % (name, uid[0]), shape, dt))

        cst = sb("cst", [128, 7, 128], F32)
        ident_bf = sb("ident_bf", [128, 128], BF16)
        trineg_bf = sb("trineg_bf", [128, 128], BF16)
        irep_bf = sb("irep_bf", [128, 512], BF16)
        ones_bf = sb("ones_bf", [128, 64], BF16)
        gpre = sb("gpre_s", [128, D], F32)
        gpost = sb("gpost_s", [128, D], F32)
        gple = sb("gple_s", [128, D], F32)
        bfb = sb("bfb_s", [128, 8], F32)
        pw2 = sb("pw2_s", [128, NIT], F32)
        negh = sb("negh", [128, 1], F32)
        taum = sb("taum", [128, 1], F32)
        ident_f = cst[:, 0, :]
        perm_f = cst[:, 1, :]
        causneg_f = cst[:, 3, :]
        U_f = cst[:, 4, :]
        sel0_f = cst[:, 5, :]
        ones_f = cst[:, 6, :]

        dma_in("sp", cst[:], cst_d[:, :, :], "cst", "c0")
        dma_in("sp", gpre[:], gpre_d[:, :], "gpre", "c1")
        dma_in("sp", gpost[:], gpost_d[:, :], "gpost", "c2")
        dma_in("sp", gple[:], gple_d[:, :], "gple", "c3")
        dma_in("sp", bfb[:], bfb_d[:, :], "bfb", "c4")
        dma_in("sp", pw2[:], pw2_d[:, :], "pw2", "c5")
        dma_in("pool", ident_bf[:], cst_d[:, 0, :], "ident_bf", "c6")
        dma_in("pool", trineg_bf[:], cst_d[:, 2, :], "trineg_bf", "c7")
        for k in range(4):
            op("pool", lambda h, k=k: h.dma_start(out=irep_bf[:, k * 128:(k + 1) * 128], in_=cst_d[:, 0, :]),
               writes=[("irep", k)], lane="c8")
        IREP = [("irep", k) for k in range(4)]
        op("pool", lambda h: h.memset(ones_bf[:], 1.0), writes=["ones_bf"])
        op("pool", lambda h: h.memset(negh[:], -0.5), writes=["negh"])
        op("pool", lambda h: h.memset(taum[:], -1.0e8), writes=["taum"])

        out_marks = []

        for b in range(nseq):
            T.fence()
            with contextlib.ExitStack() as sq:
                hT = sb("hT", [128, 8, S], BF16, sq)
                qaT = sb("qaT", [128, 4, S], BF16, sq)
                qbT = sb("qbT", [128, 4, S], BF16, sq)
                with contextlib.ExitStack() as p12:
                    qiT = sb("qiT", [128, 4, S], BF16, p12)
                    kaT = sb("kaT", [128, S], BF16, p12)
                    kiT = sb("kiT", [128, S], BF16, p12)
                    kbT = sb("kbT", [128, 4, S], BF16, p12)
                    va = sb("va", [128, NT, 128], BF16, p12)
                    vb = sb("vb", [128, NT, 512], BF16, p12)
                    wi = sb("wi", [128, NT, 8], F32, p12)
                    fbs = sb("fbs", [128, NT, 8], F32, p12)
                    csb = sb("csb", [128, NT, 8], F32, p12)
                    carry = sb("carry", [128, NT, 8], F32, p12)
                    with contextlib.ExitStack() as p1:
                        T.enabled = STAGE >= 1
                        cosT = sb("cosT_s", [128, S], F32, p1)
                        sinT = sb("sinT_s", [128, S], F32, p1)
                        xt = [sb("xt%d" % k, [128, D], F32, p1) for k in range(2)]
                        junk = sb("junk1", [128, D], BF16, p1)
                        hb = [sb("hb%d" % k, [128, D], BF16, p1) for k in range(2)]
                        wbuf = [sb("wbuf%d" % k, [128, 8, 512], BF16, p1) for k in range(2)]
                        xs = [sb("xs%d" % k, [128, 512], F32, p1) for k in range(2)]
                        t1 = [sb("t1_0", [128, 512], F32, p1)] * 2
                        t2 = [sb("t2_0", [128, 512], F32, p1)] * 2
                        st = sb("st1", [128, 4], F32, p1)
                        wsm = sb("wsm", [128, 8, 80], BF16, p1)
                        sm16 = sb("sm16", [128, 16], F32, p1)
                        PA = [ps("PA%d" % k, [128, 512], F32, p1) for k in range(4)]
                        PB = [ps("PB%d" % k, [128, 512], F32, p1) for k in range(2)]
                        PTB = ps("PTB1", [128, 1024], BF16, p1)
                        PC = ps("PC1", [128, 512], F32, p1)

                        dma_in("sp", cosT[:], cos_d[:, :], "cosT", "cos")
                        dma_in("sp", sinT[:], sin_d[:, :], "sinT", "sin")
                        op("pool", lambda h: h.memset(va[:, :, 64:128], 1.0), writes=["va_ones"])

                        wl_state = {"n": 0}

                        def load_w(segments):
                            k = wl_state["n"] % 2
                            wl_state["n"] += 1
                            c = 0
                            for (c0, ncol) in segments:
                                for kc0 in range(0, 8, 4):
                                    op("pool", lambda h, k=k, c=c, c0=c0, ncol=ncol, kc0=kc0: h.dma_start(
                                        out=wbuf[k][:, kc0:kc0 + 4, c:c + ncol],
                                        in_=w_in_d[kc0 * 128:(kc0 + 4) * 128, c0:c0 + ncol].rearrange("(kc kp) n -> kp kc n", kp=128)),
                                       writes=[("wbuf", k)], lane="wbuf%d" % k)
                                c += ncol
                            return k

                        for t in range(NT):
                            k = t % 2
                            dma_in("sp", xt[k][:], x_d[b, t * 128:(t + 1) * 128, :], ("xt", k), "xt%d" % k)
                            op("act", lambda h, k=k: h.activation(out=junk[:], in_=xt[k][:], func=AF.Square, accum_out=st[:, 0:1]),
                               reads=[("xt", k)], writes=["junk1", "st0"])
                            op("dve", lambda h: h.tensor_scalar(out=st[:, 1:2], in0=st[:, 0:1], scalar1=1.0 / D, scalar2=EPS,
                                                                op0=ALU.mult, op1=ALU.add), reads=["st0"], writes=["st1"])
                            op("pool", lambda h: h.tensor_tensor(out=st[:, 2:3], in0=st[:, 1:2], in1=negh[:], op=ALU.pow),
                               reads=["st1", "negh"], writes=["st2"])
                            op("dve", lambda h, k=k: h.scalar_tensor_tensor(out=hb[k][:], in0=xt[k][:], scalar=st[:, 2:3], in1=gpre[:],
                                                                            op0=ALU.mult, op1=ALU.mult),
                               reads=[("xt", k), "st2", "gpre"], writes=[("hb", k)])
                            for kc in range(8):
                                op("pe", lambda h, k=k, kc=kc: h.transpose(out=PTB[:, kc * 128:(kc + 1) * 128], in_=hb[k][:, kc * 128:(kc + 1) * 128],
                                                                           identity=ident_bf[:]),
                                   reads=[("hb", k), "ident_bf"], writes=["PTB1"])
                            op("act", lambda h, t=t: h.copy(out=hT[:, :, t * 128:(t + 1) * 128], in_=PTB[:, :].rearrange("p (kc n) -> p kc n", kc=8)),
                               reads=["PTB1"], writes=[("hT", t // 4)])

                        T.enabled = STAGE >= 1.2
                        pa_i = [0]

                        def proj_fm(k, cofs, tg):
                            slot = pa_i[0] % 4
                            pa_i[0] += 1
                            for kc in range(8):
                                op("pe", lambda h, kc=kc, slot=slot: h.matmul(PA[slot][:, :], lhsT=wbuf[k][:, kc, cofs:cofs + 128],
                                                                             rhs=hT[:, kc, tg * 512:(tg + 1) * 512], start=(kc == 0), stop=(kc == 7)),
                                   reads=[("wbuf", k), ("hT", tg)], writes=[("PA", slot)])
                            return slot

                        rp_i = [0]

                        def rope_evac(slot, dst_ap, dst_key, tg):
                            r = rp_i[0] % 2
                            rp_i[0] += 1
                            tsl = slice(tg * 512, (tg + 1) * 512)
                            op("act", lambda h: h.copy(out=xs[r][:], in_=PA[slot][:, :]), reads=[("PA", slot)], writes=[("xs", r)])
                            op("pe", lambda h: h.matmul(PB[r][:, :], lhsT=perm_f, rhs=xs[r][:], start=True, stop=True),
                               reads=[("xs", r), "cst"], writes=[("PB", r)])
                            op("dve", lambda h: h.tensor_tensor(out=t1[r][:], in0=PB[r][:, :], in1=sinT[:, tsl], op=ALU.mult),
                               reads=[("PB", r), "sinT"], writes=["t1"])
                            op("pool", lambda h: h.tensor_tensor(out=t2[r][:], in0=xs[r][:], in1=cosT[:, tsl], op=ALU.mult),
                               reads=[("xs", r), "cosT"], writes=["t2"])
                            op("dve", lambda h: h.tensor_tensor(out=dst_ap, in0=t1[r][:], in1=t2[r][:], op=ALU.add),
                               reads=["t1", "t2"], writes=[dst_key])

                        for (nm, dst) in (("qa", qaT), ("qi", qiT)):
                            k = load_w([(OFF[nm], 512)])
                            for c in range(4):
                                for tg in range(4):
                                    slot = proj_fm(k, c * 128, tg)
                                    rope_evac(slot, dst[:, c, tg * 512:(tg + 1) * 512], (nm + "T", c, tg), tg)
                        T.enabled = STAGE >= 1.3
                        k = load_w([(OFF["ka"], 64), (OFF["ka"], 64), (OFF["ki"], 64), (OFF["ki"], 64)])
                        for c, (nm, dst) in enumerate((("ka", kaT), ("ki", kiT))):
                            for tg in range(4):
                                slot = proj_fm(k, c * 128, tg)
                                rope_evac(slot, dst[:, tg * 512:(tg + 1) * 512], (nm + "T", tg), tg)
                        T.enabled = STAGE >= 1.4
                        for (nm, dst) in (("qb", qbT), ("kb", kbT)):
                            k = load_w([(OFF[nm], 512)])
                            for c in range(4):
                                for tg in range(4):
                                    slot = proj_fm(k, c * 128, tg)
                                    op("act", lambda h, slot=slot, dst=dst, c=c, tg=tg: h.copy(out=dst[:, c, tg * 512:(tg + 1) * 512], in_=PA[slot][:, :]),
                                       reads=[("PA", slot)], writes=[(nm + "T", c, tg)])
                        T.enabled = STAGE >= 1.5
                        k = load_w([(OFF["vb"], 512)])
                        k2 = load_w([(OFF["va"], 64), (OFF["wi"] - 64, 72), (OFF["fb"] - 64, 72)])
                        op("dve", lambda h: h.tensor_copy(out=wsm[:, :, 0:64], in_=wbuf[k2][:, :, 0:64]), reads=[("wbuf", k2)], writes=["wsm"])
                        op("dve", lambda h: h.tensor_copy(out=wsm[:, :, 64:72], in_=wbuf[k2][:, :, 128:136]), reads=[("wbuf", k2)], writes=["wsm"])
                        op("dve", lambda h: h.tensor_copy(out=wsm[:, :, 72:80], in_=wbuf[k2][:, :, 200:208]), reads=[("wbuf", k2)], writes=["wsm"])
                        for t in range(NT):
                            slot = pa_i[0] % 4
                            pa_i[0] += 1
                            for kc in range(8):
                                op("pe", lambda h, kc=kc, slot=slot, t=t: h.matmul(PA[slot][:, :], lhsT=hT[:, kc, t * 128:(t + 1) * 128],
                                                                                  rhs=wbuf[k][:, kc, 0:512], start=(kc == 0), stop=(kc == 7)),
                                   reads=[("wbuf", k), ("hT", t // 4)], writes=[("PA", slot)])
                            op("act", lambda h, slot=slot, t=t: h.copy(out=vb[:, t, :], in_=PA[slot][:, :]), reads=[("PA", slot)], writes=[("vb", t)])
                            if STAGE < 1.6:
                                continue
                            for kc in range(8):
                                op("pe", lambda h, kc=kc, t=t: h.matmul(PC[:, 0:80], lhsT=hT[:, kc, t * 128:(t + 1) * 128],
                                                                        rhs=wsm[:, kc, 0:80], start=(kc == 0), stop=(kc == 7)),
                                   reads=["wsm", ("hT", t // 4)], writes=["PC1"])
                            op("act", lambda h, t=t: h.copy(out=va[:, t, 0:64], in_=PC[:, 0:64]), reads=["PC1"], writes=[("va", t)])
                            op("act", lambda h: h.copy(out=sm16[:], in_=PC[:, 64:80]), reads=["PC1"], writes=["sm16"])
                            op("act", lambda h, t=t: h.mul(out=wi[:, t, :], in_=sm16[:, 0:8], mul=IDX_SCALE),
                               reads=["sm16"], writes=[("wi", t)])
                            op("pool", lambda h, t=t: h.tensor_tensor(out=fbs[:, t, :], in0=sm16[:, 8:16], in1=bfb[:], op=ALU.add),
                               reads=["sm16", "bfb"], writes=["fbs"])
                    T.fence()
                    with contextlib.ExitStack() as p2:
                        T.enabled = STAGE >= 2
                        score = [sb("score%d" % k, [128, S], F32, p2) for k in range(2)]
                        tmp = [sb("tmp%d" % k, [128, 512], F32, p2) for k in range(2)]
                        junk2 = sb("junk2", [128, S], BF16, p2)
                        mneg = [sb("mneg%d" % k, [128, S], BF16, p2) for k in range(2)]
                        ptd = [sb("ptd%d" % k, [128, 512], BF16, p2) for k in range(3)]
                        ptf = [sb("ptf%d" % k, [128, 128], BF16, p2) for k in range(4)]
                        dend = sb("dend", [128, 1024], F32, p2)
                        denf = sb("denf", [128, 512], F32, p2)
                        sst = [sb("sst%d" % k, [128, 8 + NIT], F32, p2) for k in range(2)]
                        crefb = sb("crefb", [128, NT, 8], F32, p2)
                        biasT = sb("biasT", [128, NT, NT, 8], F32, p2)
                        IDX = [ps("IDX%d" % k, [128, 512], F32, p2) for k in range(2)]
                        DS = [ps("DS%d" % k, [128, 512], F32, p2) for k in range(2)]
                        DACC = ps("DACC", [128, 1024], F32, p2)
                        FS = ps("FS", [128, 512], F32, p2)
                        FACC = ps("FACC", [128, 512], F32, p2)
                        cnt = {"idx": 0, "ptd": 0, "fs": 0}
                        fl = fbs[:].rearrange("p t h -> p (t h)")
                        cl = csb[:].rearrange("p t h -> p (t h)")
                        op("act", lambda h: h.activation(out=fl, in_=fl, func=AF.Exp, scale=-1.0), reads=["fbs"], writes=["fbs"])
                        op("act", lambda h: h.activation(out=fl, in_=fl, func=AF.Ln, bias=1.0, scale=1.0), reads=["fbs"], writes=["fbs"])
                        op("dve", lambda h: h.tensor_scalar(out=fl, in0=fl, scalar1=-1.0, scalar2=None, op0=ALU.mult), reads=["fbs"], writes=["fbs"])
                        op("pe", lambda h: h.matmul(IDX[0][:, 0:128], lhsT=U_f, rhs=fl, start=True, stop=True), reads=["fbs", "cst"], writes=[("IDX", 0)])
                        op("pe", lambda h: h.matmul(IDX[1][:, 0:128], lhsT=ones_f, rhs=fl, start=True, stop=True), reads=["fbs", "cst"], writes=[("IDX", 1)])
                        op("dve", lambda h: h.memset(carry[:, 0, :], 0.0), writes=["carry"])
                        for t in range(1, NT):
                            op("dve", lambda h, t=t: h.tensor_tensor(out=carry[:, t, :], in0=carry[:, t - 1, :], in1=IDX[1][:, (t - 1) * 8:t * 8], op=ALU.add),
                               reads=[("IDX", 1), "carry"], writes=["carry"])
                        op("dve", lambda h: h.tensor_tensor(out=cl, in0=IDX[0][:, 0:128], in1=carry[:].rearrange("p t h -> p (t h)"), op=ALU.add),
                           reads=[("IDX", 0), "carry"], writes=["csb"])
                        op("pe", lambda h: h.matmul(IDX[0][:, 0:128], lhsT=sel0_f, rhs=cl, start=True, stop=True), reads=["csb", "cst"], writes=[("IDX", 0)])
                        op("act", lambda h: h.copy(out=crefb[:].rearrange("p t h -> p (t h)"), in_=IDX[0][:, 0:128]), reads=[("IDX", 0)], writes=["crefb"])
                        for i in range(NT):
                            op("dve", lambda h, i=i: h.tensor_tensor(out=biasT[:, i, 0:i + 1, :],
                                                                     in0=crefb[:, i, :].unsqueeze(1).broadcast_to([128, i + 1, 8]),
                                                                     in1=csb[:, 0:i + 1, :], op=ALU.subtract),
                               reads=["crefb", "csb"], writes=["biasT"])

                        def dsa_index(i):
                            sp_ = i % 2
                            L = 128 * (i + 1)
                            sc = score[sp_]
                            nch = (L + 511) // 512
                            for hd in range(8):
                                c, r = hd // 2, hd % 2
                                for kk in range(nch):
                                    w = min(512, L - 512 * kk)
                                    sl = cnt["idx"] % 2
                                    cnt["idx"] += 1
                                    op("pe", lambda h, sl=sl, w=w, kk=kk, c=c, r=r: h.matmul(
                                        IDX[sl][:, 0:w], lhsT=qiT[64 * r:64 * r + 64, c, i * 128:(i + 1) * 128],
                                        rhs=kiT[64 * r:64 * r + 64, kk * 512:kk * 512 + w], start=True, stop=True),
                                       reads=[("qiT", c, i // 4), ("kiT", kk)], writes=[("IDX", sl)])
                                    op("act", lambda h, sl=sl, w=w: h.activation(out=tmp[sl][:, 0:w], in_=IDX[sl][:, 0:w], func=AF.Relu),
                                       reads=[("IDX", sl)], writes=[("tmp", sl)])
                                    if hd == 0:
                                        op("dve", lambda h, sl=sl, w=w, kk=kk: h.tensor_scalar(
                                            out=sc[:, kk * 512:kk * 512 + w], in0=tmp[sl][:, 0:w], scalar1=wi[:, i, 0:1], scalar2=None, op0=ALU.mult),
                                           reads=[("tmp", sl), ("wi", i)], writes=[("score", sp_)])
                                    else:
                                        op("dve", lambda h, sl=sl, w=w, kk=kk, hd=hd: h.scalar_tensor_tensor(
                                            out=sc[:, kk * 512:kk * 512 + w], in0=tmp[sl][:, 0:w], scalar=wi[:, i, hd:hd + 1],
                                            in1=sc[:, kk * 512:kk * 512 + w], op0=ALU.mult, op1=ALU.add),
                                           reads=[("tmp", sl), ("wi", i), ("score", sp_)], writes=[("score", sp_)])
                            op("dve", lambda h: h.tensor_tensor(out=sc[:, i * 128:L], in0=sc[:, i * 128:L], in1=causneg_f, op=ALU.add),
                               reads=[("score", sp_), "cst"], writes=[("score", sp_)])
                            s_ = sst[sp_]
                            SK = ("sst", sp_)
                            if i >= 2:
                                op("dve", lambda h: h.tensor_reduce(out=s_[:, 0:1], in_=sc[:, 0:128 * i], axis=AX.X, op=ALU.min),
                                   reads=[("score", sp_)], writes=[SK])
                                op("dve", lambda h: h.tensor_reduce(out=s_[:, 1:2], in_=sc[:, 0:L], axis=AX.X, op=ALU.max),
                                   reads=[("score", sp_)], writes=[SK])
                                op("dve", lambda h: h.tensor_tensor(out=s_[:, 2:3], in0=s_[:, 1:2], in1=s_[:, 0:1], op=ALU.subtract),
                                   reads=[SK], writes=[SK])
                                op("dve", lambda h: h.tensor_scalar(out=s_[:, 8:8 + NIT], in0=pw2[:], scalar1=s_[:, 2:3], scalar2=None, op0=ALU.mult),
                                   reads=[SK, "pw2"], writes=[SK])
                                for it in range(NIT):
                                    op("dve", lambda h, it=it: h.tensor_tensor(out=s_[:, 3:4], in0=s_[:, 0:1], in1=s_[:, 8 + it:9 + it], op=ALU.add),
                                       reads=[SK], writes=[SK])
                                    op("dve", lambda h: h.tensor_scalar(out=junk2[:, 0:L], in0=sc[:, 0:L], scalar1=s_[:, 3:4], scalar2=None,
                                                                        op0=ALU.is_ge, op1=ALU.add, accum_out=s_[:, 4:5]),
                                       reads=[SK, ("score", sp_)], writes=[SK, "junk2"])
                                    op("dve", lambda h, it=it: h.scalar_tensor_tensor(out=s_[:, 5:6], in0=s_[:, 4:5], scalar=TOPK - 0.5, in1=s_[:, 8 + it:9 + it],
                                                                                      op0=ALU.is_ge, op1=ALU.mult),
                                       reads=[SK], writes=[SK])
                                    op("dve", lambda h: h.tensor_tensor(out=s_[:, 0:1], in0=s_[:, 0:1], in1=s_[:, 5:6], op=ALU.add),
                                       reads=[SK], writes=[SK])
                                tau = s_[:, 0:1]
                                tk = [SK]
                            else:
                                tau = taum[:]
                                tk = ["taum"]
                            op("dve", lambda h: h.tensor_scalar(out=mneg[sp_][:, 0:L], in0=sc[:, 0:L], scalar1=tau, scalar2=MASKNEG,
                                                                op0=ALU.is_lt, op1=ALU.mult),
                               reads=tk + [("score", sp_)], writes=[("mneg", sp_)])

                        def dsa_attn(i):
                            sp_ = i % 2
                            for j in range(i + 1):
                                for half in range(2):
                                    D_ = DS[half]
                                    op("pe", lambda h, j=j, D_=D_: h.matmul(D_[:, :], lhsT=mneg[sp_][:, j * 128:(j + 1) * 128], rhs=irep_bf[:],
                                                                            start=True, stop=False),
                                       reads=[("mneg", sp_)] + IREP, writes=[("DS", half)])
                                    for hh in range(4):
                                        c, r = hh, half
                                        op("pe", lambda h, j=j, D_=D_, hh=hh, c=c, r=r: h.matmul(
                                            D_[:, hh * 128:(hh + 1) * 128], lhsT=kaT[64 * r:64 * r + 64, j * 128:(j + 1) * 128],
                                            rhs=qaT[64 * r:64 * r + 64, c, i * 128:(i + 1) * 128], start=False, stop=(hh == 3)),
                                           reads=[("kaT", j // 4), ("qaT", c, i // 4), ("qa_blk", i)], writes=[("DS", half)])
                                    pk = cnt["ptd"] % 3
                                    cnt["ptd"] += 1
                                    op("act", lambda h, D_=D_, pk=pk: h.activation(out=ptd[pk][:], in_=D_[:, :], func=AF.Exp, scale=0.125),
                                       reads=[("DS", half)], writes=[("ptd", pk)])
                                    op("pe", lambda h, j=j, pk=pk, half=half: h.matmul(DACC[:, half * 512:(half + 1) * 512], lhsT=va[:, j, :], rhs=ptd[pk][:],
                                                                                      start=(j == 0), stop=(j == i)),
                                       reads=[("ptd", pk), ("va", j), "va_ones"], writes=[("DACC", half)])
                            op("act", lambda h: h.copy(out=dend[64:128, :], in_=DACC[64:128, :]), reads=[("DACC", 0), ("DACC", 1)], writes=["dend"])
                            op("dve", lambda h: h.reciprocal(out=dend[64:128, :], in_=dend[64:128, :]), reads=["dend"], writes=["dend"])
                            for r in range(2):
                                op("dve", lambda h, r=r: h.tensor_tensor(
                                    out=qaT[64 * r:64 * r + 64, :, i * 128:(i + 1) * 128],
                                    in0=DACC[0:64, r * 512:(r + 1) * 512].rearrange("p (c q) -> p c q", c=4),
                                    in1=dend[64:128, r * 512:(r + 1) * 512].rearrange("p (c q) -> p c q", c=4), op=ALU.mult),
                                   reads=[("DACC", 0), ("DACC", 1), "dend"], writes=[("qa_blk", i)])

                        def fox_attn(i):
                            for hg in range(2):
                                for hh in range(4):
                                    hd = hg * 4 + hh
                                    c, r = hd // 2, hd % 2
                                    for j in range(i + 1):
                                        sl = cnt["fs"] % 4
                                        cnt["fs"] += 1
                                        F_ = FS[:, sl * 128:(sl + 1) * 128]
                                        op("pe", lambda h, j=j, F_=F_, c=c, r=r: h.matmul(
                                            F_, lhsT=kbT[64 * r:64 * r + 64, c, j * 128:(j + 1) * 128],
                                            rhs=qbT[64 * r:64 * r + 64, c, i * 128:(i + 1) * 128], start=True, stop=(j != i)),
                                           reads=[("kbT", c, j // 4), ("qbT", c, i // 4), ("qb_blk", i, c // 2)], writes=[("FS", sl)])
                                        if j == i:
                                            op("pe", lambda h, F_=F_: h.matmul(F_, lhsT=ident_bf[:], rhs=trineg_bf[:], start=False, stop=True),
                                               reads=["ident_bf", "trineg_bf"], writes=[("FS", sl)])
                                        op("act", lambda h, j=j, F_=F_, sl=sl, hd=hd: h.activation(out=ptf[sl][:], in_=F_, func=AF.Exp, scale=0.125,
                                                                                                  bias=biasT[:, i, j, hd:hd + 1]),
                                           reads=[("FS", sl), "biasT"], writes=[("ptf", sl)])
                                        op("pe", lambda h, j=j, sl=sl, hd=hd, hh=hh: h.matmul(FACC[0:64, hh * 128:(hh + 1) * 128], lhsT=vb[:, j, hd * 64:(hd + 1) * 64],
                                                                                              rhs=ptf[sl][:], start=(j == 0), stop=(j == i)),
                                           reads=[("ptf", sl), ("vb", j)], writes=["FACCn"])
                                        op("pe", lambda h, j=j, sl=sl, hh=hh: h.matmul(FACC[64:128, hh * 128:(hh + 1) * 128], lhsT=ones_bf[:, 0:64],
                                                                                       rhs=ptf[sl][:], start=(j == 0), stop=(j == i)),
                                           reads=[("ptf", sl), "ones_bf"], writes=["FACCd"])
                                FK = ["FACCn", "FACCd"]
                                op("act", lambda h: h.copy(out=denf[64:128, :], in_=FACC[64:128, :]), reads=FK, writes=["denf"])
                                op("dve", lambda h: h.reciprocal(out=denf[64:128, :], in_=denf[64:128, :]), reads=["denf"], writes=["denf"])
                                for r in range(2):
                                    op("dve", lambda h, r=r, hg=hg: h.tensor_tensor(
                                        out=qbT[64 * r:64 * r + 64, 2 * hg:2 * hg + 2, i * 128:(i + 1) * 128],
                                        in0=FACC[0:64, :].rearrange("p (c r q) -> p c r q", c=2, r=2)[:, :, r, :],
                                        in1=denf[64:128, :].rearrange("p (c r q) -> p c r q", c=2, r=2)[:, :, r, :], op=ALU.mult),
                                       reads=FK + ["denf"], writes=[("qb_blk", i, hg)])

                        dsa_index(0)
                        for i in range(NT):
                            if i + 1 < NT:
                                dsa_index(i + 1)
                            dsa_attn(i)
                            fox_attn(i)
                T.fence()
                with contextlib.ExitStack() as p3:
                    T.enabled = STAGE >= 3
                    wo = sb("wo", [128, 8, D], BF16, p3)
                    wpg = sb("wpg", [128, 8, D], BF16, p3)
                    wp = sb("wp", [128, 2, D], BF16, p3)
                    mT = sb("mT", [128, 8, 1024], BF16, p3)
                    wg = [sb("wg%d" % k, [128, 8, 128], BF16, p3) for k in range(2)]
                    wmc = [sb("wmc%d" % k, [128, 8, 256], BF16, p3) for k in range(2)]
                    wbr = [sb("wbr%d" % k, [128, 4, 256], BF16, p3) for k in range(2)]
                    th = [sb("th%d" % k, [128, 512], BF16, p3) for k in range(4)]
                    pa = [sb("pa%d" % k, [128, 512], F32, p3) for k in range(2)]
                    pb = [sb("pb%d" % k, [128, 512], F32, p3) for k in range(2)]
                    x3 = [sb("x3_%d" % k, [128, D], F32, p3) for k in range(2)]
                    pbf = [sb("pbf%d" % k, [128, 256], BF16, p3) for k in range(2)]
                    pT = sb("pT", [128, 2, 128], BF16, p3)
                    junk3 = sb("junk3", [128, D], BF16, p3)
                    tA = sb("tA", [128, D], F32, p3)
                    x1 = sb("x1", [128, D], F32, p3)
                    x1b = sb("x1b", [128, D], BF16, p3)
                    x1T = sb("x1T", [128, 8, 128], BF16, p3)
                    gth = sb("gth", [128, D], F32, p3)
                    ge2 = sb("ge2", [128, D], F32, p3)
                    fin = [sb("fin%d" % k, [128, D], F32, p3) for k in range(2)]
                    s3 = sb("s3", [128, 8], F32, p3)
                    PQ = [ps("PQ%d" % k, [128, 1024], F32, p3) for k in range(3)]
                    PR = ps("PR", [128, 512], F32, p3)
                    PT3 = ps("PT3", [128, 1024], BF16, p3)

                    def load_cast(dst, key, lane, src2d, nkc, ncol, col0, kcstep=4):
                        scol, dcol = (0, 0) if col0 is None else col0
                        for kc0 in range(0, nkc, kcstep):
                            n = min(kcstep, nkc - kc0)
                            op("pool", lambda h, kc0=kc0, n=n: h.dma_start(
                                out=dst[:, kc0:kc0 + n, dcol:dcol + ncol],
                                in_=src2d[kc0 * 128:(kc0 + n) * 128, scol:scol + ncol].rearrange("(kc kp) n -> kp kc n", kp=128)),
                               writes=[key], lane=lane)

                    for gi in range(8):
                        k = gi % 2
                        gname = "ga" if gi < 4 else "gb"
                        fc = gi % 4
                        load_cast(wg[k], ("wg", k), "wg%d" % k, w_in_d, 8, 128, (OFF[gname] + fc * 128, 0))
                        attT = qaT if gi < 4 else qbT
                        blk = "qa_blk" if gi < 4 else "qb_blk"
                        bsuf = () if gi < 4 else (fc // 2,)
                        for tg in range(4):
                            pq = PQ[0][:, (tg % 2) * 512:(tg % 2 + 1) * 512]
                            pqk = ("PQ", 0, tg % 2)
                            for kc in range(8):
                                op("pe", lambda h, kc=kc, pq=pq, k=k, tg=tg: h.matmul(pq, lhsT=wg[k][:, kc, :], rhs=hT[:, kc, tg * 512:(tg + 1) * 512],
                                                                                     start=(kc == 0), stop=(kc == 7)),
                                   reads=[("wg", k), ("hT", tg)], writes=[pqk])
                            tk_ = tg % 2
                            op("act", lambda h, pq=pq, tk_=tk_: h.activation(out=th[tk_][:], in_=pq, func=AF.Tanh, scale=0.5), reads=[pqk], writes=[("th", tk_)])
                            op("dve", lambda h, pq=pq, tk_=tk_: h.scalar_tensor_tensor(out=pa[tk_][:], in0=th[tk_][:], scalar=1.0, in1=pq, op0=ALU.add, op1=ALU.mult),
                               reads=[pqk, ("th", tk_)], writes=[("pa", tk_)])
                            akeys = [(blk, ii) + bsuf for ii in range(tg * 4, tg * 4 + 4)]
                            op("dve", lambda h, tk_=tk_, attT=attT, fc=fc, tg=tg: h.scalar_tensor_tensor(
                                out=attT[:, fc, tg * 512:(tg + 1) * 512], in0=pa[tk_][:], scalar=0.5, in1=attT[:, fc, tg * 512:(tg + 1) * 512],
                                op0=ALU.mult, op1=ALU.mult), reads=[("pa", tk_)] + akeys, writes=akeys)
                    load_cast(wo, "wo", "wo", w_o_d, 8, D, None, kcstep=2)
                    load_cast(wpg, "wpg", "wpg", w_pg_d, 8, D, None, kcstep=2)
                    load_cast(wp, "wp", "wp", w_p_d, 2, D, None, kcstep=2)

                    for half in range(2):
                        for dc in range(8):
                            k = dc % 2
                            load_cast(wmc[k], ("wmc", k), "wmc%d" % k, w_m_d, 8, 128, (dc * 128, 0))
                            load_cast(wmc[k], ("wmc", k), "wmc%d" % k, w_m_d, 8, 128, (D + dc * 128, 128))
                            load_cast(wbr[k], ("wbr", k), "wbr%d" % k, w_ba_d, 4, 128, (dc * 128, 0))
                            load_cast(wbr[k], ("wbr", k), "wbr%d" % k, w_bb_d, 4, 128, (dc * 128, 128))
                            for tgl in range(2):
                                tg = half * 2 + tgl
                                tsl = slice(tg * 512, (tg + 1) * 512)
                                sl2 = slice(tgl * 512, (tgl + 1) * 512)
                                ma, mb_, ya = PQ[0][:, sl2], PQ[1][:, sl2], PQ[2][:, sl2]
                                yb = PR[:, :]
                                for kc in range(8):
                                    op("pe", lambda h, kc=kc, ma=ma, k=k, tsl=tsl: h.matmul(ma, lhsT=wmc[k][:, kc, 0:128], rhs=hT[:, kc, tsl], start=(kc == 0), stop=(kc == 7)),
                                       reads=[("wmc", k), ("hT", tg)], writes=[("PQ", 0, tgl)])
                                for kc in range(8):
                                    op("pe", lambda h, kc=kc, mb_=mb_, k=k, tsl=tsl: h.matmul(mb_, lhsT=wmc[k][:, kc, 128:256], rhs=hT[:, kc, tsl], start=(kc == 0), stop=(kc == 7)),
                                       reads=[("wmc", k), ("hT", tg)], writes=[("PQ", 1, tgl)])
                                ak = [("qa_blk", ii) for ii in range(tg * 4, tg * 4 + 4)]
                                bk = [("qb_blk", ii, hg_) for ii in range(tg * 4, tg * 4 + 4) for hg_ in range(2)]
                                for fc in range(4):
                                    op("pe", lambda h, fc=fc, ya=ya, k=k, tsl=tsl: h.matmul(ya, lhsT=wbr[k][:, fc, 0:128], rhs=qaT[:, fc, tsl], start=(fc == 0), stop=(fc == 3)),
                                       reads=[("wbr", k)] + ak, writes=[("PQ", 2, tgl)])
                                for fc in range(4):
                                    op("pe", lambda h, fc=fc, yb=yb, k=k, tsl=tsl: h.matmul(yb, lhsT=wbr[k][:, fc, 128:256], rhs=qbT[:, fc, tsl], start=(fc == 0), stop=(fc == 3)),
                                       reads=[("wbr", k)] + bk, writes=["PR"])
                                op("act", lambda h, ma=ma, tgl=tgl: h.activation(out=th[tgl][:], in_=ma, func=AF.Tanh, scale=0.5), reads=[("PQ", 0, tgl)], writes=[("th", tgl)])
                                op("act", lambda h, mb_=mb_, tgl=tgl: h.activation(out=th[2 + tgl][:], in_=mb_, func=AF.Tanh, scale=0.5), reads=[("PQ", 1, tgl)], writes=[("th", 2 + tgl)])
                                op("dve", lambda h, ya=ya, tgl=tgl: h.scalar_tensor_tensor(out=pa[tgl][:], in0=th[tgl][:], scalar=1.0, in1=ya, op0=ALU.add, op1=ALU.mult),
                                   reads=[("th", tgl), ("PQ", 2, tgl)], writes=[("pa", tgl)])
                                op("dve", lambda h, yb=yb, tgl=tgl: h.scalar_tensor_tensor(out=pb[tgl][:], in0=th[2 + tgl][:], scalar=1.0, in1=yb, op0=ALU.add, op1=ALU.mult),
                                   reads=[("th", 2 + tgl), "PR"], writes=[("pb", tgl)])
                                op("pool", lambda h, tgl=tgl, dc=dc, sl2=sl2: h.tensor_tensor(out=mT[:, dc, sl2], in0=pa[tgl][:], in1=pb[tgl][:], op=ALU.add),
                                   reads=[("pa", tgl), ("pb", tgl)], writes=[("mT", tgl)])
                        for tt in range(8):
                            t = half * 8 + tt
                            xk = t % 2
                            tsl = slice(tt * 128, (tt + 1) * 128)
                            dma_in("sp", x3[xk][:], x_d[b, t * 128:(t + 1) * 128, :], ("x3", xk), "x3_%d" % xk)
                            op("pool", lambda h, xk=xk, t=t: h.dma_start(out=pbf[xk][:], in_=p_d[b, t * 128:(t + 1) * 128, :]), writes=[("pbf", xk)], lane="pbf%d" % xk)
                            for hf in range(2):
                                for dc in range(8):
                                    op("pe", lambda h, hf=hf, dc=dc, tsl=tsl: h.matmul(PQ[0][:, hf * 512:(hf + 1) * 512], lhsT=mT[:, dc, tsl], rhs=wo[:, dc, hf * 512:(hf + 1) * 512],
                                                                                      start=(dc == 0), stop=(dc == 7)),
                                       reads=[("mT", tt // 4), "wo"], writes=[("PQ", 0, hf)])
                            OK_ = [("PQ", 0, 0), ("PQ", 0, 1)]
                            op("act", lambda h: h.activation(out=junk3[:], in_=PQ[0][:, :], func=AF.Square, accum_out=s3[:, 0:1]), reads=OK_, writes=["junk3", "s3a"])
                            op("dve", lambda h: h.tensor_scalar(out=s3[:, 1:2], in0=s3[:, 0:1], scalar1=1.0 / D, scalar2=4.0 * EPS, op0=ALU.mult, op1=ALU.add),
                               reads=["s3a"], writes=["s3b"])
                            op("pool", lambda h: h.tensor_tensor(out=s3[:, 2:3], in0=s3[:, 1:2], in1=negh[:], op=ALU.pow), reads=["s3b", "negh"], writes=["s3c"])
                            op("dve", lambda h: h.scalar_tensor_tensor(out=tA[:], in0=PQ[0][:, :], scalar=s3[:, 2:3], in1=gpost[:], op0=ALU.mult, op1=ALU.mult),
                               reads=OK_ + ["s3c", "gpost"], writes=["tA"])
                            op("pool", lambda h, xk=xk: h.tensor_tensor(out=x1[:], in0=tA[:], in1=x3[xk][:], op=ALU.add), reads=["tA", ("x3", xk)], writes=["x1"])
                            op("act", lambda h: h.copy(out=x1b[:], in_=x1[:]), reads=["x1"], writes=["x1b"])
                            for kc in range(8):
                                op("pe", lambda h, kc=kc: h.transpose(out=PT3[:, kc * 128:(kc + 1) * 128], in_=x1b[:, kc * 128:(kc + 1) * 128], identity=ident_bf[:]),
                                   reads=["x1b", "ident_bf"], writes=["PT3"])
                            op("act", lambda h: h.copy(out=x1T[:], in_=PT3[:, :].rearrange("p (kc n) -> p kc n", kc=8)), reads=["PT3"], writes=["x1T"])
                            for hf in range(2):
                                for dc in range(8):
                                    op("pe", lambda h, hf=hf, dc=dc: h.matmul(PQ[1][:, hf * 512:(hf + 1) * 512], lhsT=x1T[:, dc, :], rhs=wpg[:, dc, hf * 512:(hf + 1) * 512],
                                                                             start=(dc == 0), stop=(dc == 7)),
                                       reads=["x1T", "wpg"], writes=[("PQ", 1, hf)])
                            for pc in range(2):
                                op("pe", lambda h, pc=pc, xk=xk: h.transpose(out=PT3[:, pc * 128:(pc + 1) * 128], in_=pbf[xk][:, pc * 128:(pc + 1) * 128], identity=ident_bf[:]),
                                   reads=[("pbf", xk), "ident_bf"], writes=["PT3"])
                            op("dve", lambda h: h.tensor_copy(out=pT[:], in_=PT3[:, 0:256].rearrange("p (kc n) -> p kc n", kc=2)), reads=["PT3"], writes=["pT"])
                            for hf in range(2):
                                for pc in range(2):
                                    op("pe", lambda h, hf=hf, pc=pc: h.matmul(PQ[2][:, hf * 512:(hf + 1) * 512], lhsT=pT[:, pc, :], rhs=wp[:, pc, hf * 512:(hf + 1) * 512],
                                                                             start=(pc == 0), stop=(pc == 1)),
                                       reads=["pT", "wp"], writes=[("PQ", 2, hf)])
                            GK = [("PQ", 1, 0), ("PQ", 1, 1)]
                            EK = [("PQ", 2, 0), ("PQ", 2, 1)]
                            op("act", lambda h: h.activation(out=gth[:], in_=PQ[1][:, :], func=AF.Tanh, scale=0.5), reads=GK, writes=["gth"])
                            op("dve", lambda h: h.scalar_tensor_tensor(out=ge2[:], in0=gth[:], scalar=1.0, in1=PQ[2][:, :], op0=ALU.add, op1=ALU.mult),
                               reads=["gth"] + EK, writes=["ge2"])
                            op("act", lambda h: h.activation(out=junk3[:], in_=ge2[:], func=AF.Square, accum_out=s3[:, 3:4]), reads=["ge2"], writes=["junk3", "s3d"])
                            op("dve", lambda h: h.tensor_scalar(out=s3[:, 4:5], in0=s3[:, 3:4], scalar1=1.0 / D, scalar2=4.0 * EPS, op0=ALU.mult, op1=ALU.add),
                               reads=["s3d"], writes=["s3e"])
                            op("pool", lambda h: h.tensor_tensor(out=s3[:, 5:6], in0=s3[:, 4:5], in1=negh[:], op=ALU.pow), reads=["s3e", "negh"], writes=["s3f"])
                            op("dve", lambda h: h.scalar_tensor_tensor(out=tA[:], in0=ge2[:], scalar=s3[:, 5:6], in1=gple[:], op0=ALU.mult, op1=ALU.mult),
                               reads=["ge2", "s3f", "gple"], writes=["tA"])
                            op("pool", lambda h, xk=xk: h.tensor_tensor(out=fin[xk][:], in0=tA[:], in1=x1[:], op=ALU.add), reads=["tA", "x1"], writes=[("fin", xk)])
                            out_marks.append(op("sp", lambda h, xk=xk, t=t: h.dma_start(out=out_d[b, t * 128:(t + 1) * 128, :], in_=fin[xk][:]),
                                                reads=[("fin", xk)], lane="fin%d" % xk))
        last = {}
        for (sk, v) in [m for m in out_marks if m]:
            last[sk] = max(last.get(sk, 0), v)
        T.wait_marks("sp", list(last.items()))
    T.close()
    build_program.stats = (T.nops, T.nwaits)
    return nc


def _consts():
    half = 8
    freqs = 500000.0 ** (-np.arange(half, dtype=np.float64) / half)
    pos = np.arange(S, dtype=np.float64)
    cosT = np.ones((128, S), np.float64)
    sinT = np.zeros((128, S), np.float64)
    perm = np.zeros((128, 128), np.float32)
    for hh in range(2):
        for d in range(16):
            f = hh * 64 + d
            ang = pos * freqs[d % 8]
            cosT[f] = np.cos(ang)
            sinT[f] = np.sin(ang)
            if d < 8:
                perm[f + 8, f] = -1.0
            else:
                perm[f - 8, f] = 1.0
    idx = np.arange(128)
    ident = np.eye(128, dtype=np.float32)
    trineg = np.where(idx[:, None] <= idx[None, :], 0.0, MASKNEG).astype(np.float32)
    causneg = np.where(idx[None, :] <= idx[:, None], 0.0, -1.0e9).astype(np.float32)
    U = (idx[:, None] <= idx[None, :]).astype(np.float32)
    sel0 = np.zeros((128, 128), np.float32)
    sel0[0, :] = 1.0
    ones = np.ones((128, 128), np.float32)
    cst = np.stack([ident, perm, trineg, causneg, U, sel0, ones], axis=1).astype(np.float32)
    pw2 = np.tile((0.5 ** np.arange(1, NIT + 1, dtype=np.float64))[None, :], (128, 1)).astype(np.float32)
    return cosT.astype(np.float32), sinT.astype(np.float32), np.ascontiguousarray(cst), pw2


def _run(inputs, nseq, seq_ids_per_core):
    x = np.asarray(inputs["x"], np.float32)
    p = np.asarray(inputs["p"], np.float32)[0]
    cosT, sinT, cst, pw2 = _consts()
    rep = lambda v: np.ascontiguousarray(np.broadcast_to(np.asarray(v, np.float32).reshape(1, -1), (128, np.asarray(v).size)))
    common = {
        "w_in": np.ascontiguousarray(np.asarray(inputs["w_in"], np.float32)[0]),
        "w_ba": np.ascontiguousarray(np.asarray(inputs["w_branch_a"], np.float32)[0]),
        "w_bb": np.ascontiguousarray(np.asarray(inputs["w_branch_b"], np.float32)[0]),
        "w_m": np.ascontiguousarray(np.asarray(inputs["w_merge"], np.float32)[0]),
        "w_o": np.ascontiguousarray(np.asarray(inputs["w_out"], np.float32)[0]),
        "w_p": np.ascontiguousarray(np.asarray(inputs["w_ple"], np.float32)[0]),
        "w_pg": np.ascontiguousarray(np.asarray(inputs["w_ple_gate"], np.float32)[0]),
        "gpre": rep(inputs["g_pre"][0]), "gpost": rep(inputs["g_post"][0]), "gple": rep(inputs["g_ple"][0]),
        "bfb": rep(inputs["b_forget"][0]),
        "cosT": cosT, "sinT": sinT, "cst": cst, "pw2": pw2,
    }
    nc = build_program(nseq)
    in_maps = []
    for c in range(NCORES):
        ids = seq_ids_per_core[c]
        m = dict(common)
        m["x"] = np.ascontiguousarray(x[ids])
        m["p"] = np.ascontiguousarray(p[ids])
        in_maps.append(m)
    res = run_bass_kernel_spmd(nc, in_maps, core_ids=list(range(NCORES)))
    return [r["out"] for r in res.results]


def kernel(**inputs):
    B = np.asarray(inputs["x"]).shape[0]
    nseq = B // NCORES
    ids = [list(range(c * nseq, (c + 1) * nseq)) for c in range(NCORES)]
    outs = _run(inputs, nseq, ids)
    return np.concatenate(outs, axis=0).astype(np.float32)
```

```python
import contextlib
import os
import numpy as np
import concourse.bass as bass
import concourse.mybir as mybir
from concourse.bass_utils import run_bass_kernel_spmd

F32 = mybir.dt.float32
BF16 = mybir.dt.bfloat16
ALU = mybir.AluOpType
AF = mybir.ActivationFunctionType
AX = mybir.AxisListType

NCORES = 8
S = 2048
D = 1024
NT = 16
DIN = 3792
OFF = dict(qa=0, ka=512, va=576, ga=640, qi=1152, ki=1664, wi=1728, qb=1736, kb=2248, vb=2760, fb=3272, gb=3280)
EPS = 1e-6
NIT = 20
STAGE = float(os.environ.get('MK_STAGE', '9'))
ATTACH = int(os.environ.get('MK_ATTACH', '0'))
TOPK = 256
MASKNEG = -30000.0
IDX_SCALE = (8 ** -0.5) * (64 ** -0.5)

ENGS = ("pe", "act", "dve", "pool", "sp")


class Trk:
    def __init__(self, nc):
        self.nc = nc
        self.h = {"pe": nc.tensor, "act": nc.scalar, "dve": nc.vector, "pool": nc.gpsimd, "sp": nc.sync}
        self.stack = contextlib.ExitStack()
        self.sems = {e: self.stack.enter_context(nc.semaphore("s_" + e)) for e in ENGS}
        self.cnt = {e: 0 for e in ENGS}
        self.lanecnt = {}
        self.clock = {e: {} for e in ENGS}
        self.snap = {}
        self.res = {}
        self.nwaits = 0
        self.nops = 0
        self.enabled = True
        self.pending = {e: None for e in ENGS}

    def _sem(self, sk):
        s = self.sems.get(sk)
        if s is None:
            s = self.stack.enter_context(self.nc.semaphore("l_%d" % len(self.sems)))
            self.sems[sk] = s
        return s

    def op(self, eng, fn, reads=(), writes=(), lane=None):
        if not self.enabled:
            return None
        deps = []
        for r in reads:
            st = self.res.get(r)
            if st and st[0]:
                deps.append(st[0])
        for w in writes:
            st = self.res.get(w)
            if st:
                if st[0]:
                    deps.append(st[0])
                deps.extend(st[1])
        clk = self.clock[eng]
        waits = {}
        if self.pending[eng]:
            deps.extend(self.pending[eng].items())
            self.pending[eng] = None
        for (sk, v) in deps:
            if sk == eng and eng == "pe":
                continue
            if clk.get(sk, 0) >= v:
                continue
            if waits.get(sk, 0) < v:
                waits[sk] = v
        if waits:
            clk = dict(clk)
            for sk, v in waits.items():
                sn = self.snap.get((sk, v))
                if sn:
                    for k2, v2 in sn.items():
                        if clk.get(k2, 0) < v2:
                            clk[k2] = v2
                if clk.get(sk, 0) < v:
                    clk[sk] = v
            self.clock[eng] = clk
        if lane is None:
            self.cnt[eng] += 1
            me = (eng, self.cnt[eng])
        else:
            sk = ("lane", lane)
            self.lanecnt[lane] = self.lanecnt.get(lane, 0) + 16
            me = (sk, self.lanecnt[lane])
        sn = dict(clk)
        sn[me[0]] = me[1]
        self.snap[me] = sn
        for r in reads:
            st = self.res.setdefault(r, [None, []])
            st[1].append(me)
        for w in writes:
            self.res[w] = [me, []]
        h = self.h[eng]
        wl = list(waits.items())
        self.nwaits += len(wl)
        self.nops += 1
        if wl and ATTACH:
            for sk, v in wl[:-1]:
                h.wait_ge(self._sem(sk), v)
            ins = fn(h)
            sk, v = wl[-1]
            ins = ins._wait_ge(self._sem(sk), v)
        else:
            for sk, v in wl:
                h.wait_ge(self._sem(sk), v)
            ins = fn(h)
        ins.then_inc(self._sem(me[0]), 16 if lane is not None else 1)
        return me

    def fence(self):
        marks = {e: c for e, c in self.cnt.items() if c}
        for ln, c in self.lanecnt.items():
            marks[("lane", ln)] = c
        for e in ENGS:
            self.pending[e] = dict(marks)

    def wait_marks(self, eng, marks):
        h = self.h[eng]
        for sk, v in marks:
            h.wait_ge(self._sem(sk), v)

    def close(self):
        self.stack.close()


def build_program(nseq, dbg=False):
    nc = bass.Bass("TRN2", target_bir_lowering=False)

    def din(name, shape):
        return nc.dram_tensor(name, shape, F32, kind="ExternalInput").ap()

    x_d = din("x", [nseq, S, D])
    p_d = din("p", [nseq, S, 256])
    w_in_d = din("w_in", [D, DIN])
    w_ba_d = din("w_ba", [512, D])
    w_bb_d = din("w_bb", [512, D])
    w_m_d = din("w_m", [D, 2 * D])
    w_o_d = din("w_o", [D, D])
    w_p_d = din("w_p", [256, D])
    w_pg_d = din("w_pg", [D, D])
    gpre_d = din("gpre", [128, D])
    gpost_d = din("gpost", [128, D])
    gple_d = din("gple", [128, D])
    bfb_d = din("bfb", [128, 8])
    cos_d = din("cosT", [128, S])
    sin_d = din("sinT", [128, S])
    cst_d = din("cst", [128, 7, 128])
    pw2_d = din("pw2", [128, NIT])
    out_d = nc.dram_tensor("out", [nseq, S, D], F32, kind="ExternalOutput").ap()
    dbg_d = {}

    T = Trk(nc)
    op = T.op
    uid = [0]

    def dma_in(eng, out_ap, in_ap, key, lane):
        return op(eng, lambda h: h.dma_start(out=out_ap, in_=in_ap), writes=[key], lane=lane)

    with contextlib.ExitStack() as g:
        def sb(name, shape, dt, es=g):
            uid[0] += 1
            return es.enter_context(nc.sbuf_tensor("%s_u%d" % (name, uid[0]), shape, dt))

        def ps(name, shape, dt, es=g):
            uid[0] += 1
            return es.enter_context(nc.psum_tensor("%s_u%d" % (name, uid[0]), shape, dt))

        cst = sb("cst", [128, 7, 128], F32)
        ident_bf = sb("ident_bf", [128, 128], BF16)
        trineg_bf = sb("trineg_bf", [128, 128], BF16)
        irep_bf = sb("irep_bf", [128, 512], BF16)
        ones_bf = sb("ones_bf", [128, 64], BF16)
        gpre = sb("gpre_s", [128, D], F32)
        gpost = sb("gpost_s", [128, D], F32)
        gple = sb("gple_s", [128, D], F32)
        bfb = sb("bfb_s", [128, 8], F32)
        pw2 = sb("pw2_s", [128, NIT], F32)
        negh = sb("negh", [128, 1], F32)
        taum = sb("taum", [128, 1], F32)
        ident_f = cst[:, 0, :]
        perm_f = cst[:, 1, :]
        causneg_f = cst[:, 3, :]
        U_f = cst[:, 4, :]
        sel0_f = cst[:, 5, :]
        ones_f = cst[:, 6, :]

        dma_in("sp", cst[:], cst_d[:, :, :], "cst", "c0")
        dma_in("sp", gpre[:], gpre_d[:, :], "gpre", "c1")
        dma_in("sp", gpost[:], gpost_d[:, :], "gpost", "c2")
        dma_in("sp", gple[:], gple_d[:, :], "gple", "c3")
        dma_in("sp", bfb[:], bfb_d[:, :], "bfb", "c4")
        dma_in("sp", pw2[:], pw2_d[:, :], "pw2", "c5")
        dma_in("pool", ident_bf[:], cst_d[:, 0, :], "ident_bf", "c6")
        dma_in("pool", trineg_bf[:], cst_d[:, 2, :], "trineg_bf", "c7")
        for k in range(4):
            op("pool", lambda h, k=k: h.dma_start(out=irep_bf[:, k * 128:(k + 1) * 128], in_=cst_d[:, 0, :]),
               writes=[("irep", k)], lane="c8")
        IREP = [("irep", k) for k in range(4)]
        op("pool", lambda h: h.memset(ones_bf[:], 1.0), writes=["ones_bf"])
        op("pool", lambda h: h.memset(negh[:], -0.5), writes=["negh"])
        op("pool", lambda h: h.memset(taum[:], -1.0e8), writes=["taum"])

        out_marks = []

        for b in range(nseq):
            T.fence()
            with contextlib.ExitStack() as sq:
                hT = sb("hT", [128, 8, S], BF16, sq)
                qaT = sb("qaT", [128, 4, S], BF16, sq)
                qbT = sb("qbT", [128, 4, S], BF16, sq)
                with contextlib.ExitStack() as p12:
                    qiT = sb("qiT", [128, 4, S], BF16, p12)
                    kaT = sb("kaT", [128, S], BF16, p12)
                    kiT = sb("kiT", [128, S], BF16, p12)
                    kbT = sb("kbT", [128, 4, S], BF16, p12)
                    va = sb("va", [128, NT, 128], BF16, p12)
                    vb = sb("vb", [128, NT, 512], BF16, p12)
                    wi = sb("wi", [128, NT, 8], F32, p12)
                    fbs = sb("fbs", [128, NT, 8], F32, p12)
                    csb = sb("csb", [128, NT, 8], F32, p12)
                    carry = sb("carry", [128, NT, 8], F32, p12)
                    with contextlib.ExitStack() as p1:
                        T.enabled = STAGE >= 1
                        cosT = sb("cosT_s", [128, S], F32, p1)
                        sinT = sb("sinT_s", [128, S], F32, p1)
                        xt = [sb("xt%d" % k, [128, D], F32, p1) for k in range(2)]
                        junk = sb("junk1", [128, D], BF16, p1)
                        hb = [sb("hb%d" % k, [128, D], BF16, p1) for k in range(2)]
                        wbuf = [sb("wbuf%d" % k, [128, 8, 512], BF16, p1) for k in range(2)]
                        xs = [sb("xs%d" % k, [128, 512], F32, p1) for k in range(2)]
                        t1 = [sb("t1_0", [128, 512], F32, p1)] * 2
                        t2 = [sb("t2_0", [128, 512], F32, p1)] * 2
                        st = sb("st1", [128, 4], F32, p1)
                        wsm = sb("wsm", [128, 8, 80], BF16, p1)
                        sm16 = sb("sm16", [128, 16], F32, p1)
                        PA = [ps("PA%d" % k, [128, 512], F32, p1) for k in range(4)]
                        PB = [ps("PB%d" % k, [128, 512], F32, p1) for k in range(2)]
                        PTB = ps("PTB1", [128, 1024], BF16, p1)
                        PC = ps("PC1", [128, 512], F32, p1)

                        dma_in("sp", cosT[:], cos_d[:, :], "cosT", "cos")
                        dma_in("sp", sinT[:], sin_d[:, :], "sinT", "sin")
                        op("pool", lambda h: h.memset(va[:, :, 64:128], 1.0), writes=["va_ones"])

                        wl_state = {"n": 0}

                        def load_w(segments):
                            k = wl_state["n"] % 2
                            wl_state["n"] += 1
                            c = 0
                            for (c0, ncol) in segments:
                                for kc0 in range(0, 8, 4):
                                    op("pool", lambda h, k=k, c=c, c0=c0, ncol=ncol, kc0=kc0: h.dma_start(
                                        out=wbuf[k][:, kc0:kc0 + 4, c:c + ncol],
                                        in_=w_in_d[kc0 * 128:(kc0 + 4) * 128, c0:c0 + ncol].rearrange("(kc kp) n -> kp kc n", kp=128)),
                                       writes=[("wbuf", k)], lane="wbuf%d" % k)
                                c += ncol
                            return k

                        for t in range(NT):
                            k = t % 2
                            dma_in("sp", xt[k][:], x_d[b, t * 128:(t + 1) * 128, :], ("xt", k), "xt%d" % k)
                            op("act", lambda h, k=k: h.activation(out=junk[:], in_=xt[k][:], func=AF.Square, accum_out=st[:, 0:1]),
                               reads=[("xt", k)], writes=["junk1", "st0"])
                            op("dve", lambda h: h.tensor_scalar(out=st[:, 1:2], in0=st[:, 0:1], scalar1=1.0 / D, scalar2=EPS,
                                                                op0=ALU.mult, op1=ALU.add), reads=["st0"], writes=["st1"])
                            op("pool", lambda h: h.tensor_tensor(out=st[:, 2:3], in0=st[:, 1:2], in1=negh[:], op=ALU.pow),
                               reads=["st1", "negh"], writes=["st2"])
                            op("dve", lambda h, k=k: h.scalar_tensor_tensor(out=hb[k][:], in0=xt[k][:], scalar=st[:, 2:3], in1=gpre[:],
                                                                            op0=ALU.mult, op1=ALU.mult),
                               reads=[("xt", k), "st2", "gpre"], writes=[("hb", k)])
                            for kc in range(8):
                                op("pe", lambda h, k=k, kc=kc: h.transpose(out=PTB[:, kc * 128:(kc + 1) * 128], in_=hb[k][:, kc * 128:(kc + 1) * 128],
                                                                           identity=ident_bf[:]),
                                   reads=[("hb", k), "ident_bf"], writes=["PTB1"])
                            op("act", lambda h, t=t: h.copy(out=hT[:, :, t * 128:(t + 1) * 128], in_=PTB[:, :].rearrange("p (kc n) -> p kc n", kc=8)),
                               reads=["PTB1"], writes=[("hT", t // 4)])

                        T.enabled = STAGE >= 1.2
                        pa_i = [0]

                        def proj_fm(k, cofs, tg):
                            slot = pa_i[0] % 4
                            pa_i[0] += 1
                            for kc in range(8):
                                op("pe", lambda h, kc=kc, slot=slot: h.matmul(PA[slot][:, :], lhsT=wbuf[k][:, kc, cofs:cofs + 128],
                                                                             rhs=hT[:, kc, tg * 512:(tg + 1) * 512], start=(kc == 0), stop=(kc == 7)),
                                   reads=[("wbuf", k), ("hT", tg)], writes=[("PA", slot)])
                            return slot

                        rp_i = [0]

                        def rope_evac(slot, dst_ap, dst_key, tg):
                            r = rp_i[0] % 2
                            rp_i[0] += 1
                            tsl = slice(tg * 512, (tg + 1) * 512)
                            op("act", lambda h: h.copy(out=xs[r][:], in_=PA[slot][:, :]), reads=[("PA", slot)], writes=[("xs", r)])
                            op("pe", lambda h: h.matmul(PB[r][:, :], lhsT=perm_f, rhs=xs[r][:], start=True, stop=True),
                               reads=[("xs", r), "cst"], writes=[("PB", r)])
                            op("dve", lambda h: h.tensor_tensor(out=t1[r][:], in0=PB[r][:, :], in1=sinT[:, tsl], op=ALU.mult),
                               reads=[("PB", r), "sinT"], writes=["t1"])
                            op("pool", lambda h: h.tensor_tensor(out=t2[r][:], in0=xs[r][:], in1=cosT[:, tsl], op=ALU.mult),
                               reads=[("xs", r), "cosT"], writes=["t2"])
                            op("dve", lambda h: h.tensor_tensor(out=dst_ap, in0=t1[r][:], in1=t2[r][:], op=ALU.add),
                               reads=["t1", "t2"], writes=[dst_key])

                        for (nm, dst) in (("qa", qaT), ("qi", qiT)):
                            k = load_w([(OFF[nm], 512)])
                            for c in range(4):
                                for tg in range(4):
                                    slot = proj_fm(k, c * 128, tg)
                                    rope_evac(slot, dst[:, c, tg * 512:(tg + 1) * 512], (nm + "T", c, tg), tg)
                        T.enabled = STAGE >= 1.3
                        k = load_w([(OFF["ka"], 64), (OFF["ka"], 64), (OFF["ki"], 64), (OFF["ki"], 64)])
                        for c, (nm, dst) in enumerate((("ka", kaT), ("ki", kiT))):
                            for tg in range(4):
                                slot = proj_fm(k, c * 128, tg)
                                rope_evac(slot, dst[:, tg * 512:(tg + 1) * 512], (nm + "T", tg), tg)
                        T.enabled = STAGE >= 1.4
                        for (nm, dst) in (("qb", qbT), ("kb", kbT)):
                            k = load_w([(OFF[nm], 512)])
                            for c in range(4):
                                for tg in range(4):
                                    slot = proj_fm(k, c * 128, tg)
                                    op("act", lambda h, slot=slot, dst=dst, c=c, tg=tg: h.copy(out=dst[:, c, tg * 512:(tg + 1) * 512], in_=PA[slot][:, :]),
                                       reads=[("PA", slot)], writes=[(nm + "T", c, tg)])
                        T.enabled = STAGE >= 1.5
                        k = load_w([(OFF["vb"], 512)])
                        k2 = load_w([(OFF["va"], 64), (OFF["wi"] - 64, 72), (OFF["fb"] - 64, 72)])
                        op("dve", lambda h: h.tensor_copy(out=wsm[:, :, 0:64], in_=wbuf[k2][:, :, 0:64]), reads=[("wbuf", k2)], writes=["wsm"])
                        op("dve", lambda h: h.tensor_copy(out=wsm[:, :, 64:72], in_=wbuf[k2][:, :, 128:136]), reads=[("wbuf", k2)], writes=["wsm"])
                        op("dve", lambda h: h.tensor_copy(out=wsm[:, :, 72:80], in_=wbuf[k2][:, :, 200:208]), reads=[("wbuf", k2)], writes=["wsm"])
                        for t in range(NT):
                            slot = pa_i[0] % 4
                            pa_i[0] += 1
                            for kc in range(8):
                                op("pe", lambda h, kc=kc, slot=slot, t=t: h.matmul(PA[slot][:, :], lhsT=hT[:, kc, t * 128:(t + 1) * 128],
                                                                                  rhs=wbuf[k][:, kc, 0:512], start=(kc == 0), stop=(kc == 7)),
                                   reads=[("wbuf", k), ("hT", t // 4)], writes=[("PA", slot)])
                            op("act", lambda h, slot=slot, t=t: h.copy(out=vb[:, t, :], in_=PA[slot][:, :]), reads=[("PA", slot)], writes=[("vb", t)])
                            if STAGE < 1.6:
                                continue
                            for kc in range(8):
                                op("pe", lambda h, kc=kc, t=t: h.matmul(PC[:, 0:80], lhsT=hT[:, kc, t * 128:(t + 1) * 128],
                                                                        rhs=wsm[:, kc, 0:80], start=(kc == 0), stop=(kc == 7)),
                                   reads=["wsm", ("hT", t // 4)], writes=["PC1"])
                            op("act", lambda h, t=t: h.copy(out=va[:, t, 0:64], in_=PC[:, 0:64]), reads=["PC1"], writes=[("va", t)])
                            op("act", lambda h: h.copy(out=sm16[:], in_=PC[:, 64:80]), reads=["PC1"], writes=["sm16"])
                            op("act", lambda h, t=t: h.mul(out=wi[:, t, :], in_=sm16[:, 0:8], mul=IDX_SCALE),
                               reads=["sm16"], writes=[("wi", t)])
                            op("pool", lambda h, t=t: h.tensor_tensor(out=fbs[:, t, :], in0=sm16[:, 8:16], in1=bfb[:], op=ALU.add),
                               reads=["sm16", "bfb"], writes=["fbs"])
                    T.fence()
                    with contextlib.ExitStack() as p2:
                        T.enabled = STAGE >= 2
                        score = [sb("score%d" % k, [128, S], F32, p2) for k in range(2)]
                        tmp = [sb("tmp%d" % k, [128, 512], F32, p2) for k in range(2)]
                        junk2 = sb("junk2", [128, S], BF16, p2)
                        mneg = [sb("mneg%d" % k, [128, S], BF16, p2) for k in range(2)]
                        ptd = [sb("ptd%d" % k, [128, 512], BF16, p2) for k in range(3)]
                        ptf = [sb("ptf%d" % k, [128, 128], BF16, p2) for k in range(4)]
                        dend = sb("dend", [128, 1024], F32, p2)
                        denf = sb("denf", [128, 512], F32, p2)
                        sst = [sb("sst%d" % k, [128, 8 + NIT], F32, p2) for k in range(2)]
                        crefb = sb("crefb", [128, NT, 8], F32, p2)
                        biasT = sb("biasT", [128, NT, NT, 8], F32, p2)
                        IDX = ps("IDX", [128, 512], F32, p2)
                        DS = [ps("DS%d" % k, [128, 512], F32, p2) for k in range(2)]
                        DACC = ps("DACC", [128, 1024], F32, p2)
                        FS = [ps("FS%d" % k, [128, 512], F32, p2) for k in range(2)]
                        FACC = ps("FACC", [128, 512], F32, p2)
                        cnt = {"idx": 0, "ptd": 0, "fs": 0}
                        fl = fbs[:].rearrange("p t h -> p (t h)")
                        cl = csb[:].rearrange("p t h -> p (t h)")
                        op("act", lambda h: h.activation(out=fl, in_=fl, func=AF.Exp, scale=-1.0), reads=["fbs"], writes=["fbs"])
                        op("act", lambda h: h.activation(out=fl, in_=fl, func=AF.Ln, bias=1.0, scale=1.0), reads=["fbs"], writes=["fbs"])
                        op("dve", lambda h: h.tensor_scalar(out=fl, in0=fl, scalar1=-1.0, scalar2=None, op0=ALU.mult), reads=["fbs"], writes=["fbs"])
                        op("pe", lambda h: h.matmul(IDX[:, 0:128], lhsT=U_f, rhs=fl, start=True, stop=True), reads=["fbs", "cst"], writes=[("IDX", 0)])
                        op("pe", lambda h: h.matmul(FS[0][:, 0:128], lhsT=ones_f, rhs=fl, start=True, stop=True), reads=["fbs", "cst"], writes=[("FS", 0)])
                        op("dve", lambda h: h.memset(carry[:, 0, :], 0.0), writes=["carry"])
                        for t in range(1, NT):
                            op("dve", lambda h, t=t: h.tensor_tensor(out=carry[:, t, :], in0=carry[:, t - 1, :], in1=FS[0][:, (t - 1) * 8:t * 8], op=ALU.add),
                               reads=[("FS", 0), "carry"], writes=["carry"])
                        op("dve", lambda h: h.tensor_tensor(out=cl, in0=IDX[:, 0:128], in1=carry[:].rearrange("p t h -> p (t h)"), op=ALU.add),
                           reads=[("IDX", 0), "carry"], writes=["csb"])
                        op("pe", lambda h: h.matmul(IDX[:, 0:128], lhsT=sel0_f, rhs=cl, start=True, stop=True), reads=["csb", "cst"], writes=[("IDX", 0)])
                        op("act", lambda h: h.copy(out=crefb[:].rearrange("p t h -> p (t h)"), in_=IDX[:, 0:128]), reads=[("IDX", 0)], writes=["crefb"])
                        for i in range(NT):
                            op("dve", lambda h, i=i: h.tensor_tensor(out=biasT[:, i, 0:i + 1, :],
                                                                     in0=crefb[:, i, :].unsqueeze(1).broadcast_to([128, i + 1, 8]),
                                                                     in1=csb[:, 0:i + 1, :], op=ALU.subtract),
                               reads=["crefb", "csb"], writes=["biasT"])

                        def dsa_index(i):
                            items = []
                            sp_ = i % 2
                            L = 128 * (i + 1)
                            sc = score[sp_]
                            nch = (L + 511) // 512
                            s_ = sst[sp_]
                            SK = ("sst", sp_)
                            for hd in range(8):
                                c, r = hd // 2, hd % 2
                                for kk in range(nch):
                                    def chunk(hd=hd, c=c, r=r, kk=kk):
                                        w = min(512, L - 512 * kk)
                                        sl = cnt["idx"] % 2
                                        cnt["idx"] += 1
                                        op("pe", lambda h: h.matmul(
                                            IDX[:, 0:w], lhsT=qiT[64 * r:64 * r + 64, c, i * 128:(i + 1) * 128],
                                            rhs=kiT[64 * r:64 * r + 64, kk * 512:kk * 512 + w], start=True, stop=True),
                                           reads=[("qiT", c, i // 4), ("kiT", kk)], writes=[("IDX", 0)])
                                        op("act", lambda h: h.activation(out=tmp[sl][:, 0:w], in_=IDX[:, 0:w], func=AF.Relu),
                                           reads=[("IDX", 0)], writes=[("tmp", sl)])
                                        if hd == 0:
                                            op("dve", lambda h: h.tensor_scalar(
                                                out=sc[:, kk * 512:kk * 512 + w], in0=tmp[sl][:, 0:w], scalar1=wi[:, i, 0:1], scalar2=None, op0=ALU.mult),
                                               reads=[("tmp", sl), ("wi", i)], writes=[("score", sp_)])
                                        else:
                                            op("dve", lambda h: h.scalar_tensor_tensor(
                                                out=sc[:, kk * 512:kk * 512 + w], in0=tmp[sl][:, 0:w], scalar=wi[:, i, hd:hd + 1],
                                                in1=sc[:, kk * 512:kk * 512 + w], op0=ALU.mult, op1=ALU.add),
                                               reads=[("tmp", sl), ("wi", i), ("score", sp_)], writes=[("score", sp_)])
                                    items.append(chunk)

                            def prep():
                                op("dve", lambda h: h.tensor_tensor(out=sc[:, i * 128:L], in0=sc[:, i * 128:L], in1=causneg_f, op=ALU.add),
                                   reads=[("score", sp_), "cst"], writes=[("score", sp_)])
                                if i >= 2:
                                    op("dve", lambda h: h.tensor_reduce(out=s_[:, 0:1], in_=sc[:, 0:128 * i], axis=AX.X, op=ALU.min),
                                       reads=[("score", sp_)], writes=[SK])
                                    op("dve", lambda h: h.tensor_reduce(out=s_[:, 1:2], in_=sc[:, 0:L], axis=AX.X, op=ALU.max),
                                       reads=[("score", sp_)], writes=[SK])
                                    op("dve", lambda h: h.tensor_tensor(out=s_[:, 2:3], in0=s_[:, 1:2], in1=s_[:, 0:1], op=ALU.subtract),
                                       reads=[SK], writes=[SK])
                                    op("dve", lambda h: h.tensor_scalar(out=s_[:, 8:8 + NIT], in0=pw2[:], scalar1=s_[:, 2:3], scalar2=None, op0=ALU.mult),
                                       reads=[SK, "pw2"], writes=[SK])
                            items.append(prep)
                            if i >= 2:
                                for it in range(NIT):
                                    def iteration(it=it):
                                        op("dve", lambda h: h.tensor_tensor(out=s_[:, 3:4], in0=s_[:, 0:1], in1=s_[:, 8 + it:9 + it], op=ALU.add),
                                           reads=[SK], writes=[SK])
                                        op("dve", lambda h: h.tensor_scalar(out=junk2[:, 0:L], in0=sc[:, 0:L], scalar1=s_[:, 3:4], scalar2=None,
                                                                            op0=ALU.is_ge, op1=ALU.add, accum_out=s_[:, 4:5]),
                                           reads=[SK, ("score", sp_)], writes=[SK, "junk2"])
                                        op("dve", lambda h: h.scalar_tensor_tensor(out=s_[:, 5:6], in0=s_[:, 4:5], scalar=TOPK - 0.5, in1=s_[:, 8 + it:9 + it],
                                                                                   op0=ALU.is_ge, op1=ALU.mult),
                                           reads=[SK], writes=[SK])
                                        op("dve", lambda h: h.tensor_tensor(out=s_[:, 0:1], in0=s_[:, 0:1], in1=s_[:, 5:6], op=ALU.add),
                                           reads=[SK], writes=[SK])
                                    items.append(iteration)

                            def fin():
                                if i >= 2:
                                    tau, tk = s_[:, 0:1], [SK]
                                else:
                                    tau, tk = taum[:], ["taum"]
                                op("dve", lambda h: h.tensor_scalar(out=mneg[sp_][:, 0:L], in0=sc[:, 0:L], scalar1=tau, scalar2=MASKNEG,
                                                                    op0=ALU.is_lt, op1=ALU.mult),
                                   reads=tk + [("score", sp_)], writes=[("mneg", sp_)])
                            items.append(fin)
                            return items

                        class Pipe:
                            def __init__(self, skew, batch):
                                self.q, self.skew, self.batch, self.pf = [], skew, batch, []

                            def push(self, front, back):
                                self.pf.append((front, back))
                                if len(self.pf) >= self.batch:
                                    self._go()

                            def _go(self):
                                for f, _ in self.pf:
                                    f()
                                self.q.extend(bk for _, bk in self.pf)
                                self.pf = []
                                while len(self.q) > self.skew:
                                    self.q.pop(0)()

                            def flush(self):
                                if self.pf:
                                    self._go()
                                while self.q:
                                    self.q.pop(0)()

                        def dsa_units(i):
                            sp_ = i % 2
                            units = []
                            for j in range(i + 1):
                                for half in range(2):
                                    pk = cnt["ptd"] % 3
                                    cnt["ptd"] += 1

                                    def front(j=j, half=half):
                                        D_ = DS[half]
                                        op("pe", lambda h: h.matmul(D_[:, :], lhsT=mneg[sp_][:, j * 128:(j + 1) * 128], rhs=irep_bf[:],
                                                                    start=True, stop=False),
                                           reads=[("mneg", sp_)] + IREP, writes=[("DS", half)])
                                        for hh in range(4):
                                            c, r = hh, half
                                            op("pe", lambda h, hh=hh, c=c, r=r: h.matmul(
                                                D_[:, hh * 128:(hh + 1) * 128], lhsT=kaT[64 * r:64 * r + 64, j * 128:(j + 1) * 128],
                                                rhs=qaT[64 * r:64 * r + 64, c, i * 128:(i + 1) * 128], start=False, stop=(hh == 3)),
                                               reads=[("kaT", j // 4), ("qaT", c, i // 4), ("qa_blk", i)], writes=[("DS", half)])

                                    def back(j=j, half=half, pk=pk):
                                        D_ = DS[half]
                                        op("act", lambda h: h.activation(out=ptd[pk][:], in_=D_[:, :], func=AF.Exp, scale=0.125),
                                           reads=[("DS", half)], writes=[("ptd", pk)])
                                        op("pe", lambda h: h.matmul(DACC[:, half * 512:(half + 1) * 512], lhsT=va[:, j, :], rhs=ptd[pk][:],
                                                                    start=(j == 0), stop=(j == i)),
                                           reads=[("ptd", pk), ("va", j), "va_ones"], writes=[("DACC", half)])
                                    units.append((front, back))

                            def epi():
                                op("act", lambda h: h.copy(out=dend[64:128, :], in_=DACC[64:128, :]), reads=[("DACC", 0), ("DACC", 1)], writes=["dend"])
                                op("dve", lambda h: h.reciprocal(out=dend[64:128, :], in_=dend[64:128, :]), reads=["dend"], writes=["dend"])
                                for r in range(2):
                                    op("dve", lambda h, r=r: h.tensor_tensor(
                                        out=qaT[64 * r:64 * r + 64, :, i * 128:(i + 1) * 128],
                                        in0=DACC[0:64, r * 512:(r + 1) * 512].rearrange("p (c q) -> p c q", c=4),
                                        in1=dend[64:128, r * 512:(r + 1) * 512].rearrange("p (c q) -> p c q", c=4), op=ALU.mult),
                                       reads=[("DACC", 0), ("DACC", 1), "dend"], writes=[("qa_blk", i)])
                            units.append((lambda: None, epi))
                            return units

                        def fox_units(i):
                            units = []
                            for hg in range(2):
                                for hh in range(4):
                                    hd = hg * 4 + hh
                                    c, r = hd // 2, hd % 2
                                    for j in range(i + 1):
                                        sl = cnt["fs"] % 2
                                        cnt["fs"] += 1

                                        def front(j=j, sl=sl, c=c, r=r):
                                            F_ = FS[sl][:, 0:128]
                                            op("pe", lambda h: h.matmul(
                                                F_, lhsT=kbT[64 * r:64 * r + 64, c, j * 128:(j + 1) * 128],
                                                rhs=qbT[64 * r:64 * r + 64, c, i * 128:(i + 1) * 128], start=True, stop=(j != i)),
                                               reads=[("kbT", c, j // 4), ("qbT", c, i // 4), ("qb_blk", i, c // 2)], writes=[("FS", sl)])
                                            if j == i:
                                                op("pe", lambda h: h.matmul(F_, lhsT=ident_bf[:], rhs=trineg_bf[:], start=False, stop=True),
                                                   reads=["ident_bf", "trineg_bf"], writes=[("FS", sl)])

                                        def back(j=j, sl=sl, hd=hd, hh=hh):
                                            F_ = FS[sl][:, 0:128]
                                            op("act", lambda h: h.activation(out=ptf[sl][:], in_=F_, func=AF.Exp, scale=0.125,
                                                                             bias=biasT[:, i, j, hd:hd + 1]),
                                               reads=[("FS", sl), "biasT"], writes=[("ptf", sl)])
                                            op("pe", lambda h: h.matmul(FACC[0:64, hh * 128:(hh + 1) * 128], lhsT=vb[:, j, hd * 64:(hd + 1) * 64],
                                                                        rhs=ptf[sl][:], start=(j == 0), stop=(j == i)),
                                               reads=[("ptf", sl), ("vb", j)], writes=["FACCn"])
                                            op("pe", lambda h: h.matmul(FACC[64:128, hh * 128:(hh + 1) * 128], lhsT=ones_bf[:, 0:64],
                                                                        rhs=ptf[sl][:], start=(j == 0), stop=(j == i)),
                                               reads=[("ptf", sl), "ones_bf"], writes=["FACCd"])
                                        units.append((front, back))

                                def epi(hg=hg):
                                    FK = ["FACCn", "FACCd"]
                                    op("act", lambda h: h.copy(out=denf[64:128, :], in_=FACC[64:128, :]), reads=FK, writes=["denf"])
                                    op("dve", lambda h: h.reciprocal(out=denf[64:128, :], in_=denf[64:128, :]), reads=["denf"], writes=["denf"])
                                    for r in range(2):
                                        op("dve", lambda h, r=r: h.tensor_tensor(
                                            out=qbT[64 * r:64 * r + 64, 2 * hg:2 * hg + 2, i * 128:(i + 1) * 128],
                                            in0=FACC[0:64, :].rearrange("p (c r q) -> p c r q", c=2, r=2)[:, :, r, :],
                                            in1=denf[64:128, :].rearrange("p (c r q) -> p c r q", c=2, r=2)[:, :, r, :], op=ALU.mult),
                                           reads=FK + ["denf"], writes=[("qb_blk", i, hg)])
                                units.append((None, epi))
                            return units

                        dpipe = Pipe(1, 1)
                        fpipe = Pipe(1, 1)
                        for it_ in dsa_index(0):
                            it_()
                        for i in range(NT):
                            items = dsa_index(i + 1) if i + 1 < NT else []
                            du = dsa_units(i)
                            fu = fox_units(i)
                            nF = len(fu)
                            rate = -(-len(items) // max(1, int(0.7 * nF)))
                            di = 0
                            ii = 0
                            for n_, (f, bk) in enumerate(fu):
                                if f is None:
                                    fpipe.flush()
                                    bk()
                                else:
                                    fpipe.push(f, bk)
                                for _ in range(rate):
                                    if ii < len(items):
                                        items[ii]()
                                        ii += 1
                                if n_ % 4 == 3 and di < len(du):
                                    dpipe.push(*du[di])
                                    di += 1
                            while ii < len(items):
                                items[ii]()
                                ii += 1
                            while di < len(du):
                                dpipe.push(*du[di])
                                di += 1
                        dpipe.flush()
                        fpipe.flush()
                T.fence()
                with contextlib.ExitStack() as p3:
                    T.enabled = STAGE >= 3
                    wo = sb("wo", [128, 8, D], BF16, p3)
                    wpg = sb("wpg", [128, 8, D], BF16, p3)
                    wp = sb("wp", [128, 2, D], BF16, p3)
                    mT = sb("mT", [128, 8, 1024], BF16, p3)
                    wg = [sb("wg%d" % k, [128, 8, 128], BF16, p3) for k in range(2)]
                    wmc = [sb("wmc%d" % k, [128, 8, 256], BF16, p3) for k in range(2)]
                    wbr = [sb("wbr%d" % k, [128, 4, 256], BF16, p3) for k in range(2)]
                    th = [sb("th%d" % k, [128, 512], BF16, p3) for k in range(4)]
                    pa = [sb("pa%d" % k, [128, 512], F32, p3) for k in range(2)]
                    pb = [sb("pb%d" % k, [128, 512], F32, p3) for k in range(2)]
                    x3 = [sb("x3_%d" % k, [128, D], F32, p3) for k in range(2)]
                    pbf = [sb("pbf%d" % k, [128, 256], BF16, p3) for k in range(2)]
                    pT = sb("pT", [128, 2, 128], BF16, p3)
                    junk3 = sb("junk3", [128, D], BF16, p3)
                    tA = sb("tA", [128, D], F32, p3)
                    x1 = sb("x1", [128, D], F32, p3)
                    x1b = sb("x1b", [128, D], BF16, p3)
                    x1T = sb("x1T", [128, 8, 128], BF16, p3)
                    gth = sb("gth", [128, D], F32, p3)
                    ge2 = sb("ge2", [128, D], F32, p3)
                    fin = [sb("fin%d" % k, [128, D], F32, p3) for k in range(2)]
                    s3 = sb("s3", [128, 8], F32, p3)
                    PQ = [ps("PQ%d" % k, [128, 1024], F32, p3) for k in range(3)]
                    PR = ps("PR", [128, 512], F32, p3)
                    PT3 = ps("PT3", [128, 1024], BF16, p3)

                    def load_cast(dst, key, lane, src2d, nkc, ncol, col0, kcstep=4):
                        scol, dcol = (0, 0) if col0 is None else col0
                        for kc0 in range(0, nkc, kcstep):
                            n = min(kcstep, nkc - kc0)
                            op("pool", lambda h, kc0=kc0, n=n: h.dma_start(
                                out=dst[:, kc0:kc0 + n, dcol:dcol + ncol],
                                in_=src2d[kc0 * 128:(kc0 + n) * 128, scol:scol + ncol].rearrange("(kc kp) n -> kp kc n", kp=128)),
                               writes=[key], lane=lane)

                    for gi in range(8):
                        k = gi % 2
                        gname = "ga" if gi < 4 else "gb"
                        fc = gi % 4
                        load_cast(wg[k], ("wg", k), "wg%d" % k, w_in_d, 8, 128, (OFF[gname] + fc * 128, 0))
                        attT = qaT if gi < 4 else qbT
                        blk = "qa_blk" if gi < 4 else "qb_blk"
                        bsuf = () if gi < 4 else (fc // 2,)
                        for tg in range(4):
                            pq = PQ[0][:, (tg % 2) * 512:(tg % 2 + 1) * 512]
                            pqk = ("PQ", 0, tg % 2)
                            for kc in range(8):
                                op("pe", lambda h, kc=kc, pq=pq, k=k, tg=tg: h.matmul(pq, lhsT=wg[k][:, kc, :], rhs=hT[:, kc, tg * 512:(tg + 1) * 512],
                                                                                     start=(kc == 0), stop=(kc == 7)),
                                   reads=[("wg", k), ("hT", tg)], writes=[pqk])
                            tk_ = tg % 2
                            op("act", lambda h, pq=pq, tk_=tk_: h.activation(out=th[tk_][:], in_=pq, func=AF.Tanh, scale=0.5), reads=[pqk], writes=[("th", tk_)])
                            op("dve", lambda h, pq=pq, tk_=tk_: h.scalar_tensor_tensor(out=pa[tk_][:], in0=th[tk_][:], scalar=1.0, in1=pq, op0=ALU.add, op1=ALU.mult),
                               reads=[pqk, ("th", tk_)], writes=[("pa", tk_)])
                            akeys = [(blk, ii) + bsuf for ii in range(tg * 4, tg * 4 + 4)]
                            op("dve", lambda h, tk_=tk_, attT=attT, fc=fc, tg=tg: h.scalar_tensor_tensor(
                                out=attT[:, fc, tg * 512:(tg + 1) * 512], in0=pa[tk_][:], scalar=0.5, in1=attT[:, fc, tg * 512:(tg + 1) * 512],
                                op0=ALU.mult, op1=ALU.mult), reads=[("pa", tk_)] + akeys, writes=akeys)
                    load_cast(wo, "wo", "wo", w_o_d, 8, D, None, kcstep=2)
                    load_cast(wpg, "wpg", "wpg", w_pg_d, 8, D, None, kcstep=2)
                    load_cast(wp, "wp", "wp", w_p_d, 2, D, None, kcstep=2)

                    for half in range(2):
                        for dc in range(8):
                            k = dc % 2
                            load_cast(wmc[k], ("wmc", k), "wmc%d" % k, w_m_d, 8, 128, (dc * 128, 0))
                            load_cast(wmc[k], ("wmc", k), "wmc%d" % k, w_m_d, 8, 128, (D + dc * 128, 128))
                            load_cast(wbr[k], ("wbr", k), "wbr%d" % k, w_ba_d, 4, 128, (dc * 128, 0))
                            load_cast(wbr[k], ("wbr", k), "wbr%d" % k, w_bb_d, 4, 128, (dc * 128, 128))
                            for tgl in range(2):
                                tg = half * 2 + tgl
                                tsl = slice(tg * 512, (tg + 1) * 512)
                                sl2 = slice(tgl * 512, (tgl + 1) * 512)
                                ma, mb_, ya = PQ[0][:, sl2], PQ[1][:, sl2], PQ[2][:, sl2]
                                yb = PR[:, :]
                                for kc in range(8):
                                    op("pe", lambda h, kc=kc, ma=ma, k=k, tsl=tsl: h.matmul(ma, lhsT=wmc[k][:, kc, 0:128], rhs=hT[:, kc, tsl], start=(kc == 0), stop=(kc == 7)),
                                       reads=[("wmc", k), ("hT", tg)], writes=[("PQ", 0, tgl)])
                                for kc in range(8):
                                    op("pe", lambda h, kc=kc, mb_=mb_, k=k, tsl=tsl: h.matmul(mb_, lhsT=wmc[k][:, kc, 128:256], rhs=hT[:, kc, tsl], start=(kc == 0), stop=(kc == 7)),
                                       reads=[("wmc", k), ("hT", tg)], writes=[("PQ", 1, tgl)])
                                ak = [("qa_blk", ii) for ii in range(tg * 4, tg * 4 + 4)]
                                bk = [("qb_blk", ii, hg_) for ii in range(tg * 4, tg * 4 + 4) for hg_ in range(2)]
                                for fc in range(4):
                                    op("pe", lambda h, fc=fc, ya=ya, k=k, tsl=tsl: h.matmul(ya, lhsT=wbr[k][:, fc, 0:128], rhs=qaT[:, fc, tsl], start=(fc == 0), stop=(fc == 3)),
                                       reads=[("wbr", k)] + ak, writes=[("PQ", 2, tgl)])
                                for fc in range(4):
                                    op("pe", lambda h, fc=fc, yb=yb, k=k, tsl=tsl: h.matmul(yb, lhsT=wbr[k][:, fc, 128:256], rhs=qbT[:, fc, tsl], start=(fc == 0), stop=(fc == 3)),
                                       reads=[("wbr", k)] + bk, writes=["PR"])
                                op("act", lambda h, ma=ma, tgl=tgl: h.activation(out=th[tgl][:], in_=ma, func=AF.Tanh, scale=0.5), reads=[("PQ", 0, tgl)], writes=[("th", tgl)])
                                op("act", lambda h, mb_=mb_, tgl=tgl: h.activation(out=th[2 + tgl][:], in_=mb_, func=AF.Tanh, scale=0.5), reads=[("PQ", 1, tgl)], writes=[("th", 2 + tgl)])
                                op("dve", lambda h, ya=ya, tgl=tgl: h.scalar_tensor_tensor(out=pa[tgl][:], in0=th[tgl][:], scalar=1.0, in1=ya, op0=ALU.add, op1=ALU.mult),
                                   reads=[("th", tgl), ("PQ", 2, tgl)], writes=[("pa", tgl)])
                                op("dve", lambda h, yb=yb, tgl=tgl: h.scalar_tensor_tensor(out=pb[tgl][:], in0=th[2 + tgl][:], scalar=1.0, in1=yb, op0=ALU.add, op1=ALU.mult),
                                   reads=[("th", 2 + tgl), "PR"], writes=[("pb", tgl)])
                                op("pool", lambda h, tgl=tgl, dc=dc, sl2=sl2: h.tensor_tensor(out=mT[:, dc, sl2], in0=pa[tgl][:], in1=pb[tgl][:], op=ALU.add),
                                   reads=[("pa", tgl), ("pb", tgl)], writes=[("mT", tgl)])
                        for tt in range(8):
                            t = half * 8 + tt
                            xk = t % 2
                            tsl = slice(tt * 128, (tt + 1) * 128)
                            dma_in("sp", x3[xk][:], x_d[b, t * 128:(t + 1) * 128, :], ("x3", xk), "x3_%d" % xk)
                            op("pool", lambda h, xk=xk, t=t: h.dma_start(out=pbf[xk][:], in_=p_d[b, t * 128:(t + 1) * 128, :]), writes=[("pbf", xk)], lane="pbf%d" % xk)
                            for hf in range(2):
                                for dc in range(8):
                                    op("pe", lambda h, hf=hf, dc=dc, tsl=tsl: h.matmul(PQ[0][:, hf * 512:(hf + 1) * 512], lhsT=mT[:, dc, tsl], rhs=wo[:, dc, hf * 512:(hf + 1) * 512],
                                                                                      start=(dc == 0), stop=(dc == 7)),
                                       reads=[("mT", tt // 4), "wo"], writes=[("PQ", 0, hf)])
                            OK_ = [("PQ", 0, 0), ("PQ", 0, 1)]
                            op("act", lambda h: h.activation(out=junk3[:], in_=PQ[0][:, :], func=AF.Square, accum_out=s3[:, 0:1]), reads=OK_, writes=["junk3", "s3a"])
                            op("dve", lambda h: h.tensor_scalar(out=s3[:, 1:2], in0=s3[:, 0:1], scalar1=1.0 / D, scalar2=4.0 * EPS, op0=ALU.mult, op1=ALU.add),
                               reads=["s3a"], writes=["s3b"])
                            op("pool", lambda h: h.tensor_tensor(out=s3[:, 2:3], in0=s3[:, 1:2], in1=negh[:], op=ALU.pow), reads=["s3b", "negh"], writes=["s3c"])
                            op("dve", lambda h: h.scalar_tensor_tensor(out=tA[:], in0=PQ[0][:, :], scalar=s3[:, 2:3], in1=gpost[:], op0=ALU.mult, op1=ALU.mult),
                               reads=OK_ + ["s3c", "gpost"], writes=["tA"])
                            op("pool", lambda h, xk=xk: h.tensor_tensor(out=x1[:], in0=tA[:], in1=x3[xk][:], op=ALU.add), reads=["tA", ("x3", xk)], writes=["x1"])
                            op("act", lambda h: h.copy(out=x1b[:], in_=x1[:]), reads=["x1"], writes=["x1b"])
                            for kc in range(8):
                                op("pe", lambda h, kc=kc: h.transpose(out=PT3[:, kc * 128:(kc + 1) * 128], in_=x1b[:, kc * 128:(kc + 1) * 128], identity=ident_bf[:]),
                                   reads=["x1b", "ident_bf"], writes=["PT3"])
                            op("act", lambda h: h.copy(out=x1T[:], in_=PT3[:, :].rearrange("p (kc n) -> p kc n", kc=8)), reads=["PT3"], writes=["x1T"])
                            for hf in range(2):
                                for dc in range(8):
                                    op("pe", lambda h, hf=hf, dc=dc: h.matmul(PQ[1][:, hf * 512:(hf + 1) * 512], lhsT=x1T[:, dc, :], rhs=wpg[:, dc, hf * 512:(hf + 1) * 512],
                                                                             start=(dc == 0), stop=(dc == 7)),
                                       reads=["x1T", "wpg"], writes=[("PQ", 1, hf)])
                            for pc in range(2):
                                op("pe", lambda h, pc=pc, xk=xk: h.transpose(out=PT3[:, pc * 128:(pc + 1) * 128], in_=pbf[xk][:, pc * 128:(pc + 1) * 128], identity=ident_bf[:]),
                                   reads=[("pbf", xk), "ident_bf"], writes=["PT3"])
                            op("dve", lambda h: h.tensor_copy(out=pT[:], in_=PT3[:, 0:256].rearrange("p (kc n) -> p kc n", kc=2)), reads=["PT3"], writes=["pT"])
                            for hf in range(2):
                                for pc in range(2):
                                    op("pe", lambda h, hf=hf, pc=pc: h.matmul(PQ[2][:, hf * 512:(hf + 1) * 512], lhsT=pT[:, pc, :], rhs=wp[:, pc, hf * 512:(hf + 1) * 512],
                                                                             start=(pc == 0), stop=(pc == 1)),
                                       reads=["pT", "wp"], writes=[("PQ", 2, hf)])
                            GK = [("PQ", 1, 0), ("PQ", 1, 1)]
                            EK = [("PQ", 2, 0), ("PQ", 2, 1)]
                            op("act", lambda h: h.activation(out=gth[:], in_=PQ[1][:, :], func=AF.Tanh, scale=0.5), reads=GK, writes=["gth"])
                            op("dve", lambda h: h.scalar_tensor_tensor(out=ge2[:], in0=gth[:], scalar=1.0, in1=PQ[2][:, :], op0=ALU.add, op1=ALU.mult),
                               reads=["gth"] + EK, writes=["ge2"])
                            op("act", lambda h: h.activation(out=junk3[:], in_=ge2[:], func=AF.Square, accum_out=s3[:, 3:4]), reads=["ge2"], writes=["junk3", "s3d"])
                            op("dve", lambda h: h.tensor_scalar(out=s3[:, 4:5], in0=s3[:, 3:4], scalar1=1.0 / D, scalar2=4.0 * EPS, op0=ALU.mult, op1=ALU.add),
                               reads=["s3d"], writes=["s3e"])
                            op("pool", lambda h: h.tensor_tensor(out=s3[:, 5:6], in0=s3[:, 4:5], in1=negh[:], op=ALU.pow), reads=["s3e", "negh"], writes=["s3f"])
                            op("dve", lambda h: h.scalar_tensor_tensor(out=tA[:], in0=ge2[:], scalar=s3[:, 5:6], in1=gple[:], op0=ALU.mult, op1=ALU.mult),
                               reads=["ge2", "s3f", "gple"], writes=["tA"])
                            op("pool", lambda h, xk=xk: h.tensor_tensor(out=fin[xk][:], in0=tA[:], in1=x1[:], op=ALU.add), reads=["tA", "x1"], writes=[("fin", xk)])
                            out_marks.append(op("sp", lambda h, xk=xk, t=t: h.dma_start(out=out_d[b, t * 128:(t + 1) * 128, :], in_=fin[xk][:]),
                                                reads=[("fin", xk)], lane="fin%d" % xk))
        last = {}
        for (sk, v) in [m for m in out_marks if m]:
            last[sk] = max(last.get(sk, 0), v)
        T.wait_marks("sp", list(last.items()))
    T.close()
    build_program.stats = (T.nops, T.nwaits)
    return nc


def _consts():
    half = 8
    freqs = 500000.0 ** (-np.arange(half, dtype=np.float64) / half)
    pos = np.arange(S, dtype=np.float64)
    cosT = np.ones((128, S), np.float64)
    sinT = np.zeros((128, S), np.float64)
    perm = np.zeros((128, 128), np.float32)
    for hh in range(2):
        for d in range(16):
            f = hh * 64 + d
            ang = pos * freqs[d % 8]
            cosT[f] = np.cos(ang)
            sinT[f] = np.sin(ang)
            if d < 8:
                perm[f + 8, f] = -1.0
            else:
                perm[f - 8, f] = 1.0
    idx = np.arange(128)
    ident = np.eye(128, dtype=np.float32)
    trineg = np.where(idx[:, None] <= idx[None, :], 0.0, MASKNEG).astype(np.float32)
    causneg = np.where(idx[None, :] <= idx[:, None], 0.0, -1.0e9).astype(np.float32)
    U = (idx[:, None] <= idx[None, :]).astype(np.float32)
    sel0 = np.zeros((128, 128), np.float32)
    sel0[0, :] = 1.0
    ones = np.ones((128, 128), np.float32)
    cst = np.stack([ident, perm, trineg, causneg, U, sel0, ones], axis=1).astype(np.float32)
    pw2 = np.tile((0.5 ** np.arange(1, NIT + 1, dtype=np.float64))[None, :], (128, 1)).astype(np.float32)
    return cosT.astype(np.float32), sinT.astype(np.float32), np.ascontiguousarray(cst), pw2


def _run(inputs, nseq, seq_ids_per_core):
    x = np.asarray(inputs["x"], np.float32)
    p = np.asarray(inputs["p"], np.float32)[0]
    cosT, sinT, cst, pw2 = _consts()
    rep = lambda v: np.ascontiguousarray(np.broadcast_to(np.asarray(v, np.float32).reshape(1, -1), (128, np.asarray(v).size)))
    common = {
        "w_in": np.ascontiguousarray(np.asarray(inputs["w_in"], np.float32)[0]),
        "w_ba": np.ascontiguousarray(np.asarray(inputs["w_branch_a"], np.float32)[0]),
        "w_bb": np.ascontiguousarray(np.asarray(inputs["w_branch_b"], np.float32)[0]),
        "w_m": np.ascontiguousarray(np.asarray(inputs["w_merge"], np.float32)[0]),
        "w_o": np.ascontiguousarray(np.asarray(inputs["w_out"], np.float32)[0]),
        "w_p": np.ascontiguousarray(np.asarray(inputs["w_ple"], np.float32)[0]),
        "w_pg": np.ascontiguousarray(np.asarray(inputs["w_ple_gate"], np.float32)[0]),
        "gpre": rep(inputs["g_pre"][0]), "gpost": rep(inputs["g_post"][0]), "gple": rep(inputs["g_ple"][0]),
        "bfb": rep(inputs["b_forget"][0]),
        "cosT": cosT, "sinT": sinT, "cst": cst, "pw2": pw2,
    }
    nc = build_program(nseq)
    in_maps = []
    for c in range(NCORES):
        ids = seq_ids_per_core[c]
        m = dict(common)
        m["x"] = np.ascontiguousarray(x[ids])
        m["p"] = np.ascontiguousarray(p[ids])
        in_maps.append(m)
    res = run_bass_kernel_spmd(nc, in_maps, core_ids=list(range(NCORES)))
    return [r["out"] for r in res.results]


def kernel(**inputs):
    B = np.asarray(inputs["x"]).shape[0]
    nseq = B // NCORES
    ids = [list(range(c * nseq, (c + 1) * nseq)) for c in range(NCORES)]
    outs = _run(inputs, nseq, ids)
    return np.concatenate(outs, axis=0).astype(np.float32)
```

```python
import contextlib
import os
import numpy as np
import concourse.bass as bass
import concourse.mybir as mybir
from concourse.bass_utils import run_bass_kernel_spmd

F32 = mybir.dt.float32
BF16 = mybir.dt.bfloat16
ALU = mybir.AluOpType
AF = mybir.ActivationFunctionType
AX = mybir.AxisListType

NCORES = 8
S = 2048
D = 1024
NT = 16
DIN = 3792
OFF = dict(qa=0, ka=512, va=576, ga=640, qi=1152, ki=1664, wi=1728, qb=1736, kb=2248, vb=2760, fb=3272, gb=3280)
EPS = 1e-6
NIT = 16
STAGE = float(os.environ.get('MK_STAGE', '9'))
ATTACH = int(os.environ.get('MK_ATTACH', '0'))
TOPK = 256
MASKNEG = -30000.0
IDX_SCALE = (8 ** -0.5) * (64 ** -0.5)

ENGS = ("pe", "act", "dve", "pool", "sp")


class Trk:
    def __init__(self, nc):
        self.nc = nc
        self.h = {"pe": nc.tensor, "act": nc.scalar, "dve": nc.vector, "pool": nc.gpsimd, "sp": nc.sync}
        self.stack = contextlib.ExitStack()
        self.sems = {e: self.stack.enter_context(nc.semaphore("s_" + e)) for e in ENGS}
        self.cnt = {e: 0 for e in ENGS}
        self.lanecnt = {}
        self.clock = {e: {} for e in ENGS}
        self.snap = {}
        self.res = {}
        self.nwaits = 0
        self.nops = 0
        self.enabled = True
        self.pending = {e: None for e in ENGS}

    def _sem(self, sk):
        s = self.sems.get(sk)
        if s is None:
            s = self.stack.enter_context(self.nc.semaphore("l_%d" % len(self.sems)))
            self.sems[sk] = s
        return s

    def op(self, eng, fn, reads=(), writes=(), lane=None):
        if not self.enabled:
            return None
        deps = []
        for r in reads:
            st = self.res.get(r)
            if st and st[0]:
                deps.append(st[0])
        for w in writes:
            st = self.res.get(w)
            if st:
                if st[0]:
                    deps.append(st[0])
                deps.extend(st[1])
        clk = self.clock[eng]
        waits = {}
        if self.pending[eng]:
            deps.extend(self.pending[eng].items())
            self.pending[eng] = None
        for (sk, v) in deps:
            if sk == eng and eng == "pe":
                continue
            if clk.get(sk, 0) >= v:
                continue
            if waits.get(sk, 0) < v:
                waits[sk] = v
        if waits:
            clk = dict(clk)
            for sk, v in waits.items():
                sn = self.snap.get((sk, v))
                if sn:
                    for k2, v2 in sn.items():
                        if clk.get(k2, 0) < v2:
                            clk[k2] = v2
                if clk.get(sk, 0) < v:
                    clk[sk] = v
            self.clock[eng] = clk
        if lane is None:
            self.cnt[eng] += 1
            me = (eng, self.cnt[eng])
        else:
            sk = ("lane", lane)
            self.lanecnt[lane] = self.lanecnt.get(lane, 0) + 16
            me = (sk, self.lanecnt[lane])
        sn = dict(clk)
        sn[me[0]] = me[1]
        self.snap[me] = sn
        for r in reads:
            st = self.res.setdefault(r, [None, []])
            st[1].append(me)
        for w in writes:
            self.res[w] = [me, []]
        h = self.h[eng]
        wl = list(waits.items())
        self.nwaits += len(wl)
        self.nops += 1
        if wl and ATTACH:
            for sk, v in wl[:-1]:
                h.wait_ge(self._sem(sk), v)
            ins = fn(h)
            sk, v = wl[-1]
            ins = ins._wait_ge(self._sem(sk), v)
        else:
            for sk, v in wl:
                h.wait_ge(self._sem(sk), v)
            ins = fn(h)
        ins.then_inc(self._sem(me[0]), 16 if lane is not None else 1)
        return me

    def fence(self):
        marks = {e: c for e, c in self.cnt.items() if c}
        for ln, c in self.lanecnt.items():
            marks[("lane", ln)] = c
        for e in ENGS:
            self.pending[e] = dict(marks)

    def wait_marks(self, eng, marks):
        h = self.h[eng]
        for sk, v in marks:
            h.wait_ge(self._sem(sk), v)

    def close(self):
        self.stack.close()


def build_program(nseq, dbg=False):
    nc = bass.Bass("TRN2", target_bir_lowering=False)

    def din(name, shape):
        return nc.dram_tensor(name, shape, F32, kind="ExternalInput").ap()

    x_d = din("x", [nseq, S, D])
    p_d = din("p", [nseq, S, 256])
    w_in_d = din("w_in", [D, DIN])
    w_ba_d = din("w_ba", [512, D])
    w_bb_d = din("w_bb", [512, D])
    w_m_d = din("w_m", [D, 2 * D])
    w_o_d = din("w_o", [D, D])
    w_p_d = din("w_p", [256, D])
    w_pg_d = din("w_pg", [D, D])
    gpre_d = din("gpre", [128, D])
    gpost_d = din("gpost", [128, D])
    gple_d = din("gple", [128, D])
    bfb_d = din("bfb", [128, 8])
    cos_d = din("cosT", [128, S])
    sin_d = din("sinT", [128, S])
    cst_d = din("cst", [128, 7, 128])
    pw2_d = din("pw2", [128, NIT + 1])
    out_d = nc.dram_tensor("out", [nseq, S, D], F32, kind="ExternalOutput").ap()
    dbg_d = {}

    T = Trk(nc)
    op = T.op
    uid = [0]

    def dma_in(eng, out_ap, in_ap, key, lane):
        return op(eng, lambda h: h.dma_start(out=out_ap, in_=in_ap), writes=[key], lane=lane)

    with contextlib.ExitStack() as g:
        def sb(name, shape, dt, es=g):
            uid[0] += 1
            return es.enter_context(nc.sbuf_tensor("%s_u%d" % (name, uid[0]), shape, dt))

        def ps(name, shape, dt, es=g):
            uid[0] += 1
            return es.enter_context(nc.psum_tensor("%s_u%d" % (name, uid[0]), shape, dt))

        cst = sb("cst", [128, 7, 128], F32)
        ident_bf = sb("ident_bf", [128, 128], BF16)
        trineg_bf = sb("trineg_bf", [128, 128], BF16)
        irep_bf = sb("irep_bf", [128, 512], BF16)
        ones_bf = sb("ones_bf", [128, 64], BF16)
        gpre = sb("gpre_s", [128, D], F32)
        gpost = sb("gpost_s", [128, D], F32)
        gple = sb("gple_s", [128, D], F32)
        bfb = sb("bfb_s", [128, 8], F32)
        pw2 = sb("pw2_s", [128, NIT + 1], F32)
        negh = sb("negh", [128, 1], F32)
        taum = sb("taum", [128, 1], F32)
        ident_f = cst[:, 0, :]
        perm_f = cst[:, 1, :]
        causneg_f = cst[:, 3, :]
        U_f = cst[:, 4, :]
        sel0_f = cst[:, 5, :]
        ones_f = cst[:, 6, :]

        dma_in("sp", cst[:], cst_d[:, :, :], "cst", "c0")
        dma_in("sp", gpre[:], gpre_d[:, :], "gpre", "c1")
        dma_in("sp", gpost[:], gpost_d[:, :], "gpost", "c2")
        dma_in("sp", gple[:], gple_d[:, :], "gple", "c3")
        dma_in("sp", bfb[:], bfb_d[:, :], "bfb", "c4")
        dma_in("sp", pw2[:], pw2_d[:, :], "pw2", "c5")
        dma_in("pool", ident_bf[:], cst_d[:, 0, :], "ident_bf", "c6")
        dma_in("pool", trineg_bf[:], cst_d[:, 2, :], "trineg_bf", "c7")
        for k in range(4):
            op("pool", lambda h, k=k: h.dma_start(out=irep_bf[:, k * 128:(k + 1) * 128], in_=cst_d[:, 0, :]),
               writes=[("irep", k)], lane="c8")
        IREP = [("irep", k) for k in range(4)]
        op("pool", lambda h: h.memset(ones_bf[:], 1.0), writes=["ones_bf"])
        op("pool", lambda h: h.memset(negh[:], -0.5), writes=["negh"])
        op("pool", lambda h: h.memset(taum[:], -1.0e8), writes=["taum"])

        out_marks = []

        for b in range(nseq):
            T.fence()
            with contextlib.ExitStack() as sq:
                hT = sb("hT", [128, 8, S], BF16, sq)
                qaT = sb("qaT", [128, 4, S], BF16, sq)
                qbT = sb("qbT", [128, 4, S], BF16, sq)
                with contextlib.ExitStack() as p12:
                    qiT = sb("qiT", [128, 4, S], BF16, p12)
                    kaT = sb("kaT", [128, S], BF16, p12)
                    kiT = sb("kiT", [128, S], BF16, p12)
                    kbT = sb("kbT", [128, 4, S], BF16, p12)
                    va = sb("va", [128, NT, 128], BF16, p12)
                    vb = sb("vb", [128, NT, 512], BF16, p12)
                    wi = sb("wi", [128, NT, 8], F32, p12)
                    fbs = sb("fbs", [128, NT, 8], F32, p12)
                    csb = sb("csb", [128, NT, 8], F32, p12)
                    carry = sb("carry", [128, NT, 8], F32, p12)
                    with contextlib.ExitStack() as p1:
                        T.enabled = STAGE >= 1
                        cosT = sb("cosT_s", [128, S], F32, p1)
                        sinT = sb("sinT_s", [128, S], F32, p1)
                        xt = [sb("xt%d" % k, [128, D], F32, p1) for k in range(2)]
                        junk = sb("junk1", [128, D], BF16, p1)
                        hb = [sb("hb%d" % k, [128, D], BF16, p1) for k in range(2)]
                        wbuf = [sb("wbuf%d" % k, [128, 8, 512], BF16, p1) for k in range(2)]
                        xs = [sb("xs%d" % k, [128, 512], F32, p1) for k in range(2)]
                        t1 = [sb("t1_0", [128, 512], F32, p1)] * 2
                        t2 = [sb("t2_0", [128, 512], F32, p1)] * 2
                        st = sb("st1", [128, 4], F32, p1)
                        wsm = sb("wsm", [128, 8, 80], BF16, p1)
                        sm16 = sb("sm16", [128, 16], F32, p1)
                        PA = [ps("PA%d" % k, [128, 512], F32, p1) for k in range(4)]
                        PB = [ps("PB%d" % k, [128, 512], F32, p1) for k in range(2)]
                        PTB = ps("PTB1", [128, 1024], BF16, p1)
                        PC = ps("PC1", [128, 512], F32, p1)

                        dma_in("sp", cosT[:], cos_d[:, :], "cosT", "cos")
                        dma_in("sp", sinT[:], sin_d[:, :], "sinT", "sin")
                        op("pool", lambda h: h.memset(va[:, :, 64:128], 1.0), writes=["va_ones"])

                        wl_state = {"n": 0}

                        def load_w(segments):
                            k = wl_state["n"] % 2
                            wl_state["n"] += 1
                            c = 0
                            for (c0, ncol) in segments:
                                for kc0 in range(0, 8, 4):
                                    op("pool", lambda h, k=k, c=c, c0=c0, ncol=ncol, kc0=kc0: h.dma_start(
                                        out=wbuf[k][:, kc0:kc0 + 4, c:c + ncol],
                                        in_=w_in_d[kc0 * 128:(kc0 + 4) * 128, c0:c0 + ncol].rearrange("(kc kp) n -> kp kc n", kp=128)),
                                       writes=[("wbuf", k)], lane="wbuf%d" % k)
                                c += ncol
                            return k

                        for t in range(NT):
                            k = t % 2
                            dma_in("sp", xt[k][:], x_d[b, t * 128:(t + 1) * 128, :], ("xt", k), "xt%d" % k)
                            op("act", lambda h, k=k: h.activation(out=junk[:], in_=xt[k][:], func=AF.Square, accum_out=st[:, 0:1]),
                               reads=[("xt", k)], writes=["junk1", "st0"])
                            op("dve", lambda h: h.tensor_scalar(out=st[:, 1:2], in0=st[:, 0:1], scalar1=1.0 / D, scalar2=EPS,
                                                                op0=ALU.mult, op1=ALU.add), reads=["st0"], writes=["st1"])
                            op("pool", lambda h: h.tensor_tensor(out=st[:, 2:3], in0=st[:, 1:2], in1=negh[:], op=ALU.pow),
                               reads=["st1", "negh"], writes=["st2"])
                            op("dve", lambda h, k=k: h.scalar_tensor_tensor(out=hb[k][:], in0=xt[k][:], scalar=st[:, 2:3], in1=gpre[:],
                                                                            op0=ALU.mult, op1=ALU.mult),
                               reads=[("xt", k), "st2", "gpre"], writes=[("hb", k)])
                            for kc in range(8):
                                op("pe", lambda h, k=k, kc=kc: h.transpose(out=PTB[:, kc * 128:(kc + 1) * 128], in_=hb[k][:, kc * 128:(kc + 1) * 128],
                                                                           identity=ident_bf[:]),
                                   reads=[("hb", k), "ident_bf"], writes=["PTB1"])
                            op("act", lambda h, t=t: h.copy(out=hT[:, :, t * 128:(t + 1) * 128], in_=PTB[:, :].rearrange("p (kc n) -> p kc n", kc=8)),
                               reads=["PTB1"], writes=[("hT", t // 4)])

                        T.enabled = STAGE >= 1.2
                        pa_i = [0]

                        def proj_fm(k, cofs, tg):
                            slot = pa_i[0] % 4
                            pa_i[0] += 1
                            for kc in range(8):
                                op("pe", lambda h, kc=kc, slot=slot: h.matmul(PA[slot][:, :], lhsT=wbuf[k][:, kc, cofs:cofs + 128],
                                                                             rhs=hT[:, kc, tg * 512:(tg + 1) * 512], start=(kc == 0), stop=(kc == 7)),
                                   reads=[("wbuf", k), ("hT", tg)], writes=[("PA", slot)])
                            return slot

                        rp_i = [0]

                        def rope_evac(slot, dst_ap, dst_key, tg):
                            r = rp_i[0] % 2
                            rp_i[0] += 1
                            tsl = slice(tg * 512, (tg + 1) * 512)
                            op("act", lambda h: h.copy(out=xs[r][:], in_=PA[slot][:, :]), reads=[("PA", slot)], writes=[("xs", r)])
                            op("pe", lambda h: h.matmul(PB[r][:, :], lhsT=perm_f, rhs=xs[r][:], start=True, stop=True),
                               reads=[("xs", r), "cst"], writes=[("PB", r)])
                            op("dve", lambda h: h.tensor_tensor(out=t1[r][:], in0=PB[r][:, :], in1=sinT[:, tsl], op=ALU.mult),
                               reads=[("PB", r), "sinT"], writes=["t1"])
                            op("pool", lambda h: h.tensor_tensor(out=t2[r][:], in0=xs[r][:], in1=cosT[:, tsl], op=ALU.mult),
                               reads=[("xs", r), "cosT"], writes=["t2"])
                            op("dve", lambda h: h.tensor_tensor(out=dst_ap, in0=t1[r][:], in1=t2[r][:], op=ALU.add),
                               reads=["t1", "t2"], writes=[dst_key])

                        for (nm, dst) in (("qa", qaT), ("qi", qiT)):
                            k = load_w([(OFF[nm], 512)])
                            for c in range(4):
                                for tg in range(4):
                                    slot = proj_fm(k, c * 128, tg)
                                    rope_evac(slot, dst[:, c, tg * 512:(tg + 1) * 512], (nm + "T", c, tg), tg)
                        T.enabled = STAGE >= 1.3
                        k = load_w([(OFF["ka"], 64), (OFF["ka"], 64), (OFF["ki"], 64), (OFF["ki"], 64)])
                        for c, (nm, dst) in enumerate((("ka", kaT), ("ki", kiT))):
                            for tg in range(4):
                                slot = proj_fm(k, c * 128, tg)
                                rope_evac(slot, dst[:, tg * 512:(tg + 1) * 512], (nm + "T", tg), tg)
                        T.enabled = STAGE >= 1.4
                        for (nm, dst) in (("qb", qbT), ("kb", kbT)):
                            k = load_w([(OFF[nm], 512)])
                            for c in range(4):
                                for tg in range(4):
                                    slot = proj_fm(k, c * 128, tg)
                                    op("act", lambda h, slot=slot, dst=dst, c=c, tg=tg: h.copy(out=dst[:, c, tg * 512:(tg + 1) * 512], in_=PA[slot][:, :]),
                                       reads=[("PA", slot)], writes=[(nm + "T", c, tg)])
                        T.enabled = STAGE >= 1.5
                        k = load_w([(OFF["vb"], 512)])
                        k2 = load_w([(OFF["va"], 64), (OFF["wi"] - 64, 72), (OFF["fb"] - 64, 72)])
                        op("dve", lambda h: h.tensor_copy(out=wsm[:, :, 0:64], in_=wbuf[k2][:, :, 0:64]), reads=[("wbuf", k2)], writes=["wsm"])
                        op("dve", lambda h: h.tensor_copy(out=wsm[:, :, 64:72], in_=wbuf[k2][:, :, 128:136]), reads=[("wbuf", k2)], writes=["wsm"])
                        op("dve", lambda h: h.tensor_copy(out=wsm[:, :, 72:80], in_=wbuf[k2][:, :, 200:208]), reads=[("wbuf", k2)], writes=["wsm"])
                        for t in range(NT):
                            slot = pa_i[0] % 4
                            pa_i[0] += 1
                            for kc in range(8):
                                op("pe", lambda h, kc=kc, slot=slot, t=t: h.matmul(PA[slot][:, :], lhsT=hT[:, kc, t * 128:(t + 1) * 128],
                                                                                  rhs=wbuf[k][:, kc, 0:512], start=(kc == 0), stop=(kc == 7)),
                                   reads=[("wbuf", k), ("hT", t // 4)], writes=[("PA", slot)])
                            op("act", lambda h, slot=slot, t=t: h.copy(out=vb[:, t, :], in_=PA[slot][:, :]), reads=[("PA", slot)], writes=[("vb", t)])
                            if STAGE < 1.6:
                                continue
                            for kc in range(8):
                                op("pe", lambda h, kc=kc, t=t: h.matmul(PC[:, 0:80], lhsT=hT[:, kc, t * 128:(t + 1) * 128],
                                                                        rhs=wsm[:, kc, 0:80], start=(kc == 0), stop=(kc == 7)),
                                   reads=["wsm", ("hT", t // 4)], writes=["PC1"])
                            op("act", lambda h, t=t: h.copy(out=va[:, t, 0:64], in_=PC[:, 0:64]), reads=["PC1"], writes=[("va", t)])
                            op("act", lambda h: h.copy(out=sm16[:], in_=PC[:, 64:80]), reads=["PC1"], writes=["sm16"])
                            op("act", lambda h, t=t: h.mul(out=wi[:, t, :], in_=sm16[:, 0:8], mul=IDX_SCALE),
                               reads=["sm16"], writes=[("wi", t)])
                            op("pool", lambda h, t=t: h.tensor_tensor(out=fbs[:, t, :], in0=sm16[:, 8:16], in1=bfb[:], op=ALU.add),
                               reads=["sm16", "bfb"], writes=["fbs"])
                    T.fence()
                    with contextlib.ExitStack() as p2:
                        T.enabled = STAGE >= 2
                        score = [sb("score%d" % k, [128, S], F32, p2) for k in range(2)]
                        tmp = [sb("tmp%d" % k, [128, 512], F32, p2) for k in range(2)]
                        junk2 = sb("junk2", [128, S], BF16, p2)
                        mneg = [sb("mneg%d" % k, [128, S], BF16, p2) for k in range(3)]
                        ptd = [sb("ptd%d" % k, [128, 512], BF16, p2) for k in range(3)]
                        ptf = [sb("ptf%d" % k, [128, 128], BF16, p2) for k in range(3)]
                        dend = sb("dend", [128, 512], F32, p2)
                        denf = sb("denf", [128, 512], F32, p2)
                        sst = [sb("sst%d" % k, [128, 8 + NIT + 1], F32, p2) for k in range(2)]
                        crefb = sb("crefb", [128, NT, 8], F32, p2)
                        biasT = sb("biasT", [128, NT, NT, 8], F32, p2)
                        IDX = ps("IDX", [128, 512], F32, p2)
                        DS = [ps("DS%d" % k, [128, 512], F32, p2) for k in range(2)]
                        DACC = ps("DACC", [128, 512], F32, p2)
                        FS = [ps("FS%d" % k, [128, 512], F32, p2) for k in range(3)]
                        FACC = ps("FACC", [128, 512], F32, p2)
                        cnt = {"idx": 0, "ptd": 0, "fs": 0}
                        fl = fbs[:].rearrange("p t h -> p (t h)")
                        cl = csb[:].rearrange("p t h -> p (t h)")
                        op("act", lambda h: h.activation(out=fl, in_=fl, func=AF.Exp, scale=-1.0), reads=["fbs"], writes=["fbs"])
                        op("act", lambda h: h.activation(out=fl, in_=fl, func=AF.Ln, bias=1.0, scale=1.0), reads=["fbs"], writes=["fbs"])
                        op("dve", lambda h: h.tensor_scalar(out=fl, in0=fl, scalar1=-1.0, scalar2=None, op0=ALU.mult), reads=["fbs"], writes=["fbs"])
                        op("pe", lambda h: h.matmul(IDX[:, 0:128], lhsT=U_f, rhs=fl, start=True, stop=True), reads=["fbs", "cst"], writes=[("IDX", 0)])
                        op("pe", lambda h: h.matmul(FS[0][:, 0:128], lhsT=ones_f, rhs=fl, start=True, stop=True), reads=["fbs", "cst"], writes=[("FS", 0)])
                        op("dve", lambda h: h.memset(carry[:, 0, :], 0.0), writes=["carry"])
                        for t in range(1, NT):
                            op("dve", lambda h, t=t: h.tensor_tensor(out=carry[:, t, :], in0=carry[:, t - 1, :], in1=FS[0][:, (t - 1) * 8:t * 8], op=ALU.add),
                               reads=[("FS", 0), "carry"], writes=["carry"])
                        op("dve", lambda h: h.tensor_tensor(out=cl, in0=IDX[:, 0:128], in1=carry[:].rearrange("p t h -> p (t h)"), op=ALU.add),
                           reads=[("IDX", 0), "carry"], writes=["csb"])
                        op("pe", lambda h: h.matmul(IDX[:, 0:128], lhsT=sel0_f, rhs=cl, start=True, stop=True), reads=["csb", "cst"], writes=[("IDX", 0)])
                        op("act", lambda h: h.copy(out=crefb[:].rearrange("p t h -> p (t h)"), in_=IDX[:, 0:128]), reads=[("IDX", 0)], writes=["crefb"])
                        for i in range(NT):
                            op("dve", lambda h, i=i: h.tensor_tensor(out=biasT[:, i, 0:i + 1, :],
                                                                     in0=crefb[:, i, :].unsqueeze(1).broadcast_to([128, i + 1, 8]),
                                                                     in1=csb[:, 0:i + 1, :], op=ALU.subtract),
                               reads=["crefb", "csb"], writes=["biasT"])

                        def dsa_index(i):
                            items = []
                            sp_ = i % 2
                            L = 128 * (i + 1)
                            sc = score[sp_]
                            nch = (L + 511) // 512
                            s_ = sst[sp_]
                            SK = ("sst", sp_)
                            for hd in range(8):
                                c, r = hd // 2, hd % 2
                                for kk in range(nch):
                                    def chunk(hd=hd, c=c, r=r, kk=kk):
                                        w = min(512, L - 512 * kk)
                                        sl = cnt["idx"] % 2
                                        cnt["idx"] += 1
                                        op("pe", lambda h: h.matmul(
                                            IDX[:, 0:w], lhsT=qiT[64 * r:64 * r + 64, c, i * 128:(i + 1) * 128],
                                            rhs=kiT[64 * r:64 * r + 64, kk * 512:kk * 512 + w], start=True, stop=True),
                                           reads=[("qiT", c, i // 4), ("kiT", kk)], writes=[("IDX", 0)])
                                        op("act", lambda h: h.activation(out=tmp[sl][:, 0:w], in_=IDX[:, 0:w], func=AF.Relu),
                                           reads=[("IDX", 0)], writes=[("tmp", sl)])
                                        if hd == 0:
                                            op("dve", lambda h: h.tensor_scalar(
                                                out=sc[:, kk * 512:kk * 512 + w], in0=tmp[sl][:, 0:w], scalar1=wi[:, i, 0:1], scalar2=None, op0=ALU.mult),
                                               reads=[("tmp", sl), ("wi", i)], writes=[("score", sp_)])
                                        else:
                                            op("dve", lambda h: h.scalar_tensor_tensor(
                                                out=sc[:, kk * 512:kk * 512 + w], in0=tmp[sl][:, 0:w], scalar=wi[:, i, hd:hd + 1],
                                                in1=sc[:, kk * 512:kk * 512 + w], op0=ALU.mult, op1=ALU.add),
                                               reads=[("tmp", sl), ("wi", i), ("score", sp_)], writes=[("score", sp_)])
                                    items.append(chunk)

                            def prep():
                                op("dve", lambda h: h.tensor_tensor(out=sc[:, i * 128:L], in0=sc[:, i * 128:L], in1=causneg_f, op=ALU.add),
                                   reads=[("score", sp_), "cst"], writes=[("score", sp_)])
                                if i >= 2:
                                    op("dve", lambda h: h.tensor_reduce(out=s_[:, 0:1], in_=sc[:, 0:128 * i], axis=AX.X, op=ALU.min),
                                       reads=[("score", sp_)], writes=[SK])
                                    op("dve", lambda h: h.tensor_reduce(out=s_[:, 1:2], in_=sc[:, 0:L], axis=AX.X, op=ALU.max),
                                       reads=[("score", sp_)], writes=[SK])
                                    op("dve", lambda h: h.tensor_tensor(out=s_[:, 2:3], in0=s_[:, 1:2], in1=s_[:, 0:1], op=ALU.subtract),
                                       reads=[SK], writes=[SK])
                                    op("dve", lambda h: h.tensor_scalar(out=s_[:, 8:9 + NIT], in0=pw2[:], scalar1=s_[:, 2:3], scalar2=None, op0=ALU.mult),
                                       reads=[SK, "pw2"], writes=[SK])
                                    op("dve", lambda h: h.tensor_tensor(out=s_[:, 3:4], in0=s_[:, 0:1], in1=s_[:, 8:9], op=ALU.add),
                                       reads=[SK], writes=[SK])
                            items.append(prep)
                            if i >= 2:
                                for it in range(NIT):
                                    def iteration(it=it):
                                        op("dve", lambda h: h.tensor_scalar(out=junk2[:, 0:L], in0=sc[:, 0:L], scalar1=s_[:, 3:4], scalar2=None,
                                                                            op0=ALU.is_ge, op1=ALU.add, accum_out=s_[:, 4:5]),
                                           reads=[SK, ("score", sp_)], writes=[SK, "junk2"])
                                        op("dve", lambda h: h.scalar_tensor_tensor(out=s_[:, 5:6], in0=s_[:, 4:5], scalar=TOPK - 0.5, in1=s_[:, 8 + it:9 + it],
                                                                                   op0=ALU.is_ge, op1=ALU.mult),
                                           reads=[SK], writes=[SK])
                                        op("dve", lambda h: h.scalar_tensor_tensor(out=s_[:, 3:4], in0=s_[:, 5:6], scalar=s_[:, 9 + it:10 + it], in1=s_[:, 3:4],
                                                                                   op0=ALU.subtract, op1=ALU.add),
                                           reads=[SK], writes=[SK])
                                    items.append(iteration)

                            def fin():
                                if i >= 2:
                                    tau, tk = s_[:, 3:4], [SK]
                                else:
                                    tau, tk = taum[:], ["taum"]
                                op("dve", lambda h: h.tensor_scalar(out=mneg[i % 3][:, 0:L], in0=sc[:, 0:L], scalar1=tau, scalar2=MASKNEG,
                                                                    op0=ALU.is_lt, op1=ALU.mult),
                                   reads=tk + [("score", sp_)], writes=[("mneg", i % 3)])
                            items.append(fin)
                            return items

                        class Pipe:
                            def __init__(self, skew, batch):
                                self.q, self.skew, self.batch, self.pf = [], skew, batch, []

                            def push(self, front, back):
                                self.pf.append((front, back))
                                if len(self.pf) >= self.batch:
                                    self._go()

                            def _go(self):
                                for f, _ in self.pf:
                                    f()
                                self.q.extend(bk for _, bk in self.pf)
                                self.pf = []
                                while len(self.q) > self.skew:
                                    self.q.pop(0)()

                            def flush(self):
                                if self.pf:
                                    self._go()
                                while self.q:
                                    self.q.pop(0)()

                        def dsa_units(i):
                            units = []
                            for half in range(2):
                                for j in range(i + 1):
                                    pk = cnt["ptd"] % 3
                                    ds = cnt["ptd"] % 2
                                    cnt["ptd"] += 1

                                    def front(j=j, half=half, ds=ds):
                                        D_ = DS[ds]
                                        op("pe", lambda h: h.matmul(D_[:, :], lhsT=mneg[i % 3][:, j * 128:(j + 1) * 128], rhs=irep_bf[:],
                                                                    start=True, stop=False),
                                           reads=[("mneg", i % 3)] + IREP, writes=[("DS", ds)])
                                        for hh in range(4):
                                            c, r = hh, half
                                            op("pe", lambda h, hh=hh, c=c, r=r: h.matmul(
                                                D_[:, hh * 128:(hh + 1) * 128], lhsT=kaT[64 * r:64 * r + 64, j * 128:(j + 1) * 128],
                                                rhs=qaT[64 * r:64 * r + 64, c, i * 128:(i + 1) * 128], start=False, stop=(hh == 3)),
                                               reads=[("kaT", j // 4), ("qaT", c, i // 4), ("qa_blk", i, r)], writes=[("DS", ds)])

                                    def back(j=j, half=half, pk=pk, ds=ds):
                                        D_ = DS[ds]
                                        op("act", lambda h: h.activation(out=ptd[pk][:], in_=D_[:, :], func=AF.Exp, scale=0.125),
                                           reads=[("DS", ds)], writes=[("ptd", pk)])
                                        op("pe", lambda h: h.matmul(DACC[:, :], lhsT=va[:, j, :], rhs=ptd[pk][:],
                                                                    start=(j == 0), stop=(j == i)),
                                           reads=[("ptd", pk), ("va", j), "va_ones"], writes=["DACC"])
                                    units.append((front, back))

                                def epi(half=half):
                                    r = half
                                    op("act", lambda h: h.copy(out=dend[64:128, :], in_=DACC[64:128, :]), reads=["DACC"], writes=["dend"])
                                    op("dve", lambda h: h.reciprocal(out=dend[64:128, :], in_=dend[64:128, :]), reads=["dend"], writes=["dend"])
                                    op("dve", lambda h: h.tensor_tensor(
                                        out=qaT[64 * r:64 * r + 64, :, i * 128:(i + 1) * 128],
                                        in0=DACC[0:64, :].rearrange("p (c q) -> p c q", c=4),
                                        in1=dend[64:128, :].rearrange("p (c q) -> p c q", c=4), op=ALU.mult),
                                       reads=["DACC", "dend"], writes=[("qa_blk", i, r)])
                                units.append((lambda: None, epi))
                            return units

                        def fox_units(i):
                            units = []
                            for hg in range(2):
                                for hh in range(4):
                                    hd = hg * 4 + hh
                                    c, r = hd // 2, hd % 2
                                    for j in range(i + 1):
                                        sl = cnt["fs"] % 3
                                        cnt["fs"] += 1

                                        def front(j=j, sl=sl, c=c, r=r):
                                            F_ = FS[sl][:, 0:128]
                                            op("pe", lambda h: h.matmul(
                                                F_, lhsT=kbT[64 * r:64 * r + 64, c, j * 128:(j + 1) * 128],
                                                rhs=qbT[64 * r:64 * r + 64, c, i * 128:(i + 1) * 128], start=True, stop=(j != i)),
                                               reads=[("kbT", c, j // 4), ("qbT", c, i // 4), ("qb_blk", i, c // 2)], writes=[("FS", sl)])
                                            if j == i:
                                                op("pe", lambda h: h.matmul(F_, lhsT=ident_bf[:], rhs=trineg_bf[:], start=False, stop=True),
                                                   reads=["ident_bf", "trineg_bf"], writes=[("FS", sl)])

                                        def back(j=j, sl=sl, hd=hd, hh=hh):
                                            F_ = FS[sl][:, 0:128]
                                            op("act", lambda h: h.activation(out=ptf[sl][:], in_=F_, func=AF.Exp, scale=0.125,
                                                                             bias=biasT[:, i, j, hd:hd + 1]),
                                               reads=[("FS", sl), "biasT"], writes=[("ptf", sl)])
                                            op("pe", lambda h: h.matmul(FACC[0:64, hh * 128:(hh + 1) * 128], lhsT=vb[:, j, hd * 64:(hd + 1) * 64],
                                                                        rhs=ptf[sl][:], start=(j == 0), stop=(j == i)),
                                               reads=[("ptf", sl), ("vb", j)], writes=["FACCn"])
                                            op("pe", lambda h: h.matmul(FACC[64:128, hh * 128:(hh + 1) * 128], lhsT=ones_bf[:, 0:64],
                                                                        rhs=ptf[sl][:], start=(j == 0), stop=(j == i)),
                                               reads=[("ptf", sl), "ones_bf"], writes=["FACCd"])
                                        units.append((front, back))

                                def epi(hg=hg):
                                    FK = ["FACCn", "FACCd"]
                                    op("act", lambda h: h.copy(out=denf[64:128, :], in_=FACC[64:128, :]), reads=FK, writes=["denf"])
                                    op("dve", lambda h: h.reciprocal(out=denf[64:128, :], in_=denf[64:128, :]), reads=["denf"], writes=["denf"])
                                    for r in range(2):
                                        op("dve", lambda h, r=r: h.tensor_tensor(
                                            out=qbT[64 * r:64 * r + 64, 2 * hg:2 * hg + 2, i * 128:(i + 1) * 128],
                                            in0=FACC[0:64, :].rearrange("p (c r q) -> p c r q", c=2, r=2)[:, :, r, :],
                                            in1=denf[64:128, :].rearrange("p (c r q) -> p c r q", c=2, r=2)[:, :, r, :], op=ALU.mult),
                                           reads=FK + ["denf"], writes=[("qb_blk", i, hg)])
                                units.append((None, epi))
                            return units

                        dpipe = Pipe(1, 1)
                        fpipe = Pipe(2, 1)
                        for it_ in dsa_index(0) + dsa_index(1):
                            it_()
                        for i in range(NT):
                            items = dsa_index(i + 2) if i + 2 < NT else []
                            du = dsa_units(i)
                            fu = fox_units(i)
                            nF = len(fu)
                            rate = -(-len(items) // max(1, int(0.7 * nF)))
                            di = 0
                            ii = 0
                            for n_, (f, bk) in enumerate(fu):
                                if f is None:
                                    fpipe.flush()
                                    bk()
                                else:
                                    fpipe.push(f, bk)
                                for _ in range(rate):
                                    if ii < len(items):
                                        items[ii]()
                                        ii += 1
                                if n_ % 4 == 3 and di < len(du):
                                    dpipe.push(*du[di])
                                    di += 1
                            while ii < len(items):
                                items[ii]()
                                ii += 1
                            while di < len(du):
                                dpipe.push(*du[di])
                                di += 1
                        dpipe.flush()
                        fpipe.flush()
                T.fence()
                with contextlib.ExitStack() as p3:
                    T.enabled = STAGE >= 3
                    wo = sb("wo", [128, 8, D], BF16, p3)
                    wpg = sb("wpg", [128, 8, D], BF16, p3)
                    wp = sb("wp", [128, 2, D], BF16, p3)
                    mT = sb("mT", [128, 8, 1024], BF16, p3)
                    wg = [sb("wg%d" % k, [128, 8, 128], BF16, p3) for k in range(2)]
                    wmc = [sb("wmc%d" % k, [128, 8, 256], BF16, p3) for k in range(2)]
                    wbr = [sb("wbr%d" % k, [128, 4, 256], BF16, p3) for k in range(2)]
                    th = [sb("th%d" % k, [128, 512], BF16, p3) for k in range(4)]
                    pa = [sb("pa%d" % k, [128, 512], F32, p3) for k in range(2)]
                    pb = [sb("pb%d" % k, [128, 512], F32, p3) for k in range(2)]
                    x3 = [sb("x3_%d" % k, [128, D], F32, p3) for k in range(2)]
                    pbf = [sb("pbf%d" % k, [128, 256], BF16, p3) for k in range(2)]
                    pT = sb("pT", [128, 2, 128], BF16, p3)
                    junk3 = sb("junk3", [128, D], BF16, p3)
                    tA = sb("tA", [128, D], F32, p3)
                    x1 = sb("x1", [128, D], F32, p3)
                    x1b = sb("x1b", [128, D], BF16, p3)
                    x1T = sb("x1T", [128, 8, 128], BF16, p3)
                    gth = sb("gth", [128, D], F32, p3)
                    ge2 = sb("ge2", [128, D], F32, p3)
                    fin = [sb("fin%d" % k, [128, D], F32, p3) for k in range(2)]
                    s3 = sb("s3", [128, 8], F32, p3)
                    PQ = [ps("PQ%d" % k, [128, 1024], F32, p3) for k in range(3)]
                    PR = ps("PR", [128, 512], F32, p3)
                    PT3 = ps("PT3", [128, 1024], BF16, p3)

                    def load_cast(dst, key, lane, src2d, nkc, ncol, col0, kcstep=4):
                        scol, dcol = (0, 0) if col0 is None else col0
                        for kc0 in range(0, nkc, kcstep):
                            n = min(kcstep, nkc - kc0)
                            op("pool", lambda h, kc0=kc0, n=n: h.dma_start(
                                out=dst[:, kc0:kc0 + n, dcol:dcol + ncol],
                                in_=src2d[kc0 * 128:(kc0 + n) * 128, scol:scol + ncol].rearrange("(kc kp) n -> kp kc n", kp=128)),
                               writes=[key], lane=lane)

                    for gi in range(8):
                        k = gi % 2
                        gname = "ga" if gi < 4 else "gb"
                        fc = gi % 4
                        load_cast(wg[k], ("wg", k), "wg%d" % k, w_in_d, 8, 128, (OFF[gname] + fc * 128, 0))
                        attT = qaT if gi < 4 else qbT
                        blk = "qa_blk" if gi < 4 else "qb_blk"
                        bsuf = None
                        for tg in range(4):
                            pq = PQ[0][:, (tg % 2) * 512:(tg % 2 + 1) * 512]
                            pqk = ("PQ", 0, tg % 2)
                            for kc in range(8):
                                op("pe", lambda h, kc=kc, pq=pq, k=k, tg=tg: h.matmul(pq, lhsT=wg[k][:, kc, :], rhs=hT[:, kc, tg * 512:(tg + 1) * 512],
                                                                                     start=(kc == 0), stop=(kc == 7)),
                                   reads=[("wg", k), ("hT", tg)], writes=[pqk])
                            tk_ = tg % 2
                            op("act", lambda h, pq=pq, tk_=tk_: h.activation(out=th[tk_][:], in_=pq, func=AF.Tanh, scale=0.5), reads=[pqk], writes=[("th", tk_)])
                            op("dve", lambda h, pq=pq, tk_=tk_: h.scalar_tensor_tensor(out=pa[tk_][:], in0=th[tk_][:], scalar=1.0, in1=pq, op0=ALU.add, op1=ALU.mult),
                               reads=[pqk, ("th", tk_)], writes=[("pa", tk_)])
                            akeys = [(blk, ii, x_) for ii in range(tg * 4, tg * 4 + 4) for x_ in ((0, 1) if gi < 4 else (fc // 2,))]
                            op("dve", lambda h, tk_=tk_, attT=attT, fc=fc, tg=tg: h.scalar_tensor_tensor(
                                out=attT[:, fc, tg * 512:(tg + 1) * 512], in0=pa[tk_][:], scalar=0.5, in1=attT[:, fc, tg * 512:(tg + 1) * 512],
                                op0=ALU.mult, op1=ALU.mult), reads=[("pa", tk_)] + akeys, writes=akeys)
                    load_cast(wo, "wo", "wo", w_o_d, 8, D, None, kcstep=2)
                    load_cast(wpg, "wpg", "wpg", w_pg_d, 8, D, None, kcstep=2)
                    load_cast(wp, "wp", "wp", w_p_d, 2, D, None, kcstep=2)

                    for half in range(2):
                        for dc in range(8):
                            k = dc % 2
                            load_cast(wmc[k], ("wmc", k), "wmc%d" % k, w_m_d, 8, 128, (dc * 128, 0))
                            load_cast(wmc[k], ("wmc", k), "wmc%d" % k, w_m_d, 8, 128, (D + dc * 128, 128))
                            load_cast(wbr[k], ("wbr", k), "wbr%d" % k, w_ba_d, 4, 128, (dc * 128, 0))
                            load_cast(wbr[k], ("wbr", k), "wbr%d" % k, w_bb_d, 4, 128, (dc * 128, 128))
                            for tgl in range(2):
                                tg = half * 2 + tgl
                                tsl = slice(tg * 512, (tg + 1) * 512)
                                sl2 = slice(tgl * 512, (tgl + 1) * 512)
                                ma, mb_, ya = PQ[0][:, sl2], PQ[1][:, sl2], PQ[2][:, sl2]
                                yb = PR[:, :]
                                for kc in range(8):
                                    op("pe", lambda h, kc=kc, ma=ma, k=k, tsl=tsl: h.matmul(ma, lhsT=wmc[k][:, kc, 0:128], rhs=hT[:, kc, tsl], start=(kc == 0), stop=(kc == 7)),
                                       reads=[("wmc", k), ("hT", tg)], writes=[("PQ", 0, tgl)])
                                for kc in range(8):
                                    op("pe", lambda h, kc=kc, mb_=mb_, k=k, tsl=tsl: h.matmul(mb_, lhsT=wmc[k][:, kc, 128:256], rhs=hT[:, kc, tsl], start=(kc == 0), stop=(kc == 7)),
                                       reads=[("wmc", k), ("hT", tg)], writes=[("PQ", 1, tgl)])
                                ak = [("qa_blk", ii, r_) for ii in range(tg * 4, tg * 4 + 4) for r_ in range(2)]
                                bk = [("qb_blk", ii, hg_) for ii in range(tg * 4, tg * 4 + 4) for hg_ in range(2)]
                                for fc in range(4):
                                    op("pe", lambda h, fc=fc, ya=ya, k=k, tsl=tsl: h.matmul(ya, lhsT=wbr[k][:, fc, 0:128], rhs=qaT[:, fc, tsl], start=(fc == 0), stop=(fc == 3)),
                                       reads=[("wbr", k)] + ak, writes=[("PQ", 2, tgl)])
                                for fc in range(4):
                                    op("pe", lambda h, fc=fc, yb=yb, k=k, tsl=tsl: h.matmul(yb, lhsT=wbr[k][:, fc, 128:256], rhs=qbT[:, fc, tsl], start=(fc == 0), stop=(fc == 3)),
                                       reads=[("wbr", k)] + bk, writes=["PR"])
                                op("act", lambda h, ma=ma, tgl=tgl: h.activation(out=th[tgl][:], in_=ma, func=AF.Tanh, scale=0.5), reads=[("PQ", 0, tgl)], writes=[("th", tgl)])
                                op("act", lambda h, mb_=mb_, tgl=tgl: h.activation(out=th[2 + tgl][:], in_=mb_, func=AF.Tanh, scale=0.5), reads=[("PQ", 1, tgl)], writes=[("th", 2 + tgl)])
                                op("dve", lambda h, ya=ya, tgl=tgl: h.scalar_tensor_tensor(out=pa[tgl][:], in0=th[tgl][:], scalar=1.0, in1=ya, op0=ALU.add, op1=ALU.mult),
                                   reads=[("th", tgl), ("PQ", 2, tgl)], writes=[("pa", tgl)])
                                op("dve", lambda h, yb=yb, tgl=tgl: h.scalar_tensor_tensor(out=pb[tgl][:], in0=th[2 + tgl][:], scalar=1.0, in1=yb, op0=ALU.add, op1=ALU.mult),
                                   reads=[("th", 2 + tgl), "PR"], writes=[("pb", tgl)])
                                op("pool", lambda h, tgl=tgl, dc=dc, sl2=sl2: h.tensor_tensor(out=mT[:, dc, sl2], in0=pa[tgl][:], in1=pb[tgl][:], op=ALU.add),
                                   reads=[("pa", tgl), ("pb", tgl)], writes=[("mT", tgl)])
                        for tt in range(8):
                            t = half * 8 + tt
                            xk = t % 2
                            tsl = slice(tt * 128, (tt + 1) * 128)
                            dma_in("sp", x3[xk][:], x_d[b, t * 128:(t + 1) * 128, :], ("x3", xk), "x3_%d" % xk)
                            op("pool", lambda h, xk=xk, t=t: h.dma_start(out=pbf[xk][:], in_=p_d[b, t * 128:(t + 1) * 128, :]), writes=[("pbf", xk)], lane="pbf%d" % xk)
                            for hf in range(2):
                                for dc in range(8):
                                    op("pe", lambda h, hf=hf, dc=dc, tsl=tsl: h.matmul(PQ[0][:, hf * 512:(hf + 1) * 512], lhsT=mT[:, dc, tsl], rhs=wo[:, dc, hf * 512:(hf + 1) * 512],
                                                                                      start=(dc == 0), stop=(dc == 7)),
                                       reads=[("mT", tt // 4), "wo"], writes=[("PQ", 0, hf)])
                            OK_ = [("PQ", 0, 0), ("PQ", 0, 1)]
                            op("act", lambda h: h.activation(out=junk3[:], in_=PQ[0][:, :], func=AF.Square, accum_out=s3[:, 0:1]), reads=OK_, writes=["junk3", "s3a"])
                            op("dve", lambda h: h.tensor_scalar(out=s3[:, 1:2], in0=s3[:, 0:1], scalar1=1.0 / D, scalar2=4.0 * EPS, op0=ALU.mult, op1=ALU.add),
                               reads=["s3a"], writes=["s3b"])
                            op("pool", lambda h: h.tensor_tensor(out=s3[:, 2:3], in0=s3[:, 1:2], in1=negh[:], op=ALU.pow), reads=["s3b", "negh"], writes=["s3c"])
                            op("dve", lambda h: h.scalar_tensor_tensor(out=tA[:], in0=PQ[0][:, :], scalar=s3[:, 2:3], in1=gpost[:], op0=ALU.mult, op1=ALU.mult),
                               reads=OK_ + ["s3c", "gpost"], writes=["tA"])
                            op("pool", lambda h, xk=xk: h.tensor_tensor(out=x1[:], in0=tA[:], in1=x3[xk][:], op=ALU.add), reads=["tA", ("x3", xk)], writes=["x1"])
                            op("act", lambda h: h.copy(out=x1b[:], in_=x1[:]), reads=["x1"], writes=["x1b"])
                            for kc in range(8):
                                op("pe", lambda h, kc=kc: h.transpose(out=PT3[:, kc * 128:(kc + 1) * 128], in_=x1b[:, kc * 128:(kc + 1) * 128], identity=ident_bf[:]),
                                   reads=["x1b", "ident_bf"], writes=["PT3"])
                            op("act", lambda h: h.copy(out=x1T[:], in_=PT3[:, :].rearrange("p (kc n) -> p kc n", kc=8)), reads=["PT3"], writes=["x1T"])
                            for hf in range(2):
                                for dc in range(8):
                                    op("pe", lambda h, hf=hf, dc=dc: h.matmul(PQ[1][:, hf * 512:(hf + 1) * 512], lhsT=x1T[:, dc, :], rhs=wpg[:, dc, hf * 512:(hf + 1) * 512],
                                                                             start=(dc == 0), stop=(dc == 7)),
                                       reads=["x1T", "wpg"], writes=[("PQ", 1, hf)])
                            for pc in range(2):
                                op("pe", lambda h, pc=pc, xk=xk: h.transpose(out=PT3[:, pc * 128:(pc + 1) * 128], in_=pbf[xk][:, pc * 128:(pc + 1) * 128], identity=ident_bf[:]),
                                   reads=[("pbf", xk), "ident_bf"], writes=["PT3"])
                            op("dve", lambda h: h.tensor_copy(out=pT[:], in_=PT3[:, 0:256].rearrange("p (kc n) -> p kc n", kc=2)), reads=["PT3"], writes=["pT"])
                            for hf in range(2):
                                for pc in range(2):
                                    op("pe", lambda h, hf=hf, pc=pc: h.matmul(PQ[2][:, hf * 512:(hf + 1) * 512], lhsT=pT[:, pc, :], rhs=wp[:, pc, hf * 512:(hf + 1) * 512],
                                                                             start=(pc == 0), stop=(pc == 1)),
                                       reads=["pT", "wp"], writes=[("PQ", 2, hf)])
                            GK = [("PQ", 1, 0), ("PQ", 1, 1)]
                            EK = [("PQ", 2, 0), ("PQ", 2, 1)]
                            op("act", lambda h: h.activation(out=gth[:], in_=PQ[1][:, :], func=AF.Tanh, scale=0.5), reads=GK, writes=["gth"])
                            op("dve", lambda h: h.scalar_tensor_tensor(out=ge2[:], in0=gth[:], scalar=1.0, in1=PQ[2][:, :], op0=ALU.add, op1=ALU.mult),
                               reads=["gth"] + EK, writes=["ge2"])
                            op("act", lambda h: h.activation(out=junk3[:], in_=ge2[:], func=AF.Square, accum_out=s3[:, 3:4]), reads=["ge2"], writes=["junk3", "s3d"])
                            op("dve", lambda h: h.tensor_scalar(out=s3[:, 4:5], in0=s3[:, 3:4], scalar1=1.0 / D, scalar2=4.0 * EPS, op0=ALU.mult, op1=ALU.add),
                               reads=["s3d"], writes=["s3e"])
                            op("pool", lambda h: h.tensor_tensor(out=s3[:, 5:6], in0=s3[:, 4:5], in1=negh[:], op=ALU.pow), reads=["s3e", "negh"], writes=["s3f"])
                            op("dve", lambda h: h.scalar_tensor_tensor(out=tA[:], in0=ge2[:], scalar=s3[:, 5:6], in1=gple[:], op0=ALU.mult, op1=ALU.mult),
                               reads=["ge2", "s3f", "gple"], writes=["tA"])
                            op("pool", lambda h, xk=xk: h.tensor_tensor(out=fin[xk][:], in0=tA[:], in1=x1[:], op=ALU.add), reads=["tA", "x1"], writes=[("fin", xk)])
                            out_marks.append(op("sp", lambda h, xk=xk, t=t: h.dma_start(out=out_d[b, t * 128:(t + 1) * 128, :], in_=fin[xk][:]),
                                                reads=[("fin", xk)], lane="fin%d" % xk))
        last = {}
        for (sk, v) in [m for m in out_marks if m]:
            last[sk] = max(last.get(sk, 0), v)
        T.wait_marks("sp", list(last.items()))
    T.close()
    build_program.stats = (T.nops, T.nwaits)
    return nc


def _consts():
    half = 8
    freqs = 500000.0 ** (-np.arange(half, dtype=np.float64) / half)
    pos = np.arange(S, dtype=np.float64)
    cosT = np.ones((128, S), np.float64)
    sinT = np.zeros((128, S), np.float64)
    perm = np.zeros((128, 128), np.float32)
    for hh in range(2):
        for d in range(16):
            f = hh * 64 + d
            ang = pos * freqs[d % 8]
            cosT[f] = np.cos(ang)
            sinT[f] = np.sin(ang)
            if d < 8:
                perm[f + 8, f] = -1.0
            else:
                perm[f - 8, f] = 1.0
    idx = np.arange(128)
    ident = np.eye(128, dtype=np.float32)
    trineg = np.where(idx[:, None] <= idx[None, :], 0.0, MASKNEG).astype(np.float32)
    causneg = np.where(idx[None, :] <= idx[:, None], 0.0, -1.0e9).astype(np.float32)
    U = (idx[:, None] <= idx[None, :]).astype(np.float32)
    sel0 = np.zeros((128, 128), np.float32)
    sel0[0, :] = 1.0
    ones = np.ones((128, 128), np.float32)
    cst = np.stack([ident, perm, trineg, causneg, U, sel0, ones], axis=1).astype(np.float32)
    pw = list(0.5 ** np.arange(1, NIT + 1, dtype=np.float64))
    pw2 = np.tile(np.array(pw + [pw[-1]])[None, :], (128, 1)).astype(np.float32)
    return cosT.astype(np.float32), sinT.astype(np.float32), np.ascontiguousarray(cst), pw2


def _run(inputs, nseq, seq_ids_per_core):
    x = np.asarray(inputs["x"], np.float32)
    p = np.asarray(inputs["p"], np.float32)[0]
    cosT, sinT, cst, pw2 = _consts()
    rep = lambda v: np.ascontiguousarray(np.broadcast_to(np.asarray(v, np.float32).reshape(1, -1), (128, np.asarray(v).size)))
    common = {
        "w_in": np.ascontiguousarray(np.asarray(inputs["w_in"], np.float32)[0]),
        "w_ba": np.ascontiguousarray(np.asarray(inputs["w_branch_a"], np.float32)[0]),
        "w_bb": np.ascontiguousarray(np.asarray(inputs["w_branch_b"], np.float32)[0]),
        "w_m": np.ascontiguousarray(np.asarray(inputs["w_merge"], np.float32)[0]),
        "w_o": np.ascontiguousarray(np.asarray(inputs["w_out"], np.float32)[0]),
        "w_p": np.ascontiguousarray(np.asarray(inputs["w_ple"], np.float32)[0]),
        "w_pg": np.ascontiguousarray(np.asarray(inputs["w_ple_gate"], np.float32)[0]),
        "gpre": rep(inputs["g_pre"][0]), "gpost": rep(inputs["g_post"][0]), "gple": rep(inputs["g_ple"][0]),
        "bfb": rep(inputs["b_forget"][0]),
        "cosT": cosT, "sinT": sinT, "cst": cst, "pw2": pw2,
    }
    nc = build_program(nseq)
    in_maps = []
    for c in range(NCORES):
        ids = seq_ids_per_core[c]
        m = dict(common)
        m["x"] = np.ascontiguousarray(x[ids])
        m["p"] = np.ascontiguousarray(p[ids])
        in_maps.append(m)
    res = run_bass_kernel_spmd(nc, in_maps, core_ids=list(range(NCORES)))
    return [r["out"] for r in res.results]


def kernel(**inputs):
    B = np.asarray(inputs["x"]).shape[0]
    nseq = B // NCORES
    ids = [list(range(c * nseq, (c + 1) * nseq)) for c in range(NCORES)]
    outs = _run(inputs, nseq, ids)
    return np.concatenate(outs, axis=0).astype(np.float32)
```

```python
import contextlib
import os
import numpy as np
import concourse.bass as bass
import concourse.mybir as mybir
from concourse.bass_utils import run_bass_kernel_spmd

F32 = mybir.dt.float32
BF16 = mybir.dt.bfloat16
ALU = mybir.AluOpType
AF = mybir.ActivationFunctionType
AX = mybir.AxisListType

NCORES = 8
S = 2048
D = 1024
NT = 16
DIN = 3792
OFF = dict(qa=0, ka=512, va=576, ga=640, qi=1152, ki=1664, wi=1728, qb=1736, kb=2248, vb=2760, fb=3272, gb=3280)
EPS = 1e-6
NIT = 16
STAGE = float(os.environ.get('MK_STAGE', '9'))
ATTACH = int(os.environ.get('MK_ATTACH', '0'))
TOPK = 256
MASKNEG = -30000.0
IDX_SCALE = (8 ** -0.5) * (64 ** -0.5)

ENGS = ("pe", "act", "dve", "pool", "sp")


class Trk:
    def __init__(self, nc):
        self.nc = nc
        self.h = {"pe": nc.tensor, "act": nc.scalar, "dve": nc.vector, "pool": nc.gpsimd, "sp": nc.sync}
        self.stack = contextlib.ExitStack()
        self.sems = {e: self.stack.enter_context(nc.semaphore("s_" + e)) for e in ENGS}
        self.cnt = {e: 0 for e in ENGS}
        self.lanecnt = {}
        self.clock = {e: {} for e in ENGS}
        self.snap = {}
        self.res = {}
        self.nwaits = 0
        self.nops = 0
        self.enabled = True
        self.pending = {e: None for e in ENGS}

    def _sem(self, sk):
        s = self.sems.get(sk)
        if s is None:
            s = self.stack.enter_context(self.nc.semaphore("l_%d" % len(self.sems)))
            self.sems[sk] = s
        return s

    def op(self, eng, fn, reads=(), writes=(), lane=None):
        if not self.enabled:
            return None
        deps = []
        for r in reads:
            st = self.res.get(r)
            if st and st[0]:
                deps.append(st[0])
        for w in writes:
            st = self.res.get(w)
            if st:
                if st[0]:
                    deps.append(st[0])
                deps.extend(st[1])
        clk = self.clock[eng]
        waits = {}
        if self.pending[eng]:
            deps.extend(self.pending[eng].items())
            self.pending[eng] = None
        for (sk, v) in deps:
            if sk == eng and eng == "pe":
                continue
            if clk.get(sk, 0) >= v:
                continue
            if waits.get(sk, 0) < v:
                waits[sk] = v
        if waits:
            clk = dict(clk)
            for sk, v in waits.items():
                sn = self.snap.get((sk, v))
                if sn:
                    for k2, v2 in sn.items():
                        if clk.get(k2, 0) < v2:
                            clk[k2] = v2
                if clk.get(sk, 0) < v:
                    clk[sk] = v
            self.clock[eng] = clk
        if lane is None:
            self.cnt[eng] += 1
            me = (eng, self.cnt[eng])
        else:
            sk = ("lane", lane)
            self.lanecnt[lane] = self.lanecnt.get(lane, 0) + 16
            me = (sk, self.lanecnt[lane])
        sn = dict(clk)
        sn[me[0]] = me[1]
        self.snap[me] = sn
        for r in reads:
            st = self.res.setdefault(r, [None, []])
            st[1].append(me)
        for w in writes:
            self.res[w] = [me, []]
        h = self.h[eng]
        wl = list(waits.items())
        self.nwaits += len(wl)
        self.nops += 1
        if wl and ATTACH:
            for sk, v in wl[:-1]:
                h.wait_ge(self._sem(sk), v)
            ins = fn(h)
            sk, v = wl[-1]
            ins = ins._wait_ge(self._sem(sk), v)
        else:
            for sk, v in wl:
                h.wait_ge(self._sem(sk), v)
            ins = fn(h)
        ins.then_inc(self._sem(me[0]), 16 if lane is not None else 1)
        return me

    def fence(self):
        marks = {e: c for e, c in self.cnt.items() if c}
        for ln, c in self.lanecnt.items():
            marks[("lane", ln)] = c
        for e in ENGS:
            self.pending[e] = dict(marks)

    def wait_marks(self, eng, marks):
        h = self.h[eng]
        for sk, v in marks:
            h.wait_ge(self._sem(sk), v)

    def close(self):
        self.stack.close()


def build_program(nseq, dbg=False):
    nc = bass.Bass("TRN2", target_bir_lowering=False)

    def din(name, shape):
        return nc.dram_tensor(name, shape, F32, kind="ExternalInput").ap()

    x_d = din("x", [nseq, S, D])
    p_d = din("p", [nseq, S, 256])
    w_in_d = din("w_in", [D, DIN])
    w_ba_d = din("w_ba", [512, D])
    w_bb_d = din("w_bb", [512, D])
    w_m_d = din("w_m", [D, 2 * D])
    w_o_d = din("w_o", [D, D])
    w_p_d = din("w_p", [256, D])
    w_pg_d = din("w_pg", [D, D])
    gpre_d = din("gpre", [128, D])
    gpost_d = din("gpost", [128, D])
    gple_d = din("gple", [128, D])
    bfb_d = din("bfb", [128, 8])
    cos_d = din("cosT", [128, S])
    sin_d = din("sinT", [128, S])
    cst_d = din("cst", [128, 7, 128])
    pw2_d = din("pw2", [128, NIT + 1])
    out_d = nc.dram_tensor("out", [nseq, S, D], F32, kind="ExternalOutput").ap()
    dbg_d = {}

    T = Trk(nc)
    op = T.op
    uid = [0]

    def dma_in(eng, out_ap, in_ap, key, lane):
        return op(eng, lambda h: h.dma_start(out=out_ap, in_=in_ap), writes=[key], lane=lane)

    with contextlib.ExitStack() as g:
        def sb(name, shape, dt, es=g):
            uid[0] += 1
            return es.enter_context(nc.sbuf_tensor("%s_u%d" % (name, uid[0]), shape, dt))

        def ps(name, shape, dt, es=g):
            uid[0] += 1
            return es.enter_context(nc.psum_tensor("%s_u%d" % (name, uid[0]), shape, dt))

        cst = sb("cst", [128, 7, 128], F32)
        ident_bf = sb("ident_bf", [128, 128], BF16)
        trineg_bf = sb("trineg_bf", [128, 128], BF16)
        irep_bf = sb("irep_bf", [128, 512], BF16)
        ones_bf = sb("ones_bf", [128, 64], BF16)
        gpre = sb("gpre_s", [128, D], F32)
        gpost = sb("gpost_s", [128, D], F32)
        gple = sb("gple_s", [128, D], F32)
        bfb = sb("bfb_s", [128, 8], F32)
        pw2 = sb("pw2_s", [128, NIT + 1], F32)
        negh = sb("negh", [128, 1], F32)
        taum = sb("taum", [128, 1], F32)
        ident_f = cst[:, 0, :]
        perm_f = cst[:, 1, :]
        causneg_f = cst[:, 3, :]
        U_f = cst[:, 4, :]
        sel0_f = cst[:, 5, :]
        ones_f = cst[:, 6, :]

        dma_in("sp", cst[:], cst_d[:, :, :], "cst", "c0")
        dma_in("sp", gpre[:], gpre_d[:, :], "gpre", "c1")
        dma_in("sp", gpost[:], gpost_d[:, :], "gpost", "c2")
        dma_in("sp", gple[:], gple_d[:, :], "gple", "c3")
        dma_in("sp", bfb[:], bfb_d[:, :], "bfb", "c4")
        dma_in("sp", pw2[:], pw2_d[:, :], "pw2", "c5")
        dma_in("pool", ident_bf[:], cst_d[:, 0, :], "ident_bf", "c6")
        dma_in("pool", trineg_bf[:], cst_d[:, 2, :], "trineg_bf", "c7")
        for k in range(4):
            op("pool", lambda h, k=k: h.dma_start(out=irep_bf[:, k * 128:(k + 1) * 128], in_=cst_d[:, 0, :]),
               writes=[("irep", k)], lane="c8")
        IREP = [("irep", k) for k in range(4)]
        op("pool", lambda h: h.memset(ones_bf[:], 1.0), writes=["ones_bf"])
        op("pool", lambda h: h.memset(negh[:], -0.5), writes=["negh"])
        op("pool", lambda h: h.memset(taum[:], -1.0e8), writes=["taum"])

        out_marks = []

        for b in range(nseq):
            T.fence()
            with contextlib.ExitStack() as sq:
                hT = sb("hT", [128, 8, S], BF16, sq)
                qaT = sb("qaT", [128, 4, S], BF16, sq)
                qbT = sb("qbT", [128, 4, S], BF16, sq)
                with contextlib.ExitStack() as p12:
                    qiT = sb("qiT", [128, 4, S], BF16, p12)
                    kaT = sb("kaT", [128, S], BF16, p12)
                    kiT = sb("kiT", [128, S], BF16, p12)
                    kbT = sb("kbT", [128, 4, S], BF16, p12)
                    va = sb("va", [128, NT, 128], BF16, p12)
                    vb = sb("vb", [128, NT, 512], BF16, p12)
                    wi = sb("wi", [128, NT, 8], F32, p12)
                    fbs = sb("fbs", [128, NT, 8], F32, p12)
                    csb = sb("csb", [128, NT, 8], F32, p12)
                    carry = sb("carry", [128, NT, 8], F32, p12)
                    with contextlib.ExitStack() as p1:
                        T.enabled = STAGE >= 1
                        cosT = sb("cosT_s", [128, S], F32, p1)
                        sinT = sb("sinT_s", [128, S], F32, p1)
                        xt = [sb("xt%d" % k, [128, D], F32, p1) for k in range(2)]
                        junk = sb("junk1", [128, D], BF16, p1)
                        hb = [sb("hb%d" % k, [128, D], BF16, p1) for k in range(2)]
                        wbuf = [sb("wbuf%d" % k, [128, 8, 512], BF16, p1) for k in range(2)]
                        xs = [sb("xs%d" % k, [128, 512], F32, p1) for k in range(2)]
                        t1 = [sb("t1_0", [128, 512], F32, p1)] * 2
                        t2 = [sb("t2_0", [128, 512], F32, p1)] * 2
                        st = sb("st1", [128, 4], F32, p1)
                        wsm = sb("wsm", [128, 8, 80], BF16, p1)
                        sm16 = sb("sm16", [128, 16], F32, p1)
                        wraw = sb("wraw", [128, 8, 208], BF16, p1)
                        PA = [ps("PA%d" % k, [128, 512], F32, p1) for k in range(4)]
                        PB = [ps("PB%d" % k, [128, 512], F32, p1) for k in range(2)]
                        PTB = ps("PTB1", [128, 1024], BF16, p1)
                        PC = ps("PC1", [128, 512], F32, p1)

                        dma_in("sp", cosT[:], cos_d[:, :], "cosT", "cos")
                        dma_in("sp", sinT[:], sin_d[:, :], "sinT", "sin")
                        op("pool", lambda h: h.memset(va[:, :, 64:128], 1.0), writes=["va_ones"])

                        wl_state = {"n": 0}

                        def load_w(segments):
                            k = wl_state["n"] % 2
                            wl_state["n"] += 1
                            c = 0
                            for (c0, ncol) in segments:
                                for kc0 in range(0, 8, 4):
                                    op("pool", lambda h, k=k, c=c, c0=c0, ncol=ncol, kc0=kc0: h.dma_start(
                                        out=wbuf[k][:, kc0:kc0 + 4, c:c + ncol],
                                        in_=w_in_d[kc0 * 128:(kc0 + 4) * 128, c0:c0 + ncol].rearrange("(kc kp) n -> kp kc n", kp=128)),
                                       writes=[("wbuf", k)], lane="wbuf%d" % k)
                                c += ncol
                            return k

                        for t in range(NT):
                            k = t % 2
                            dma_in("sp", xt[k][:], x_d[b, t * 128:(t + 1) * 128, :], ("xt", k), "xt%d" % k)
                            op("act", lambda h, k=k: h.activation(out=junk[:], in_=xt[k][:], func=AF.Square, accum_out=st[:, 0:1]),
                               reads=[("xt", k)], writes=["junk1", "st0"])
                            op("dve", lambda h: h.tensor_scalar(out=st[:, 1:2], in0=st[:, 0:1], scalar1=1.0 / D, scalar2=EPS,
                                                                op0=ALU.mult, op1=ALU.add), reads=["st0"], writes=["st1"])
                            op("pool", lambda h: h.tensor_tensor(out=st[:, 2:3], in0=st[:, 1:2], in1=negh[:], op=ALU.pow),
                               reads=["st1", "negh"], writes=["st2"])
                            op("dve", lambda h, k=k: h.scalar_tensor_tensor(out=hb[k][:], in0=xt[k][:], scalar=st[:, 2:3], in1=gpre[:],
                                                                            op0=ALU.mult, op1=ALU.mult),
                               reads=[("xt", k), "st2", "gpre"], writes=[("hb", k)])
                            for kc in range(8):
                                op("pe", lambda h, k=k, kc=kc: h.transpose(out=PTB[:, kc * 128:(kc + 1) * 128], in_=hb[k][:, kc * 128:(kc + 1) * 128],
                                                                           identity=ident_bf[:]),
                                   reads=[("hb", k), "ident_bf"], writes=["PTB1"])
                            op("act", lambda h, t=t: h.copy(out=hT[:, :, t * 128:(t + 1) * 128], in_=PTB[:, :].rearrange("p (kc n) -> p kc n", kc=8)),
                               reads=["PTB1"], writes=[("hT", t // 4)])

                        T.enabled = STAGE >= 1.2
                        pa_i = [0]

                        def proj_fm(k, cofs, tg):
                            slot = pa_i[0] % 4
                            pa_i[0] += 1
                            for kc in range(8):
                                op("pe", lambda h, kc=kc, slot=slot: h.matmul(PA[slot][:, :], lhsT=wbuf[k][:, kc, cofs:cofs + 128],
                                                                             rhs=hT[:, kc, tg * 512:(tg + 1) * 512], start=(kc == 0), stop=(kc == 7)),
                                   reads=[("wbuf", k), ("hT", tg)], writes=[("PA", slot)])
                            return slot

                        rp_i = [0]

                        def rope_evac(slot, dst_ap, dst_key, tg):
                            r = rp_i[0] % 2
                            rp_i[0] += 1
                            tsl = slice(tg * 512, (tg + 1) * 512)
                            op("act", lambda h: h.copy(out=xs[r][:], in_=PA[slot][:, :]), reads=[("PA", slot)], writes=[("xs", r)])
                            op("pe", lambda h: h.matmul(PB[r][:, :], lhsT=perm_f, rhs=xs[r][:], start=True, stop=True),
                               reads=[("xs", r), "cst"], writes=[("PB", r)])
                            op("dve", lambda h: h.tensor_tensor(out=t1[r][:], in0=PB[r][:, :], in1=sinT[:, tsl], op=ALU.mult),
                               reads=[("PB", r), "sinT"], writes=["t1"])
                            op("pool", lambda h: h.tensor_tensor(out=t2[r][:], in0=xs[r][:], in1=cosT[:, tsl], op=ALU.mult),
                               reads=[("xs", r), "cosT"], writes=["t2"])
                            op("dve", lambda h: h.tensor_tensor(out=dst_ap, in0=t1[r][:], in1=t2[r][:], op=ALU.add),
                               reads=["t1", "t2"], writes=[dst_key])

                        for (c0, ncol, dc0) in ((OFF["va"], 64, 0), (OFF["wi"] - 64, 72, 64), (OFF["fb"] - 64, 72, 136)):
                            for kc0 in range(0, 8, 4):
                                op("pool", lambda h, c0=c0, ncol=ncol, dc0=dc0, kc0=kc0: h.dma_start(
                                    out=wraw[:, kc0:kc0 + 4, dc0:dc0 + ncol],
                                    in_=w_in_d[kc0 * 128:(kc0 + 4) * 128, c0:c0 + ncol].rearrange("(kc kp) n -> kp kc n", kp=128)),
                                   writes=["wraw"], lane="wraw")
                        op("dve", lambda h: h.tensor_copy(out=wsm[:, :, 0:64], in_=wraw[:, :, 0:64]), reads=["wraw"], writes=["wsm"])
                        op("dve", lambda h: h.tensor_copy(out=wsm[:, :, 64:72], in_=wraw[:, :, 128:136]), reads=["wraw"], writes=["wsm"])
                        op("dve", lambda h: h.tensor_copy(out=wsm[:, :, 72:80], in_=wraw[:, :, 200:208]), reads=["wraw"], writes=["wsm"])

                        def g_rope4(nm, dst):
                            def f(k):
                                for c in range(4):
                                    for tg in range(4):
                                        slot = proj_fm(k, c * 128, tg)
                                        rope_evac(slot, dst[:, c, tg * 512:(tg + 1) * 512], (nm + "T", c, tg), tg)
                            return f

                        def g_kk(k):
                            for c, (nm, dst) in enumerate((("ka", kaT), ("ki", kiT))):
                                for tg in range(4):
                                    slot = proj_fm(k, c * 128, tg)
                                    rope_evac(slot, dst[:, tg * 512:(tg + 1) * 512], (nm + "T", tg), tg)

                        def g_plain4(nm, dst):
                            def f(k):
                                for c in range(4):
                                    for tg in range(4):
                                        slot = proj_fm(k, c * 128, tg)
                                        op("act", lambda h, slot=slot, c=c, tg=tg: h.copy(out=dst[:, c, tg * 512:(tg + 1) * 512], in_=PA[slot][:, :]),
                                           reads=[("PA", slot)], writes=[(nm + "T", c, tg)])
                            return f

                        def g_tok(k):
                            for t in range(NT):
                                slot = pa_i[0] % 4
                                pa_i[0] += 1
                                for kc in range(8):
                                    op("pe", lambda h, kc=kc, slot=slot, t=t: h.matmul(PA[slot][:, :], lhsT=hT[:, kc, t * 128:(t + 1) * 128],
                                                                                      rhs=wbuf[k][:, kc, 0:512], start=(kc == 0), stop=(kc == 7)),
                                       reads=[("wbuf", k), ("hT", t // 4)], writes=[("PA", slot)])
                                op("act", lambda h, slot=slot, t=t: h.copy(out=vb[:, t, :], in_=PA[slot][:, :]), reads=[("PA", slot)], writes=[("vb", t)])
                                for kc in range(8):
                                    op("pe", lambda h, kc=kc, t=t: h.matmul(PC[:, 0:80], lhsT=hT[:, kc, t * 128:(t + 1) * 128],
                                                                            rhs=wsm[:, kc, 0:80], start=(kc == 0), stop=(kc == 7)),
                                       reads=["wsm", ("hT", t // 4)], writes=["PC1"])
                                op("act", lambda h, t=t: h.copy(out=va[:, t, 0:64], in_=PC[:, 0:64]), reads=["PC1"], writes=[("va", t)])
                                op("act", lambda h: h.copy(out=sm16[:], in_=PC[:, 64:80]), reads=["PC1"], writes=["sm16"])
                                op("act", lambda h, t=t: h.mul(out=wi[:, t, :], in_=sm16[:, 0:8], mul=IDX_SCALE),
                                   reads=["sm16"], writes=[("wi", t)])
                                op("pool", lambda h, t=t: h.tensor_tensor(out=fbs[:, t, :], in0=sm16[:, 8:16], in1=bfb[:], op=ALU.add),
                                   reads=["sm16", "bfb"], writes=["fbs"])

                        groups = [
                            ([(OFF["qa"], 512)], g_rope4("qa", qaT)),
                            ([(OFF["qi"], 512)], g_rope4("qi", qiT)),
                            ([(OFF["ka"], 64), (OFF["ka"], 64), (OFF["ki"], 64), (OFF["ki"], 64)], g_kk),
                            ([(OFF["qb"], 512)], g_plain4("qb", qbT)),
                            ([(OFF["kb"], 512)], g_plain4("kb", kbT)),
                            ([(OFF["vb"], 512)], g_tok),
                        ]
                        kcur = load_w(groups[0][0])
                        for gi_, (segs_, fn_) in enumerate(groups):
                            knext = load_w(groups[gi_ + 1][0]) if gi_ + 1 < len(groups) else None
                            fn_(kcur)
                            kcur = knext
                    T.fence()
                    with contextlib.ExitStack() as p2:
                        T.enabled = STAGE >= 2
                        score = [sb("score%d" % k, [128, S], F32, p2) for k in range(2)]
                        tmp = [sb("tmp%d" % k, [128, 512], F32, p2) for k in range(2)]
                        junk2 = sb("junk2", [128, S], BF16, p2)
                        mneg = [sb("mneg%d" % k, [128, S], BF16, p2) for k in range(3)]
                        ptd = [sb("ptd%d" % k, [128, 512], BF16, p2) for k in range(3)]
                        ptf = [sb("ptf%d" % k, [128, 128], BF16, p2) for k in range(3)]
                        dend = sb("dend", [128, 512], F32, p2)
                        denf = sb("denf", [128, 512], F32, p2)
                        sst = [sb("sst%d" % k, [128, 8 + NIT + 1], F32, p2) for k in range(2)]
                        crefb = sb("crefb", [128, NT, 8], F32, p2)
                        biasT = sb("biasT", [128, NT, NT, 8], F32, p2)
                        IDX = ps("IDX", [128, 512], F32, p2)
                        DS = [ps("DS%d" % k, [128, 512], F32, p2) for k in range(2)]
                        DACC = ps("DACC", [128, 512], F32, p2)
                        FS = [ps("FS%d" % k, [128, 512], F32, p2) for k in range(3)]
                        FACC = ps("FACC", [128, 512], F32, p2)
                        cnt = {"idx": 0, "ptd": 0, "fs": 0}
                        fl = fbs[:].rearrange("p t h -> p (t h)")
                        cl = csb[:].rearrange("p t h -> p (t h)")
                        op("act", lambda h: h.activation(out=fl, in_=fl, func=AF.Exp, scale=-1.0), reads=["fbs"], writes=["fbs"])
                        op("act", lambda h: h.activation(out=fl, in_=fl, func=AF.Ln, bias=1.0, scale=1.0), reads=["fbs"], writes=["fbs"])
                        op("dve", lambda h: h.tensor_scalar(out=fl, in0=fl, scalar1=-1.0, scalar2=None, op0=ALU.mult), reads=["fbs"], writes=["fbs"])
                        op("pe", lambda h: h.matmul(IDX[:, 0:128], lhsT=U_f, rhs=fl, start=True, stop=True), reads=["fbs", "cst"], writes=[("IDX", 0)])
                        op("pe", lambda h: h.matmul(FS[0][:, 0:128], lhsT=ones_f, rhs=fl, start=True, stop=True), reads=["fbs", "cst"], writes=[("FS", 0)])
                        op("dve", lambda h: h.memset(carry[:, 0, :], 0.0), writes=["carry"])
                        for t in range(1, NT):
                            op("dve", lambda h, t=t: h.tensor_tensor(out=carry[:, t, :], in0=carry[:, t - 1, :], in1=FS[0][:, (t - 1) * 8:t * 8], op=ALU.add),
                               reads=[("FS", 0), "carry"], writes=["carry"])
                        op("dve", lambda h: h.tensor_tensor(out=cl, in0=IDX[:, 0:128], in1=carry[:].rearrange("p t h -> p (t h)"), op=ALU.add),
                           reads=[("IDX", 0), "carry"], writes=["csb"])
                        op("pe", lambda h: h.matmul(IDX[:, 0:128], lhsT=sel0_f, rhs=cl, start=True, stop=True), reads=["csb", "cst"], writes=[("IDX", 0)])
                        op("act", lambda h: h.copy(out=crefb[:].rearrange("p t h -> p (t h)"), in_=IDX[:, 0:128]), reads=[("IDX", 0)], writes=["crefb"])
                        for i in range(NT):
                            op("dve", lambda h, i=i: h.tensor_tensor(out=biasT[:, i, 0:i + 1, :],
                                                                     in0=crefb[:, i, :].unsqueeze(1).broadcast_to([128, i + 1, 8]),
                                                                     in1=csb[:, 0:i + 1, :], op=ALU.subtract),
                               reads=["crefb", "csb"], writes=["biasT"])

                        def dsa_index(i):
                            items = []
                            sp_ = i % 2
                            L = 128 * (i + 1)
                            sc = score[sp_]
                            nch = (L + 511) // 512
                            s_ = sst[sp_]
                            SK = ("sst", sp_)
                            for hd in range(8):
                                c, r = hd // 2, hd % 2
                                for kk in range(nch):
                                    def chunk(hd=hd, c=c, r=r, kk=kk):
                                        w = min(512, L - 512 * kk)
                                        sl = cnt["idx"] % 2
                                        cnt["idx"] += 1
                                        op("pe", lambda h: h.matmul(
                                            IDX[:, 0:w], lhsT=qiT[64 * r:64 * r + 64, c, i * 128:(i + 1) * 128],
                                            rhs=kiT[64 * r:64 * r + 64, kk * 512:kk * 512 + w], start=True, stop=True),
                                           reads=[("qiT", c, i // 4), ("kiT", kk)], writes=[("IDX", 0)])
                                        op("act", lambda h: h.activation(out=tmp[sl][:, 0:w], in_=IDX[:, 0:w], func=AF.Relu),
                                           reads=[("IDX", 0)], writes=[("tmp", sl)])
                                        if hd == 0:
                                            op("dve", lambda h: h.tensor_scalar(
                                                out=sc[:, kk * 512:kk * 512 + w], in0=tmp[sl][:, 0:w], scalar1=wi[:, i, 0:1], scalar2=None, op0=ALU.mult),
                                               reads=[("tmp", sl), ("wi", i)], writes=[("score", sp_)])
                                        else:
                                            op("dve", lambda h: h.scalar_tensor_tensor(
                                                out=sc[:, kk * 512:kk * 512 + w], in0=tmp[sl][:, 0:w], scalar=wi[:, i, hd:hd + 1],
                                                in1=sc[:, kk * 512:kk * 512 + w], op0=ALU.mult, op1=ALU.add),
                                               reads=[("tmp", sl), ("wi", i), ("score", sp_)], writes=[("score", sp_)])
                                    items.append(chunk)

                            def prep():
                                op("dve", lambda h: h.tensor_tensor(out=sc[:, i * 128:L], in0=sc[:, i * 128:L], in1=causneg_f, op=ALU.add),
                                   reads=[("score", sp_), "cst"], writes=[("score", sp_)])
                                if i >= 2:
                                    op("dve", lambda h: h.tensor_reduce(out=s_[:, 0:1], in_=sc[:, 0:128 * i], axis=AX.X, op=ALU.min),
                                       reads=[("score", sp_)], writes=[SK])
                                    op("dve", lambda h: h.tensor_reduce(out=s_[:, 1:2], in_=sc[:, 0:L], axis=AX.X, op=ALU.max),
                                       reads=[("score", sp_)], writes=[SK])
                                    op("dve", lambda h: h.tensor_tensor(out=s_[:, 2:3], in0=s_[:, 1:2], in1=s_[:, 0:1], op=ALU.subtract),
                                       reads=[SK], writes=[SK])
                                    op("dve", lambda h: h.tensor_scalar(out=s_[:, 8:9 + NIT], in0=pw2[:], scalar1=s_[:, 2:3], scalar2=None, op0=ALU.mult),
                                       reads=[SK, "pw2"], writes=[SK])
                                    op("dve", lambda h: h.tensor_tensor(out=s_[:, 3:4], in0=s_[:, 0:1], in1=s_[:, 8:9], op=ALU.add),
                                       reads=[SK], writes=[SK])
                            items.append(prep)
                            if i >= 2:
                                for it in range(NIT):
                                    def iteration(it=it):
                                        op("dve", lambda h: h.tensor_scalar(out=junk2[:, 0:L], in0=sc[:, 0:L], scalar1=s_[:, 3:4], scalar2=None,
                                                                            op0=ALU.is_ge, op1=ALU.add, accum_out=s_[:, 4:5]),
                                           reads=[SK, ("score", sp_)], writes=[SK, "junk2"])
                                        op("dve", lambda h: h.scalar_tensor_tensor(out=s_[:, 5:6], in0=s_[:, 4:5], scalar=TOPK - 0.5, in1=s_[:, 8 + it:9 + it],
                                                                                   op0=ALU.is_ge, op1=ALU.mult),
                                           reads=[SK], writes=[SK])
                                        op("dve", lambda h: h.scalar_tensor_tensor(out=s_[:, 3:4], in0=s_[:, 5:6], scalar=s_[:, 9 + it:10 + it], in1=s_[:, 3:4],
                                                                                   op0=ALU.subtract, op1=ALU.add),
                                           reads=[SK], writes=[SK])
                                    items.append(iteration)

                            def fin():
                                if i >= 2:
                                    tau, tk = s_[:, 3:4], [SK]
                                else:
                                    tau, tk = taum[:], ["taum"]
                                op("dve", lambda h: h.tensor_scalar(out=mneg[i % 3][:, 0:L], in0=sc[:, 0:L], scalar1=tau, scalar2=MASKNEG,
                                                                    op0=ALU.is_lt, op1=ALU.mult),
                                   reads=tk + [("score", sp_)], writes=[("mneg", i % 3)])
                            items.append(fin)
                            return items

                        class Pipe:
                            def __init__(self, skew, batch):
                                self.q, self.skew, self.batch, self.pf = [], skew, batch, []

                            def push(self, front, back):
                                self.pf.append((front, back))
                                if len(self.pf) >= self.batch:
                                    self._go()

                            def _go(self):
                                for f, _ in self.pf:
                                    f()
                                self.q.extend(bk for _, bk in self.pf)
                                self.pf = []
                                while len(self.q) > self.skew:
                                    self.q.pop(0)()

                            def flush(self):
                                if self.pf:
                                    self._go()
                                while self.q:
                                    self.q.pop(0)()

                        def dsa_units(i):
                            units = []
                            for half in range(2):
                                for j in range(i + 1):
                                    pk = cnt["ptd"] % 3
                                    ds = cnt["ptd"] % 2
                                    cnt["ptd"] += 1

                                    def front(j=j, half=half, ds=ds):
                                        D_ = DS[ds]
                                        op("pe", lambda h: h.matmul(D_[:, :], lhsT=mneg[i % 3][:, j * 128:(j + 1) * 128], rhs=irep_bf[:],
                                                                    start=True, stop=False),
                                           reads=[("mneg", i % 3)] + IREP, writes=[("DS", ds)])
                                        for hh in range(4):
                                            c, r = hh, half
                                            op("pe", lambda h, hh=hh, c=c, r=r: h.matmul(
                                                D_[:, hh * 128:(hh + 1) * 128], lhsT=kaT[64 * r:64 * r + 64, j * 128:(j + 1) * 128],
                                                rhs=qaT[64 * r:64 * r + 64, c, i * 128:(i + 1) * 128], start=False, stop=(hh == 3)),
                                               reads=[("kaT", j // 4), ("qaT", c, i // 4), ("qa_blk", i, r)], writes=[("DS", ds)])

                                    def back(j=j, half=half, pk=pk, ds=ds):
                                        D_ = DS[ds]
                                        op("act", lambda h: h.activation(out=ptd[pk][:], in_=D_[:, :], func=AF.Exp, scale=0.125),
                                           reads=[("DS", ds)], writes=[("ptd", pk)])
                                        op("pe", lambda h: h.matmul(DACC[:, :], lhsT=va[:, j, :], rhs=ptd[pk][:],
                                                                    start=(j == 0), stop=(j == i)),
                                           reads=[("ptd", pk), ("va", j), "va_ones"], writes=["DACC"])
                                    units.append((front, back))

                                def epi(half=half):
                                    r = half
                                    op("act", lambda h: h.copy(out=dend[64:128, :], in_=DACC[64:128, :]), reads=["DACC"], writes=["dend"])
                                    op("dve", lambda h: h.reciprocal(out=dend[64:128, :], in_=dend[64:128, :]), reads=["dend"], writes=["dend"])
                                    op("dve", lambda h: h.tensor_tensor(
                                        out=qaT[64 * r:64 * r + 64, :, i * 128:(i + 1) * 128],
                                        in0=DACC[0:64, :].rearrange("p (c q) -> p c q", c=4),
                                        in1=dend[64:128, :].rearrange("p (c q) -> p c q", c=4), op=ALU.mult),
                                       reads=["DACC", "dend"], writes=[("qa_blk", i, r)])
                                units.append((lambda: None, epi))
                            return units

                        def fox_units(i):
                            units = []
                            for hg in range(2):
                                for hh in range(4):
                                    hd = hg * 4 + hh
                                    c, r = hd // 2, hd % 2
                                    for j in range(i + 1):
                                        sl = cnt["fs"] % 3
                                        cnt["fs"] += 1

                                        def front(j=j, sl=sl, c=c, r=r):
                                            F_ = FS[sl][:, 0:128]
                                            op("pe", lambda h: h.matmul(
                                                F_, lhsT=kbT[64 * r:64 * r + 64, c, j * 128:(j + 1) * 128],
                                                rhs=qbT[64 * r:64 * r + 64, c, i * 128:(i + 1) * 128], start=True, stop=(j != i)),
                                               reads=[("kbT", c, j // 4), ("qbT", c, i // 4), ("qb_blk", i, c // 2)], writes=[("FS", sl)])
                                            if j == i:
                                                op("pe", lambda h: h.matmul(F_, lhsT=ident_bf[:], rhs=trineg_bf[:], start=False, stop=True),
                                                   reads=["ident_bf", "trineg_bf"], writes=[("FS", sl)])

                                        def back(j=j, sl=sl, hd=hd, hh=hh):
                                            F_ = FS[sl][:, 0:128]
                                            op("act", lambda h: h.activation(out=ptf[sl][:], in_=F_, func=AF.Exp, scale=0.125,
                                                                             bias=biasT[:, i, j, hd:hd + 1]),
                                               reads=[("FS", sl), "biasT"], writes=[("ptf", sl)])
                                            op("pe", lambda h: h.matmul(FACC[0:64, hh * 128:(hh + 1) * 128], lhsT=vb[:, j, hd * 64:(hd + 1) * 64],
                                                                        rhs=ptf[sl][:], start=(j == 0), stop=(j == i)),
                                               reads=[("ptf", sl), ("vb", j)], writes=["FACCn"])
                                            op("pe", lambda h: h.matmul(FACC[64:128, hh * 128:(hh + 1) * 128], lhsT=ones_bf[:, 0:64],
                                                                        rhs=ptf[sl][:], start=(j == 0), stop=(j == i)),
                                               reads=[("ptf", sl), "ones_bf"], writes=["FACCd"])
                                        units.append((front, back))

                                def epi(hg=hg):
                                    FK = ["FACCn", "FACCd"]
                                    op("act", lambda h: h.copy(out=denf[64:128, :], in_=FACC[64:128, :]), reads=FK, writes=["denf"])
                                    op("dve", lambda h: h.reciprocal(out=denf[64:128, :], in_=denf[64:128, :]), reads=["denf"], writes=["denf"])
                                    for r in range(2):
                                        op("dve", lambda h, r=r: h.tensor_tensor(
                                            out=qbT[64 * r:64 * r + 64, 2 * hg:2 * hg + 2, i * 128:(i + 1) * 128],
                                            in0=FACC[0:64, :].rearrange("p (c r q) -> p c r q", c=2, r=2)[:, :, r, :],
                                            in1=denf[64:128, :].rearrange("p (c r q) -> p c r q", c=2, r=2)[:, :, r, :], op=ALU.mult),
                                           reads=FK + ["denf"], writes=[("qb_blk", i, hg)])
                                units.append((None, epi))
                            return units

                        dpipe = Pipe(1, 1)
                        fpipe = Pipe(2, 1)
                        for it_ in dsa_index(0) + dsa_index(1):
                            it_()
                        for i in range(NT):
                            items = dsa_index(i + 2) if i + 2 < NT else []
                            du = dsa_units(i)
                            fu = fox_units(i)
                            nF = len(fu)
                            rate = -(-len(items) // max(1, int(0.7 * nF)))
                            di = 0
                            ii = 0
                            for n_, (f, bk) in enumerate(fu):
                                if f is None:
                                    fpipe.flush()
                                    bk()
                                else:
                                    fpipe.push(f, bk)
                                for _ in range(rate):
                                    if ii < len(items):
                                        items[ii]()
                                        ii += 1
                                if n_ % 4 == 3 and di < len(du):
                                    dpipe.push(*du[di])
                                    di += 1
                            while ii < len(items):
                                items[ii]()
                                ii += 1
                            while di < len(du):
                                dpipe.push(*du[di])
                                di += 1
                        dpipe.flush()
                        fpipe.flush()
                T.fence()
                with contextlib.ExitStack() as p3:
                    T.enabled = STAGE >= 3
                    wo = sb("wo", [128, 8, D], BF16, p3)
                    wpg = sb("wpg", [128, 8, D], BF16, p3)
                    wp = sb("wp", [128, 2, D], BF16, p3)
                    mT = sb("mT", [128, 8, 1024], BF16, p3)
                    wg = [sb("wg%d" % k, [128, 8, 128], BF16, p3) for k in range(2)]
                    wmc = [sb("wmc%d" % k, [128, 8, 256], BF16, p3) for k in range(2)]
                    wbr = [sb("wbr%d" % k, [128, 4, 256], BF16, p3) for k in range(2)]
                    th = [sb("th%d" % k, [128, 512], BF16, p3) for k in range(4)]
                    pa = [sb("pa%d" % k, [128, 512], F32, p3) for k in range(2)]
                    pb = [sb("pb%d" % k, [128, 512], F32, p3) for k in range(2)]
                    x3 = [sb("x3_%d" % k, [128, D], F32, p3) for k in range(2)]
                    pbf = [sb("pbf%d" % k, [128, 256], BF16, p3) for k in range(2)]
                    pT = [sb("pT%d" % k, [128, 2, 128], BF16, p3) for k in range(2)]
                    junk3 = sb("junk3", [128, D], BF16, p3)
                    tA = sb("tA", [128, D], F32, p3)
                    x1 = [sb("x1_%d" % k, [128, D], F32, p3) for k in range(2)]
                    x1b = sb("x1b", [128, D], BF16, p3)
                    x1T = [sb("x1T%d" % k, [128, 8, 128], BF16, p3) for k in range(2)]
                    gth = sb("gth", [128, D], BF16, p3)
                    ge2 = sb("ge2", [128, D], F32, p3)
                    fin = [sb("fin%d" % k, [128, D], F32, p3) for k in range(2)]
                    s3 = sb("s3", [128, 8], F32, p3)
                    PQ = [ps("PQ%d" % k, [128, 1024], F32, p3) for k in range(3)]
                    PR = ps("PR", [128, 512], F32, p3)
                    PT3 = ps("PT3", [128, 1024], BF16, p3)

                    def load_cast(dst, key, lane, src2d, nkc, ncol, col0, kcstep=4):
                        scol, dcol = (0, 0) if col0 is None else col0
                        for kc0 in range(0, nkc, kcstep):
                            n = min(kcstep, nkc - kc0)
                            op("pool", lambda h, kc0=kc0, n=n: h.dma_start(
                                out=dst[:, kc0:kc0 + n, dcol:dcol + ncol],
                                in_=src2d[kc0 * 128:(kc0 + n) * 128, scol:scol + ncol].rearrange("(kc kp) n -> kp kc n", kp=128)),
                               writes=[key], lane=lane)

                    def g_load(gi):
                        gname = "ga" if gi < 4 else "gb"
                        load_cast(wg[gi % 2], ("wg", gi % 2), "wg%d" % (gi % 2), w_in_d, 8, 128, (OFF[gname] + (gi % 4) * 128, 0))

                    def g_compute(gi):
                        k = gi % 2
                        fc = gi % 4
                        attT = qaT if gi < 4 else qbT
                        blk = "qa_blk" if gi < 4 else "qb_blk"
                        for tg in range(4):
                            pq = PQ[0][:, (tg % 2) * 512:(tg % 2 + 1) * 512]
                            pqk = ("PQ", 0, tg % 2)
                            for kc in range(8):
                                op("pe", lambda h, kc=kc, pq=pq, tg=tg: h.matmul(pq, lhsT=wg[k][:, kc, :], rhs=hT[:, kc, tg * 512:(tg + 1) * 512],
                                                                               start=(kc == 0), stop=(kc == 7)),
                                   reads=[("wg", k), ("hT", tg)], writes=[pqk])
                            tk_ = tg % 2
                            op("act", lambda h, pq=pq, tk_=tk_: h.activation(out=th[tk_][:], in_=pq, func=AF.Tanh, scale=0.5), reads=[pqk], writes=[("th", tk_)])
                            op("dve", lambda h, pq=pq, tk_=tk_: h.scalar_tensor_tensor(out=pa[tk_][:], in0=th[tk_][:], scalar=1.0, in1=pq, op0=ALU.add, op1=ALU.mult),
                               reads=[pqk, ("th", tk_)], writes=[("pa", tk_)])
                            akeys = [(blk, ii, x_) for ii in range(tg * 4, tg * 4 + 4) for x_ in ((0, 1) if gi < 4 else (fc // 2,))]
                            op("dve", lambda h, tk_=tk_, tg=tg: h.scalar_tensor_tensor(
                                out=attT[:, fc, tg * 512:(tg + 1) * 512], in0=pa[tk_][:], scalar=0.5, in1=attT[:, fc, tg * 512:(tg + 1) * 512],
                                op0=ALU.mult, op1=ALU.mult), reads=[("pa", tk_)] + akeys, writes=akeys)

                    def a_load(half, dc):
                        k = dc % 2
                        load_cast(wmc[k], ("wmc", k), "wmc%d" % k, w_m_d, 8, 128, (dc * 128, 0))
                        load_cast(wmc[k], ("wmc", k), "wmc%d" % k, w_m_d, 8, 128, (D + dc * 128, 128))
                        load_cast(wbr[k], ("wbr", k), "wbr%d" % k, w_ba_d, 4, 128, (dc * 128, 0))
                        load_cast(wbr[k], ("wbr", k), "wbr%d" % k, w_bb_d, 4, 128, (dc * 128, 128))

                    def a_compute(half, dc):
                        k = dc % 2
                        for tgl in range(2):
                            tg = half * 2 + tgl
                            tsl = slice(tg * 512, (tg + 1) * 512)
                            sl2 = slice(tgl * 512, (tgl + 1) * 512)
                            ma, mb_, ya = PQ[0][:, sl2], PQ[1][:, sl2], PQ[2][:, sl2]
                            yb = PR[:, :]
                            for kc in range(8):
                                op("pe", lambda h, kc=kc: h.matmul(ma, lhsT=wmc[k][:, kc, 0:128], rhs=hT[:, kc, tsl], start=(kc == 0), stop=(kc == 7)),
                                   reads=[("wmc", k), ("hT", tg)], writes=[("PQ", 0, tgl)])
                            for kc in range(8):
                                op("pe", lambda h, kc=kc: h.matmul(mb_, lhsT=wmc[k][:, kc, 128:256], rhs=hT[:, kc, tsl], start=(kc == 0), stop=(kc == 7)),
                                   reads=[("wmc", k), ("hT", tg)], writes=[("PQ", 1, tgl)])
                            ak = [("qa_blk", ii, r_) for ii in range(tg * 4, tg * 4 + 4) for r_ in range(2)]
                            bk = [("qb_blk", ii, hg_) for ii in range(tg * 4, tg * 4 + 4) for hg_ in range(2)]
                            for fc in range(4):
                                op("pe", lambda h, fc=fc: h.matmul(ya, lhsT=wbr[k][:, fc, 0:128], rhs=qaT[:, fc, tsl], start=(fc == 0), stop=(fc == 3)),
                                   reads=[("wbr", k)] + ak, writes=[("PQ", 2, tgl)])
                            for fc in range(4):
                                op("pe", lambda h, fc=fc: h.matmul(yb, lhsT=wbr[k][:, fc, 128:256], rhs=qbT[:, fc, tsl], start=(fc == 0), stop=(fc == 3)),
                                   reads=[("wbr", k)] + bk, writes=["PR"])
                            op("act", lambda h: h.activation(out=th[tgl][:], in_=ma, func=AF.Tanh, scale=0.5), reads=[("PQ", 0, tgl)], writes=[("th", tgl)])
                            op("act", lambda h: h.activation(out=th[2 + tgl][:], in_=mb_, func=AF.Tanh, scale=0.5), reads=[("PQ", 1, tgl)], writes=[("th", 2 + tgl)])
                            op("dve", lambda h: h.scalar_tensor_tensor(out=pa[tgl][:], in0=th[tgl][:], scalar=1.0, in1=ya, op0=ALU.add, op1=ALU.mult),
                               reads=[("th", tgl), ("PQ", 2, tgl)], writes=[("pa", tgl)])
                            op("dve", lambda h: h.scalar_tensor_tensor(out=pb[tgl][:], in0=th[2 + tgl][:], scalar=1.0, in1=yb, op0=ALU.add, op1=ALU.mult),
                               reads=[("th", 2 + tgl), "PR"], writes=[("pb", tgl)])
                            op("pool", lambda h: h.tensor_tensor(out=mT[:, dc, sl2], in0=pa[tgl][:], in1=pb[tgl][:], op=ALU.add),
                               reads=[("pa", tgl), ("pb", tgl)], writes=[("mT", tgl)])

                    def b_A(half, tt):
                        t = half * 8 + tt
                        xk = t % 2
                        o_ = PQ[0] if tt % 2 == 0 else PQ[2]
                        oi_ = 0 if tt % 2 == 0 else 2
                        tsl = slice(tt * 128, (tt + 1) * 128)
                        dma_in("sp", x3[xk][:], x_d[b, t * 128:(t + 1) * 128, :], ("x3", xk), "x3_%d" % xk)
                        op("pool", lambda h: h.dma_start(out=pbf[xk][:], in_=p_d[b, t * 128:(t + 1) * 128, :]), writes=[("pbf", xk)], lane="pbf%d" % xk)
                        for hf in range(2):
                            for dc in range(8):
                                op("pe", lambda h, hf=hf, dc=dc: h.matmul(o_[:, hf * 512:(hf + 1) * 512], lhsT=mT[:, dc, tsl], rhs=wo[:, dc, hf * 512:(hf + 1) * 512],
                                                                         start=(dc == 0), stop=(dc == 7)),
                                   reads=[("mT", tt // 4), "wo"], writes=[("PQ", oi_, hf)])

                    def b_B(half, tt):
                        t = half * 8 + tt
                        xk = t % 2
                        o_ = PQ[0] if tt % 2 == 0 else PQ[2]
                        oi_ = 0 if tt % 2 == 0 else 2
                        OK_ = [("PQ", oi_, 0), ("PQ", oi_, 1)]
                        op("act", lambda h: h.activation(out=junk3[:], in_=o_[:, :], func=AF.Square, accum_out=s3[:, 0:1]), reads=OK_, writes=["junk3", "s3a"])
                        op("dve", lambda h: h.tensor_scalar(out=s3[:, 1:2], in0=s3[:, 0:1], scalar1=1.0 / D, scalar2=4.0 * EPS, op0=ALU.mult, op1=ALU.add),
                           reads=["s3a"], writes=["s3b"])
                        op("pool", lambda h: h.tensor_tensor(out=s3[:, 2:3], in0=s3[:, 1:2], in1=negh[:], op=ALU.pow), reads=["s3b", "negh"], writes=["s3c"])
                        op("dve", lambda h: h.scalar_tensor_tensor(out=tA[:], in0=o_[:, :], scalar=s3[:, 2:3], in1=gpost[:], op0=ALU.mult, op1=ALU.mult),
                           reads=OK_ + ["s3c", "gpost"], writes=["tA"])
                        op("pool", lambda h: h.tensor_tensor(out=x1[xk][:], in0=tA[:], in1=x3[xk][:], op=ALU.add), reads=["tA", ("x3", xk)], writes=[("x1", xk)])
                        op("act", lambda h: h.copy(out=x1b[:], in_=x1[xk][:]), reads=[("x1", xk)], writes=["x1b"])
                        for kc in range(8):
                            op("pe", lambda h, kc=kc: h.transpose(out=PT3[:, kc * 128:(kc + 1) * 128], in_=x1b[:, kc * 128:(kc + 1) * 128], identity=ident_bf[:]),
                               reads=["x1b", "ident_bf"], writes=["PT3"])
                        op("act", lambda h: h.copy(out=x1T[xk][:], in_=PT3[:, :].rearrange("p (kc n) -> p kc n", kc=8)), reads=["PT3"], writes=[("x1T", xk)])
                        for pc in range(2):
                            op("pe", lambda h, pc=pc: h.transpose(out=PT3[:, pc * 128:(pc + 1) * 128], in_=pbf[xk][:, pc * 128:(pc + 1) * 128], identity=ident_bf[:]),
                               reads=[("pbf", xk), "ident_bf"], writes=["PT3"])
                        op("dve", lambda h: h.tensor_copy(out=pT[xk][:], in_=PT3[:, 0:256].rearrange("p (kc n) -> p kc n", kc=2)), reads=["PT3"], writes=[("pT", xk)])

                    def b_C(half, tt):
                        xk = (half * 8 + tt) % 2
                        for hf in range(2):
                            for dc in range(8):
                                op("pe", lambda h, hf=hf, dc=dc: h.matmul(PQ[1][:, hf * 512:(hf + 1) * 512], lhsT=x1T[xk][:, dc, :], rhs=wpg[:, dc, hf * 512:(hf + 1) * 512],
                                                                         start=(dc == 0), stop=(dc == 7)),
                                   reads=[("x1T", xk), "wpg"], writes=[("PQ", 1, hf)])

                    def b_D(half, tt):
                        t = half * 8 + tt
                        xk = t % 2
                        GK = [("PQ", 1, 0), ("PQ", 1, 1)]
                        op("act", lambda h: h.activation(out=gth[:], in_=PQ[1][:, :], func=AF.Tanh, scale=0.5), reads=GK, writes=["gth"])
                        for hf in range(2):
                            for pc in range(2):
                                op("pe", lambda h, hf=hf, pc=pc: h.matmul(PR[:, :], lhsT=pT[xk][:, pc, :], rhs=wp[:, pc, hf * 512:(hf + 1) * 512],
                                                                         start=(pc == 0), stop=(pc == 1)),
                                   reads=[("pT", xk), "wp"], writes=["PR"])
                            op("dve", lambda h, hf=hf: h.scalar_tensor_tensor(out=ge2[:, hf * 512:(hf + 1) * 512], in0=gth[:, hf * 512:(hf + 1) * 512], scalar=1.0,
                                                                              in1=PR[:, :], op0=ALU.add, op1=ALU.mult),
                               reads=["gth", "PR"], writes=[("ge2", hf)])
                        op("act", lambda h: h.activation(out=junk3[:], in_=ge2[:], func=AF.Square, accum_out=s3[:, 3:4]), reads=[("ge2", 0), ("ge2", 1)], writes=["junk3", "s3d"])
                        op("dve", lambda h: h.tensor_scalar(out=s3[:, 4:5], in0=s3[:, 3:4], scalar1=1.0 / D, scalar2=4.0 * EPS, op0=ALU.mult, op1=ALU.add),
                           reads=["s3d"], writes=["s3e"])
                        op("pool", lambda h: h.tensor_tensor(out=s3[:, 5:6], in0=s3[:, 4:5], in1=negh[:], op=ALU.pow), reads=["s3e", "negh"], writes=["s3f"])
                        op("dve", lambda h: h.scalar_tensor_tensor(out=fin[xk][:], in0=ge2[:], scalar=s3[:, 5:6], in1=gple[:], op0=ALU.mult, op1=ALU.mult),
                           reads=[("ge2", 0), ("ge2", 1), "s3f", "gple"], writes=[("fin", xk)])
                        op("pool", lambda h: h.tensor_tensor(out=fin[xk][:], in0=fin[xk][:], in1=x1[xk][:], op=ALU.add), reads=[("fin", xk), ("x1", xk)], writes=[("fin", xk)])
                        out_marks.append(op("sp", lambda h: h.dma_start(out=out_d[b, t * 128:(t + 1) * 128, :], in_=fin[xk][:]),
                                            reads=[("fin", xk)], lane="fin%d" % xk))

                    g_load(0)
                    g_load(1)
                    load_cast(wo, "wo", "wo", w_o_d, 8, D, None, kcstep=2)
                    load_cast(wpg, "wpg", "wpg", w_pg_d, 8, D, None, kcstep=2)
                    load_cast(wp, "wp", "wp", w_p_d, 2, D, None, kcstep=2)
                    for gi in range(8):
                        g_compute(gi)
                        if gi + 2 < 8:
                            g_load(gi + 2)
                    a_seq = [(hf_, dc_) for hf_ in range(2) for dc_ in range(8)]
                    a_load(*a_seq[0])
                    a_pos = [1]

                    def a_step(e):
                        if e + 1 < len(a_seq) and a_pos[0] == e + 1:
                            a_load(*a_seq[e + 1])
                            a_pos[0] += 1
                        a_compute(*a_seq[e])

                    for half in range(2):
                        for dc in range(8):
                            a_step(half * 8 + dc)
                        b_A(half, 0)
                        b_B(half, 0)
                        for tt in range(8):
                            if tt + 1 < 8:
                                b_A(half, tt + 1)
                            b_C(half, tt)
                            if tt + 1 < 8:
                                b_B(half, tt + 1)
                            b_D(half, tt)
        last = {}
        for (sk, v) in [m for m in out_marks if m]:
            last[sk] = max(last.get(sk, 0), v)
        T.wait_marks("sp", list(last.items()))
    T.close()
    build_program.stats = (T.nops, T.nwaits)
    return nc


def _consts():
    half = 8
    freqs = 500000.0 ** (-np.arange(half, dtype=np.float64) / half)
    pos = np.arange(S, dtype=np.float64)
    cosT = np.ones((128, S), np.float64)
    sinT = np.zeros((128, S), np.float64)
    perm = np.zeros((128, 128), np.float32)
    for hh in range(2):
        for d in range(16):
            f = hh * 64 + d
            ang = pos * freqs[d % 8]
            cosT[f] = np.cos(ang)
            sinT[f] = np.sin(ang)
            if d < 8:
                perm[f + 8, f] = -1.0
            else:
                perm[f - 8, f] = 1.0
    idx = np.arange(128)
    ident = np.eye(128, dtype=np.float32)
    trineg = np.where(idx[:, None] <= idx[None, :], 0.0, MASKNEG).astype(np.float32)
    causneg = np.where(idx[None, :] <= idx[:, None], 0.0, -1.0e9).astype(np.float32)
    U = (idx[:, None] <= idx[None, :]).astype(np.float32)
    sel0 = np.zeros((128, 128), np.float32)
    sel0[0, :] = 1.0
    ones = np.ones((128, 128), np.float32)
    cst = np.stack([ident, perm, trineg, causneg, U, sel0, ones], axis=1).astype(np.float32)
    pw = list(0.5 ** np.arange(1, NIT + 1, dtype=np.float64))
    pw2 = np.tile(np.array(pw + [pw[-1]])[None, :], (128, 1)).astype(np.float32)
    return cosT.astype(np.float32), sinT.astype(np.float32), np.ascontiguousarray(cst), pw2


def _run(inputs, nseq, seq_ids_per_core):
    x = np.asarray(inputs["x"], np.float32)
    p = np.asarray(inputs["p"], np.float32)[0]
    cosT, sinT, cst, pw2 = _consts()
    rep = lambda v: np.ascontiguousarray(np.broadcast_to(np.asarray(v, np.float32).reshape(1, -1), (128, np.asarray(v).size)))
    common = {
        "w_in": np.ascontiguousarray(np.asarray(inputs["w_in"], np.float32)[0]),
        "w_ba": np.ascontiguousarray(np.asarray(inputs["w_branch_a"], np.float32)[0]),
        "w_bb": np.ascontiguousarray(np.asarray(inputs["w_branch_b"], np.float32)[0]),
        "w_m": np.ascontiguousarray(np.asarray(inputs["w_merge"], np.float32)[0]),
        "w_o": np.ascontiguousarray(np.asarray(inputs["w_out"], np.float32)[0]),
        "w_p": np.ascontiguousarray(np.asarray(inputs["w_ple"], np.float32)[0]),
        "w_pg": np.ascontiguousarray(np.asarray(inputs["w_ple_gate"], np.float32)[0]),
        "gpre": rep(inputs["g_pre"][0]), "gpost": rep(inputs["g_post"][0]), "gple": rep(inputs["g_ple"][0]),
        "bfb": rep(inputs["b_forget"][0]),
        "cosT": cosT, "sinT": sinT, "cst": cst, "pw2": pw2,
    }
    nc = build_program(nseq)
    in_maps = []
    for c in range(NCORES):
        ids = seq_ids_per_core[c]
        m = dict(common)
        m["x"] = np.ascontiguousarray(x[ids])
        m["p"] = np.ascontiguousarray(p[ids])
        in_maps.append(m)
    res = run_bass_kernel_spmd(nc, in_maps, core_ids=list(range(NCORES)))
    return [r["out"] for r in res.results]


def kernel(**inputs):
    B = np.asarray(inputs["x"]).shape[0]
    nseq = B // NCORES
    ids = [list(range(c * nseq, (c + 1) * nseq)) for c in range(NCORES)]
    outs = _run(inputs, nseq, ids)
    return np.concatenate(outs, axis=0).astype(np.float32)
```

```python
import contextlib
import os
import numpy as np
import concourse.bass as bass
import concourse.mybir as mybir
from concourse.bass_utils import run_bass_kernel_spmd

F32 = mybir.dt.float32
BF16 = mybir.dt.bfloat16
ALU = mybir.AluOpType
AF = mybir.ActivationFunctionType
AX = mybir.AxisListType

NCORES = 8
S = 2048
D = 1024
NT = 16
DIN = 3792
OFF = dict(qa=0, ka=512, va=576, ga=640, qi=1152, ki=1664, wi=1728, qb=1736, kb=2248, vb=2760, fb=3272, gb=3280)
EPS = 1e-6
NIT = 14
STAGE = float(os.environ.get('MK_STAGE', '9'))
ATTACH = int(os.environ.get('MK_ATTACH', '0'))
TOPK = 256
MASKNEG = -30000.0
IDX_SCALE = (8 ** -0.5) * (64 ** -0.5)

ENGS = ("pe", "act", "dve", "pool", "sp")


class Trk:
    def __init__(self, nc):
        self.nc = nc
        self.h = {"pe": nc.tensor, "act": nc.scalar, "dve": nc.vector, "pool": nc.gpsimd, "sp": nc.sync}
        self.stack = contextlib.ExitStack()
        self.sems = {e: self.stack.enter_context(nc.semaphore("s_" + e)) for e in ENGS}
        self.cnt = {e: 0 for e in ENGS}
        self.lanecnt = {}
        self.clock = {e: {} for e in ENGS}
        self.snap = {}
        self.res = {}
        self.nwaits = 0
        self.nops = 0
        self.enabled = True
        self.pending = {e: None for e in ENGS}

    def _sem(self, sk):
        s = self.sems.get(sk)
        if s is None:
            s = self.stack.enter_context(self.nc.semaphore("l_%d" % len(self.sems)))
            self.sems[sk] = s
        return s

    def op(self, eng, fn, reads=(), writes=(), lane=None):
        if not self.enabled:
            return None
        deps = []
        for r in reads:
            st = self.res.get(r)
            if st and st[0]:
                deps.append(st[0])
        for w in writes:
            st = self.res.get(w)
            if st:
                if st[0]:
                    deps.append(st[0])
                deps.extend(st[1])
        clk = self.clock[eng]
        waits = {}
        if self.pending[eng]:
            deps.extend(self.pending[eng].items())
            self.pending[eng] = None
        for (sk, v) in deps:
            if sk == eng and eng == "pe":
                continue
            if clk.get(sk, 0) >= v:
                continue
            if waits.get(sk, 0) < v:
                waits[sk] = v
        if waits:
            clk = dict(clk)
            for sk, v in waits.items():
                sn = self.snap.get((sk, v))
                if sn:
                    for k2, v2 in sn.items():
                        if clk.get(k2, 0) < v2:
                            clk[k2] = v2
                if clk.get(sk, 0) < v:
                    clk[sk] = v
            self.clock[eng] = clk
        if lane is None:
            self.cnt[eng] += 1
            me = (eng, self.cnt[eng])
        else:
            sk = ("lane", lane)
            self.lanecnt[lane] = self.lanecnt.get(lane, 0) + 16
            me = (sk, self.lanecnt[lane])
        sn = dict(clk)
        sn[me[0]] = me[1]
        self.snap[me] = sn
        for r in reads:
            st = self.res.setdefault(r, [None, []])
            st[1].append(me)
        for w in writes:
            self.res[w] = [me, []]
        h = self.h[eng]
        wl = list(waits.items())
        self.nwaits += len(wl)
        self.nops += 1
        if wl and ATTACH:
            for sk, v in wl[:-1]:
                h.wait_ge(self._sem(sk), v)
            ins = fn(h)
            sk, v = wl[-1]
            ins = ins._wait_ge(self._sem(sk), v)
        else:
            for sk, v in wl:
                h.wait_ge(self._sem(sk), v)
            ins = fn(h)
        ins.then_inc(self._sem(me[0]), 16 if lane is not None else 1)
        return me

    def fence(self):
        marks = {e: c for e, c in self.cnt.items() if c}
        for ln, c in self.lanecnt.items():
            marks[("lane", ln)] = c
        for e in ENGS:
            self.pending[e] = dict(marks)

    def wait_marks(self, eng, marks):
        h = self.h[eng]
        for sk, v in marks:
            h.wait_ge(self._sem(sk), v)

    def close(self):
        self.stack.close()


def build_program(nseq, dbg=False):
    nc = bass.Bass("TRN2", target_bir_lowering=False)

    def din(name, shape):
        return nc.dram_tensor(name, shape, F32, kind="ExternalInput").ap()

    x_d = din("x", [nseq, S, D])
    p_d = din("p", [nseq, S, 256])
    w_in_d = din("w_in", [D, DIN])
    w_ba_d = din("w_ba", [512, D])
    w_bb_d = din("w_bb", [512, D])
    w_m_d = din("w_m", [D, 2 * D])
    w_o_d = din("w_o", [D, D])
    w_p_d = din("w_p", [256, D])
    w_pg_d = din("w_pg", [D, D])
    gpre_d = din("gpre", [128, D])
    gpost_d = din("gpost", [128, D])
    gple_d = din("gple", [128, D])
    bfb_d = din("bfb", [128, 8])
    cos_d = din("cosT", [128, S])
    sin_d = din("sinT", [128, S])
    cst_d = din("cst", [128, 7, 128])
    pw2_d = din("pw2", [128, NIT + 1])
    out_d = nc.dram_tensor("out", [nseq, S, D], F32, kind="ExternalOutput").ap()
    dbg_d = {}

    T = Trk(nc)
    op = T.op
    uid = [0]

    def dma_in(eng, out_ap, in_ap, key, lane):
        return op(eng, lambda h: h.dma_start(out=out_ap, in_=in_ap), writes=[key], lane=lane)

    with contextlib.ExitStack() as g:
        def sb(name, shape, dt, es=g):
            uid[0] += 1
            return es.enter_context(nc.sbuf_tensor("%s_u%d" % (name, uid[0]), shape, dt))

        def ps(name, shape, dt, es=g):
            uid[0] += 1
            return es.enter_context(nc.psum_tensor("%s_u%d" % (name, uid[0]), shape, dt))

        cst = sb("cst", [128, 7, 128], F32)
        ident_bf = sb("ident_bf", [128, 128], BF16)
        trineg_bf = sb("trineg_bf", [128, 128], BF16)
        irep_bf = sb("irep_bf", [128, 512], BF16)
        ones_bf = sb("ones_bf", [128, 64], BF16)
        gpre = sb("gpre_s", [128, D], F32)
        gpost = sb("gpost_s", [128, D], F32)
        gple = sb("gple_s", [128, D], F32)
        bfb = sb("bfb_s", [128, 8], F32)
        pw2 = sb("pw2_s", [128, NIT + 1], F32)
        negh = sb("negh", [128, 1], F32)
        taum = sb("taum", [128, 1], F32)
        ident_f = cst[:, 0, :]
        perm_f = cst[:, 1, :]
        causneg_f = cst[:, 3, :]
        U_f = cst[:, 4, :]
        sel0_f = cst[:, 5, :]
        ones_f = cst[:, 6, :]

        dma_in("sp", cst[:], cst_d[:, :, :], "cst", "c0")
        dma_in("sp", gpre[:], gpre_d[:, :], "gpre", "c1")
        dma_in("sp", gpost[:], gpost_d[:, :], "gpost", "c2")
        dma_in("sp", gple[:], gple_d[:, :], "gple", "c3")
        dma_in("sp", bfb[:], bfb_d[:, :], "bfb", "c4")
        dma_in("sp", pw2[:], pw2_d[:, :], "pw2", "c5")
        dma_in("pool", ident_bf[:], cst_d[:, 0, :], "ident_bf", "c6")
        dma_in("pool", trineg_bf[:], cst_d[:, 2, :], "trineg_bf", "c7")
        for k in range(4):
            op("pool", lambda h, k=k: h.dma_start(out=irep_bf[:, k * 128:(k + 1) * 128], in_=cst_d[:, 0, :]),
               writes=[("irep", k)], lane="c8")
        IREP = [("irep", k) for k in range(4)]
        op("pool", lambda h: h.memset(ones_bf[:], 1.0), writes=["ones_bf"])
        op("pool", lambda h: h.memset(negh[:], -0.5), writes=["negh"])
        op("pool", lambda h: h.memset(taum[:], -1.0e8), writes=["taum"])

        out_marks = []

        for b in range(nseq):
            T.fence()
            with contextlib.ExitStack() as sq:
                hT = sb("hT", [128, 8, S], BF16, sq)
                qaT = sb("qaT", [128, 4, S], BF16, sq)
                qbT = sb("qbT", [128, 4, S], BF16, sq)
                with contextlib.ExitStack() as p12:
                    qiT = sb("qiT", [128, 4, S], BF16, p12)
                    kaT = sb("kaT", [128, S], BF16, p12)
                    kiT = sb("kiT", [128, S], BF16, p12)
                    kbT = sb("kbT", [128, 4, S], BF16, p12)
                    va = sb("va", [128, NT, 128], BF16, p12)
                    vb = sb("vb", [128, NT, 512], BF16, p12)
                    wi = sb("wi", [128, NT, 8], F32, p12)
                    fbs = sb("fbs", [128, NT, 8], F32, p12)
                    csb = sb("csb", [128, NT, 8], F32, p12)
                    carry = sb("carry", [128, NT, 8], F32, p12)
                    with contextlib.ExitStack() as p1:
                        T.enabled = STAGE >= 1
                        cosT = sb("cosT_s", [128, S], F32, p1)
                        sinT = sb("sinT_s", [128, S], F32, p1)
                        xt = [sb("xt%d" % k, [128, D], F32, p1) for k in range(2)]
                        junk = sb("junk1", [128, D], BF16, p1)
                        hb = [sb("hb%d" % k, [128, D], BF16, p1) for k in range(2)]
                        wbuf = [sb("wbuf%d" % k, [128, 8, 512], BF16, p1) for k in range(2)]
                        xs = [sb("xs%d" % k, [128, 512], F32, p1) for k in range(2)]
                        t1 = [sb("t1_0", [128, 512], F32, p1)] * 2
                        t2 = [sb("t2_0", [128, 512], F32, p1)] * 2
                        st = sb("st1", [128, 4], F32, p1)
                        wsm = sb("wsm", [128, 8, 80], BF16, p1)
                        sm16 = sb("sm16", [128, 16], F32, p1)
                        wraw = sb("wraw", [128, 8, 208], BF16, p1)
                        PA = [ps("PA%d" % k, [128, 512], F32, p1) for k in range(4)]
                        PB = [ps("PB%d" % k, [128, 512], F32, p1) for k in range(2)]
                        PTB = ps("PTB1", [128, 1024], BF16, p1)
                        PC = ps("PC1", [128, 512], F32, p1)

                        dma_in("sp", cosT[:], cos_d[:, :], "cosT", "cos")
                        dma_in("sp", sinT[:], sin_d[:, :], "sinT", "sin")
                        op("pool", lambda h: h.memset(va[:, :, 64:128], 1.0), writes=["va_ones"])

                        wl_state = {"n": 0}

                        def load_w(segments):
                            k = wl_state["n"] % 2
                            wl_state["n"] += 1
                            c = 0
                            for (c0, ncol) in segments:
                                for kc0 in range(0, 8, 4):
                                    op("pool", lambda h, k=k, c=c, c0=c0, ncol=ncol, kc0=kc0: h.dma_start(
                                        out=wbuf[k][:, kc0:kc0 + 4, c:c + ncol],
                                        in_=w_in_d[kc0 * 128:(kc0 + 4) * 128, c0:c0 + ncol].rearrange("(kc kp) n -> kp kc n", kp=128)),
                                       writes=[("wbuf", k)], lane="wbuf%d" % k)
                                c += ncol
                            return k

                        for t in range(NT):
                            k = t % 2
                            dma_in("sp", xt[k][:], x_d[b, t * 128:(t + 1) * 128, :], ("xt", k), "xt%d" % k)
                            op("act", lambda h, k=k: h.activation(out=junk[:], in_=xt[k][:], func=AF.Square, accum_out=st[:, 0:1]),
                               reads=[("xt", k)], writes=["junk1", "st0"])
                            op("dve", lambda h: h.tensor_scalar(out=st[:, 1:2], in0=st[:, 0:1], scalar1=1.0 / D, scalar2=EPS,
                                                                op0=ALU.mult, op1=ALU.add), reads=["st0"], writes=["st1"])
                            op("pool", lambda h: h.tensor_tensor(out=st[:, 2:3], in0=st[:, 1:2], in1=negh[:], op=ALU.pow),
                               reads=["st1", "negh"], writes=["st2"])
                            op("dve", lambda h, k=k: h.scalar_tensor_tensor(out=hb[k][:], in0=xt[k][:], scalar=st[:, 2:3], in1=gpre[:],
                                                                            op0=ALU.mult, op1=ALU.mult),
                               reads=[("xt", k), "st2", "gpre"], writes=[("hb", k)])
                            for kc in range(8):
                                op("pe", lambda h, k=k, kc=kc: h.transpose(out=PTB[:, kc * 128:(kc + 1) * 128], in_=hb[k][:, kc * 128:(kc + 1) * 128],
                                                                           identity=ident_bf[:]),
                                   reads=[("hb", k), "ident_bf"], writes=["PTB1"])
                            op("act", lambda h, t=t: h.copy(out=hT[:, :, t * 128:(t + 1) * 128], in_=PTB[:, :].rearrange("p (kc n) -> p kc n", kc=8)),
                               reads=["PTB1"], writes=[("hT", t // 4)])

                        T.enabled = STAGE >= 1.2
                        pa_i = [0]

                        def proj_fm(k, cofs, tg):
                            slot = pa_i[0] % 4
                            pa_i[0] += 1
                            for kc in range(8):
                                op("pe", lambda h, kc=kc, slot=slot: h.matmul(PA[slot][:, :], lhsT=wbuf[k][:, kc, cofs:cofs + 128],
                                                                             rhs=hT[:, kc, tg * 512:(tg + 1) * 512], start=(kc == 0), stop=(kc == 7)),
                                   reads=[("wbuf", k), ("hT", tg)], writes=[("PA", slot)])
                            return slot

                        rp_i = [0]

                        def rope_evac(slot, dst_ap, dst_key, tg):
                            r = rp_i[0] % 2
                            rp_i[0] += 1
                            tsl = slice(tg * 512, (tg + 1) * 512)
                            op("act", lambda h: h.copy(out=xs[r][:], in_=PA[slot][:, :]), reads=[("PA", slot)], writes=[("xs", r)])
                            op("pe", lambda h: h.matmul(PB[r][:, :], lhsT=perm_f, rhs=xs[r][:], start=True, stop=True),
                               reads=[("xs", r), "cst"], writes=[("PB", r)])
                            op("dve", lambda h: h.tensor_tensor(out=t1[r][:], in0=PB[r][:, :], in1=sinT[:, tsl], op=ALU.mult),
                               reads=[("PB", r), "sinT"], writes=["t1"])
                            op("pool", lambda h: h.tensor_tensor(out=t2[r][:], in0=xs[r][:], in1=cosT[:, tsl], op=ALU.mult),
                               reads=[("xs", r), "cosT"], writes=["t2"])
                            op("dve", lambda h: h.tensor_tensor(out=dst_ap, in0=t1[r][:], in1=t2[r][:], op=ALU.add),
                               reads=["t1", "t2"], writes=[dst_key])

                        for (c0, ncol, dc0) in ((OFF["va"], 64, 0), (OFF["wi"] - 64, 72, 64), (OFF["fb"] - 64, 72, 136)):
                            for kc0 in range(0, 8, 4):
                                op("pool", lambda h, c0=c0, ncol=ncol, dc0=dc0, kc0=kc0: h.dma_start(
                                    out=wraw[:, kc0:kc0 + 4, dc0:dc0 + ncol],
                                    in_=w_in_d[kc0 * 128:(kc0 + 4) * 128, c0:c0 + ncol].rearrange("(kc kp) n -> kp kc n", kp=128)),
                                   writes=["wraw"], lane="wraw")
                        op("dve", lambda h: h.tensor_copy(out=wsm[:, :, 0:64], in_=wraw[:, :, 0:64]), reads=["wraw"], writes=["wsm"])
                        op("dve", lambda h: h.tensor_copy(out=wsm[:, :, 64:72], in_=wraw[:, :, 128:136]), reads=["wraw"], writes=["wsm"])
                        op("dve", lambda h: h.tensor_copy(out=wsm[:, :, 72:80], in_=wraw[:, :, 200:208]), reads=["wraw"], writes=["wsm"])

                        def g_rope4(nm, dst):
                            def f(k):
                                for c in range(4):
                                    for tg in range(4):
                                        slot = proj_fm(k, c * 128, tg)
                                        rope_evac(slot, dst[:, c, tg * 512:(tg + 1) * 512], (nm + "T", c, tg), tg)
                            return f

                        def g_kk(k):
                            for c, (nm, dst) in enumerate((("ka", kaT), ("ki", kiT))):
                                for tg in range(4):
                                    slot = proj_fm(k, c * 128, tg)
                                    rope_evac(slot, dst[:, tg * 512:(tg + 1) * 512], (nm + "T", tg), tg)

                        def g_plain4(nm, dst):
                            def f(k):
                                for c in range(4):
                                    for tg in range(4):
                                        slot = proj_fm(k, c * 128, tg)
                                        op("act", lambda h, slot=slot, c=c, tg=tg: h.copy(out=dst[:, c, tg * 512:(tg + 1) * 512], in_=PA[slot][:, :]),
                                           reads=[("PA", slot)], writes=[(nm + "T", c, tg)])
                            return f

                        def g_tok(k):
                            for t in range(NT):
                                slot = pa_i[0] % 4
                                pa_i[0] += 1
                                for kc in range(8):
                                    op("pe", lambda h, kc=kc, slot=slot, t=t: h.matmul(PA[slot][:, :], lhsT=hT[:, kc, t * 128:(t + 1) * 128],
                                                                                      rhs=wbuf[k][:, kc, 0:512], start=(kc == 0), stop=(kc == 7)),
                                       reads=[("wbuf", k), ("hT", t // 4)], writes=[("PA", slot)])
                                op("act", lambda h, slot=slot, t=t: h.copy(out=vb[:, t, :], in_=PA[slot][:, :]), reads=[("PA", slot)], writes=[("vb", t)])
                                for kc in range(8):
                                    op("pe", lambda h, kc=kc, t=t: h.matmul(PC[:, 0:80], lhsT=hT[:, kc, t * 128:(t + 1) * 128],
                                                                            rhs=wsm[:, kc, 0:80], start=(kc == 0), stop=(kc == 7)),
                                       reads=["wsm", ("hT", t // 4)], writes=["PC1"])
                                op("act", lambda h, t=t: h.copy(out=va[:, t, 0:64], in_=PC[:, 0:64]), reads=["PC1"], writes=[("va", t)])
                                op("act", lambda h: h.copy(out=sm16[:], in_=PC[:, 64:80]), reads=["PC1"], writes=["sm16"])
                                op("act", lambda h, t=t: h.mul(out=wi[:, t, :], in_=sm16[:, 0:8], mul=IDX_SCALE),
                                   reads=["sm16"], writes=[("wi", t)])
                                op("pool", lambda h, t=t: h.tensor_tensor(out=fbs[:, t, :], in0=sm16[:, 8:16], in1=bfb[:], op=ALU.add),
                                   reads=["sm16", "bfb"], writes=["fbs"])

                        groups = [
                            ([(OFF["qa"], 512)], g_rope4("qa", qaT)),
                            ([(OFF["qi"], 512)], g_rope4("qi", qiT)),
                            ([(OFF["ka"], 64), (OFF["ka"], 64), (OFF["ki"], 64), (OFF["ki"], 64)], g_kk),
                            ([(OFF["qb"], 512)], g_plain4("qb", qbT)),
                            ([(OFF["kb"], 512)], g_plain4("kb", kbT)),
                            ([(OFF["vb"], 512)], g_tok),
                        ]
                        kcur = load_w(groups[0][0])
                        for gi_, (segs_, fn_) in enumerate(groups):
                            knext = load_w(groups[gi_ + 1][0]) if gi_ + 1 < len(groups) else None
                            fn_(kcur)
                            kcur = knext
                    T.fence()
                    with contextlib.ExitStack() as p2:
                        T.enabled = STAGE >= 2
                        score = [sb("score%d" % k, [128, S], F32, p2) for k in range(2)]
                        tmp = [sb("tmp%d" % k, [128, 512], F32, p2) for k in range(2)]
                        junk2 = sb("junk2", [128, S], BF16, p2)
                        mneg = [sb("mneg%d" % k, [128, S], BF16, p2) for k in range(3)]
                        ptd = [sb("ptd%d" % k, [128, 512], BF16, p2) for k in range(3)]
                        ptf = [sb("ptf%d" % k, [128, 128], BF16, p2) for k in range(3)]
                        dend = sb("dend", [128, 512], F32, p2)
                        denf = sb("denf", [128, 512], F32, p2)
                        sst = [sb("sst%d" % k, [128, 8 + NIT + 1], F32, p2) for k in range(2)]
                        crefb = sb("crefb", [128, NT, 8], F32, p2)
                        biasT = sb("biasT", [128, NT, NT, 8], F32, p2)
                        IDX = ps("IDX", [128, 512], F32, p2)
                        DS = [ps("DS%d" % k, [128, 512], F32, p2) for k in range(2)]
                        DACC = ps("DACC", [128, 512], F32, p2)
                        FS = [ps("FS%d" % k, [128, 512], F32, p2) for k in range(3)]
                        FACC = ps("FACC", [128, 512], F32, p2)
                        cnt = {"idx": 0, "ptd": 0, "fs": 0}
                        fl = fbs[:].rearrange("p t h -> p (t h)")
                        cl = csb[:].rearrange("p t h -> p (t h)")
                        op("act", lambda h: h.activation(out=fl, in_=fl, func=AF.Exp, scale=-1.0), reads=["fbs"], writes=["fbs"])
                        op("act", lambda h: h.activation(out=fl, in_=fl, func=AF.Ln, bias=1.0, scale=1.0), reads=["fbs"], writes=["fbs"])
                        op("dve", lambda h: h.tensor_scalar(out=fl, in0=fl, scalar1=-1.0, scalar2=None, op0=ALU.mult), reads=["fbs"], writes=["fbs"])
                        op("pe", lambda h: h.matmul(IDX[:, 0:128], lhsT=U_f, rhs=fl, start=True, stop=True), reads=["fbs", "cst"], writes=[("IDX", 0)])
                        op("pe", lambda h: h.matmul(FS[0][:, 0:128], lhsT=ones_f, rhs=fl, start=True, stop=True), reads=["fbs", "cst"], writes=[("FS", 0)])
                        op("dve", lambda h: h.memset(carry[:, 0, :], 0.0), writes=["carry"])
                        for t in range(1, NT):
                            op("dve", lambda h, t=t: h.tensor_tensor(out=carry[:, t, :], in0=carry[:, t - 1, :], in1=FS[0][:, (t - 1) * 8:t * 8], op=ALU.add),
                               reads=[("FS", 0), "carry"], writes=["carry"])
                        op("dve", lambda h: h.tensor_tensor(out=cl, in0=IDX[:, 0:128], in1=carry[:].rearrange("p t h -> p (t h)"), op=ALU.add),
                           reads=[("IDX", 0), "carry"], writes=["csb"])
                        op("pe", lambda h: h.matmul(IDX[:, 0:128], lhsT=sel0_f, rhs=cl, start=True, stop=True), reads=["csb", "cst"], writes=[("IDX", 0)])
                        op("act", lambda h: h.copy(out=crefb[:].rearrange("p t h -> p (t h)"), in_=IDX[:, 0:128]), reads=[("IDX", 0)], writes=["crefb"])
                        for i in range(NT):
                            op("dve", lambda h, i=i: h.tensor_tensor(out=biasT[:, i, 0:i + 1, :],
                                                                     in0=crefb[:, i, :].unsqueeze(1).broadcast_to([128, i + 1, 8]),
                                                                     in1=csb[:, 0:i + 1, :], op=ALU.subtract),
                               reads=["crefb", "csb"], writes=["biasT"])

                        def dsa_index(i):
                            items = []
                            sp_ = i % 2
                            L = 128 * (i + 1)
                            sc = score[sp_]
                            nch = (L + 511) // 512
                            s_ = sst[sp_]
                            SK = ("sst", sp_)
                            for hd in range(8):
                                c, r = hd // 2, hd % 2
                                for kk in range(nch):
                                    def chunk(hd=hd, c=c, r=r, kk=kk):
                                        w = min(512, L - 512 * kk)
                                        sl = cnt["idx"] % 2
                                        cnt["idx"] += 1
                                        op("pe", lambda h: h.matmul(
                                            IDX[:, 0:w], lhsT=qiT[64 * r:64 * r + 64, c, i * 128:(i + 1) * 128],
                                            rhs=kiT[64 * r:64 * r + 64, kk * 512:kk * 512 + w], start=True, stop=True),
                                           reads=[("qiT", c, i // 4), ("kiT", kk)], writes=[("IDX", 0)])
                                        op("act", lambda h: h.activation(out=tmp[sl][:, 0:w], in_=IDX[:, 0:w], func=AF.Relu),
                                           reads=[("IDX", 0)], writes=[("tmp", sl)])
                                        if hd == 0:
                                            op("dve", lambda h: h.tensor_scalar(
                                                out=sc[:, kk * 512:kk * 512 + w], in0=tmp[sl][:, 0:w], scalar1=wi[:, i, 0:1], scalar2=None, op0=ALU.mult),
                                               reads=[("tmp", sl), ("wi", i)], writes=[("score", sp_)])
                                        else:
                                            op("dve", lambda h: h.scalar_tensor_tensor(
                                                out=sc[:, kk * 512:kk * 512 + w], in0=tmp[sl][:, 0:w], scalar=wi[:, i, hd:hd + 1],
                                                in1=sc[:, kk * 512:kk * 512 + w], op0=ALU.mult, op1=ALU.add),
                                               reads=[("tmp", sl), ("wi", i), ("score", sp_)], writes=[("score", sp_)])
                                    items.append(chunk)

                            def prep():
                                op("dve", lambda h: h.tensor_tensor(out=sc[:, i * 128:L], in0=sc[:, i * 128:L], in1=causneg_f, op=ALU.add),
                                   reads=[("score", sp_), "cst"], writes=[("score", sp_)])
                                if i >= 2:
                                    op("dve", lambda h: h.tensor_reduce(out=s_[:, 0:1], in_=sc[:, 0:128 * i], axis=AX.X, op=ALU.min),
                                       reads=[("score", sp_)], writes=[SK])
                                    op("dve", lambda h: h.tensor_reduce(out=s_[:, 1:2], in_=sc[:, 0:L], axis=AX.X, op=ALU.max),
                                       reads=[("score", sp_)], writes=[SK])
                                    op("dve", lambda h: h.tensor_tensor(out=s_[:, 2:3], in0=s_[:, 1:2], in1=s_[:, 0:1], op=ALU.subtract),
                                       reads=[SK], writes=[SK])
                                    op("dve", lambda h: h.tensor_scalar(out=s_[:, 8:9 + NIT], in0=pw2[:], scalar1=s_[:, 2:3], scalar2=None, op0=ALU.mult),
                                       reads=[SK, "pw2"], writes=[SK])
                                    op("dve", lambda h: h.tensor_tensor(out=s_[:, 3:4], in0=s_[:, 0:1], in1=s_[:, 8:9], op=ALU.add),
                                       reads=[SK], writes=[SK])
                            items.append(prep)
                            if i >= 2:
                                for it in range(NIT):
                                    def iteration(it=it):
                                        op("dve", lambda h: h.tensor_scalar(out=junk2[:, 0:L], in0=sc[:, 0:L], scalar1=s_[:, 3:4], scalar2=None,
                                                                            op0=ALU.is_ge, op1=ALU.add, accum_out=s_[:, 4:5]),
                                           reads=[SK, ("score", sp_)], writes=[SK, "junk2"])
                                        op("dve", lambda h: h.scalar_tensor_tensor(out=s_[:, 5:6], in0=s_[:, 4:5], scalar=TOPK - 0.5, in1=s_[:, 8 + it:9 + it],
                                                                                   op0=ALU.is_ge, op1=ALU.mult),
                                           reads=[SK], writes=[SK])
                                        op("dve", lambda h: h.scalar_tensor_tensor(out=s_[:, 3:4], in0=s_[:, 5:6], scalar=s_[:, 9 + it:10 + it], in1=s_[:, 3:4],
                                                                                   op0=ALU.subtract, op1=ALU.add),
                                           reads=[SK], writes=[SK])
                                    items.append(iteration)

                            def fin():
                                if i >= 2:
                                    tau, tk = s_[:, 3:4], [SK]
                                else:
                                    tau, tk = taum[:], ["taum"]
                                op("dve", lambda h: h.tensor_scalar(out=mneg[i % 3][:, 0:L], in0=sc[:, 0:L], scalar1=tau, scalar2=MASKNEG,
                                                                    op0=ALU.is_lt, op1=ALU.mult),
                                   reads=tk + [("score", sp_)], writes=[("mneg", i % 3)])
                            items.append(fin)
                            return items

                        class Pipe:
                            def __init__(self, skew, batch):
                                self.q, self.skew, self.batch, self.pf = [], skew, batch, []

                            def push(self, front, back):
                                self.pf.append((front, back))
                                if len(self.pf) >= self.batch:
                                    self._go()

                            def _go(self):
                                for f, _ in self.pf:
                                    f()
                                self.q.extend(bk for _, bk in self.pf)
                                self.pf = []
                                while len(self.q) > self.skew:
                                    self.q.pop(0)()

                            def flush(self):
                                if self.pf:
                                    self._go()
                                while self.q:
                                    self.q.pop(0)()

                        def dsa_units(i):
                            units = []
                            for half in range(2):
                                for j in range(i + 1):
                                    pk = cnt["ptd"] % 3
                                    ds = cnt["ptd"] % 2
                                    cnt["ptd"] += 1

                                    def front(j=j, half=half, ds=ds):
                                        D_ = DS[ds]
                                        op("pe", lambda h: h.matmul(D_[:, :], lhsT=mneg[i % 3][:, j * 128:(j + 1) * 128], rhs=irep_bf[:],
                                                                    start=True, stop=False),
                                           reads=[("mneg", i % 3)] + IREP, writes=[("DS", ds)])
                                        for hh in range(4):
                                            c, r = hh, half
                                            op("pe", lambda h, hh=hh, c=c, r=r: h.matmul(
                                                D_[:, hh * 128:(hh + 1) * 128], lhsT=kaT[64 * r:64 * r + 64, j * 128:(j + 1) * 128],
                                                rhs=qaT[64 * r:64 * r + 64, c, i * 128:(i + 1) * 128], start=False, stop=(hh == 3)),
                                               reads=[("kaT", j // 4), ("qaT", c, i // 4), ("qa_blk", i, r)], writes=[("DS", ds)])

                                    def back(j=j, half=half, pk=pk, ds=ds):
                                        D_ = DS[ds]
                                        op("act", lambda h: h.activation(out=ptd[pk][:], in_=D_[:, :], func=AF.Exp, scale=0.125),
                                           reads=[("DS", ds)], writes=[("ptd", pk)])
                                        op("pe", lambda h: h.matmul(DACC[:, :], lhsT=va[:, j, :], rhs=ptd[pk][:],
                                                                    start=(j == 0), stop=(j == i)),
                                           reads=[("ptd", pk), ("va", j), "va_ones"], writes=["DACC"])
                                    units.append((front, back))

                                def epi(half=half):
                                    r = half
                                    op("act", lambda h: h.activation(out=dend[64:128, :], in_=DACC[64:128, :], func=AF.Ln), reads=["DACC"], writes=["dend"])
                                    op("act", lambda h: h.activation(out=dend[64:128, :], in_=dend[64:128, :], func=AF.Exp, scale=-1.0), reads=["dend"], writes=["dend"])
                                    op("dve", lambda h: h.tensor_tensor(
                                        out=qaT[64 * r:64 * r + 64, :, i * 128:(i + 1) * 128],
                                        in0=DACC[0:64, :].rearrange("p (c q) -> p c q", c=4),
                                        in1=dend[64:128, :].rearrange("p (c q) -> p c q", c=4), op=ALU.mult),
                                       reads=["DACC", "dend"], writes=[("qa_blk", i, r)])
                                units.append((lambda: None, epi))
                            return units

                        def fox_units(i):
                            units = []
                            for hg in range(2):
                                for hh in range(4):
                                    hd = hg * 4 + hh
                                    c, r = hd // 2, hd % 2
                                    for j in range(i + 1):
                                        sl = cnt["fs"] % 3
                                        cnt["fs"] += 1

                                        def front(j=j, sl=sl, c=c, r=r):
                                            F_ = FS[sl][:, 0:128]
                                            op("pe", lambda h: h.matmul(
                                                F_, lhsT=kbT[64 * r:64 * r + 64, c, j * 128:(j + 1) * 128],
                                                rhs=qbT[64 * r:64 * r + 64, c, i * 128:(i + 1) * 128], start=True, stop=(j != i)),
                                               reads=[("kbT", c, j // 4), ("qbT", c, i // 4), ("qb_blk", i, c // 2)], writes=[("FS", sl)])
                                            if j == i:
                                                op("pe", lambda h: h.matmul(F_, lhsT=ident_bf[:], rhs=trineg_bf[:], start=False, stop=True),
                                                   reads=["ident_bf", "trineg_bf"], writes=[("FS", sl)])

                                        def back(j=j, sl=sl, hd=hd, hh=hh):
                                            F_ = FS[sl][:, 0:128]
                                            op("act", lambda h: h.activation(out=ptf[sl][:], in_=F_, func=AF.Exp, scale=0.125,
                                                                             bias=biasT[:, i, j, hd:hd + 1]),
                                               reads=[("FS", sl), "biasT"], writes=[("ptf", sl)])
                                            op("pe", lambda h: h.matmul(FACC[0:64, hh * 128:(hh + 1) * 128], lhsT=vb[:, j, hd * 64:(hd + 1) * 64],
                                                                        rhs=ptf[sl][:], start=(j == 0), stop=(j == i)),
                                               reads=[("ptf", sl), ("vb", j)], writes=["FACCn"])
                                            op("pe", lambda h: h.matmul(FACC[64:128, hh * 128:(hh + 1) * 128], lhsT=ones_bf[:, 0:64],
                                                                        rhs=ptf[sl][:], start=(j == 0), stop=(j == i)),
                                               reads=[("ptf", sl), "ones_bf"], writes=["FACCd"])
                                        units.append((front, back))

                                def epi(hg=hg):
                                    FK = ["FACCn", "FACCd"]
                                    op("act", lambda h: h.copy(out=denf[64:128, :], in_=FACC[64:128, :]), reads=FK, writes=["denf"])
                                    op("dve", lambda h: h.reciprocal(out=denf[64:128, :], in_=denf[64:128, :]), reads=["denf"], writes=["denf"])
                                    for r in range(2):
                                        op("dve", lambda h, r=r: h.tensor_tensor(
                                            out=qbT[64 * r:64 * r + 64, 2 * hg:2 * hg + 2, i * 128:(i + 1) * 128],
                                            in0=FACC[0:64, :].rearrange("p (c r q) -> p c r q", c=2, r=2)[:, :, r, :],
                                            in1=denf[64:128, :].rearrange("p (c r q) -> p c r q", c=2, r=2)[:, :, r, :], op=ALU.mult),
                                           reads=FK + ["denf"], writes=[("qb_blk", i, hg)])
                                units.append((None, epi))
                            return units

                        dpipe = Pipe(1, 1)
                        fpipe = Pipe(2, 1)
                        for it_ in dsa_index(0) + dsa_index(1):
                            it_()
                        for i in range(NT):
                            items = dsa_index(i + 2) if i + 2 < NT else []
                            du = dsa_units(i)
                            fu = fox_units(i)
                            nF = len(fu)
                            rate = -(-len(items) // max(1, int(0.7 * nF)))
                            di = 0
                            ii = 0
                            for n_, (f, bk) in enumerate(fu):
                                if f is None:
                                    fpipe.flush()
                                    bk()
                                else:
                                    fpipe.push(f, bk)
                                for _ in range(rate):
                                    if ii < len(items):
                                        items[ii]()
                                        ii += 1
                                if n_ % 4 == 3 and di < len(du):
                                    dpipe.push(*du[di])
                                    di += 1
                            while ii < len(items):
                                items[ii]()
                                ii += 1
                            while di < len(du):
                                dpipe.push(*du[di])
                                di += 1
                        dpipe.flush()
                        fpipe.flush()
                T.fence()
                with contextlib.ExitStack() as p3:
                    T.enabled = STAGE >= 3
                    wo = sb("wo", [128, 8, D], BF16, p3)
                    wpg = sb("wpg", [128, 8, D], BF16, p3)
                    wp = sb("wp", [128, 2, D], BF16, p3)
                    mT = sb("mT", [128, 8, 1024], BF16, p3)
                    wg = [sb("wg%d" % k, [128, 8, 128], BF16, p3) for k in range(2)]
                    wmc = [sb("wmc%d" % k, [128, 8, 256], BF16, p3) for k in range(2)]
                    wbr = [sb("wbr%d" % k, [128, 4, 256], BF16, p3) for k in range(2)]
                    th = [sb("th%d" % k, [128, 512], BF16, p3) for k in range(4)]
                    pa = [sb("pa%d" % k, [128, 512], F32, p3) for k in range(2)]
                    pb = [sb("pb%d" % k, [128, 512], F32, p3) for k in range(2)]
                    x3 = [sb("x3_%d" % k, [128, D], F32, p3) for k in range(2)]
                    pbf = [sb("pbf%d" % k, [128, 256], BF16, p3) for k in range(2)]
                    pT = [sb("pT%d" % k, [128, 2, 128], BF16, p3) for k in range(2)]
                    junk3 = sb("junk3", [128, D], BF16, p3)
                    tA = sb("tA", [128, D], F32, p3)
                    x1 = [sb("x1_%d" % k, [128, D], F32, p3) for k in range(2)]
                    x1b = sb("x1b", [128, D], BF16, p3)
                    x1T = [sb("x1T%d" % k, [128, 8, 128], BF16, p3) for k in range(2)]
                    gth = sb("gth", [128, D], BF16, p3)
                    ge2 = sb("ge2", [128, D], F32, p3)
                    fin = [sb("fin%d" % k, [128, D], F32, p3) for k in range(2)]
                    s3 = sb("s3", [128, 8], F32, p3)
                    PQ = [ps("PQ%d" % k, [128, 1024], F32, p3) for k in range(3)]
                    PR = ps("PR", [128, 512], F32, p3)
                    PT3 = ps("PT3", [128, 1024], BF16, p3)

                    def load_cast(dst, key, lane, src2d, nkc, ncol, col0, kcstep=4):
                        scol, dcol = (0, 0) if col0 is None else col0
                        for kc0 in range(0, nkc, kcstep):
                            n = min(kcstep, nkc - kc0)
                            op("pool", lambda h, kc0=kc0, n=n: h.dma_start(
                                out=dst[:, kc0:kc0 + n, dcol:dcol + ncol],
                                in_=src2d[kc0 * 128:(kc0 + n) * 128, scol:scol + ncol].rearrange("(kc kp) n -> kp kc n", kp=128)),
                               writes=[key], lane=lane)

                    def g_load(gi):
                        gname = "ga" if gi < 4 else "gb"
                        load_cast(wg[gi % 2], ("wg", gi % 2), "wg%d" % (gi % 2), w_in_d, 8, 128, (OFF[gname] + (gi % 4) * 128, 0))

                    def g_compute(gi):
                        k = gi % 2
                        fc = gi % 4
                        attT = qaT if gi < 4 else qbT
                        blk = "qa_blk" if gi < 4 else "qb_blk"
                        for tg in range(4):
                            pq = PQ[0][:, (tg % 2) * 512:(tg % 2 + 1) * 512]
                            pqk = ("PQ", 0, tg % 2)
                            for kc in range(8):
                                op("pe", lambda h, kc=kc, pq=pq, tg=tg: h.matmul(pq, lhsT=wg[k][:, kc, :], rhs=hT[:, kc, tg * 512:(tg + 1) * 512],
                                                                               start=(kc == 0), stop=(kc == 7)),
                                   reads=[("wg", k), ("hT", tg)], writes=[pqk])
                            tk_ = tg % 2
                            op("act", lambda h, pq=pq, tk_=tk_: h.activation(out=th[tk_][:], in_=pq, func=AF.Tanh, scale=0.5), reads=[pqk], writes=[("th", tk_)])
                            op("dve", lambda h, pq=pq, tk_=tk_: h.scalar_tensor_tensor(out=pa[tk_][:], in0=th[tk_][:], scalar=1.0, in1=pq, op0=ALU.add, op1=ALU.mult),
                               reads=[pqk, ("th", tk_)], writes=[("pa", tk_)])
                            akeys = [(blk, ii, x_) for ii in range(tg * 4, tg * 4 + 4) for x_ in ((0, 1) if gi < 4 else (fc // 2,))]
                            op("dve", lambda h, tk_=tk_, tg=tg: h.scalar_tensor_tensor(
                                out=attT[:, fc, tg * 512:(tg + 1) * 512], in0=pa[tk_][:], scalar=0.5, in1=attT[:, fc, tg * 512:(tg + 1) * 512],
                                op0=ALU.mult, op1=ALU.mult), reads=[("pa", tk_)] + akeys, writes=akeys)

                    def a_load(half, dc):
                        k = dc % 2
                        load_cast(wmc[k], ("wmc", k), "wmc%d" % k, w_m_d, 8, 128, (dc * 128, 0))
                        load_cast(wmc[k], ("wmc", k), "wmc%d" % k, w_m_d, 8, 128, (D + dc * 128, 128))
                        load_cast(wbr[k], ("wbr", k), "wbr%d" % k, w_ba_d, 4, 128, (dc * 128, 0))
                        load_cast(wbr[k], ("wbr", k), "wbr%d" % k, w_bb_d, 4, 128, (dc * 128, 128))

                    def a_compute(half, dc):
                        k = dc % 2
                        for tgl in range(2):
                            tg = half * 2 + tgl
                            tsl = slice(tg * 512, (tg + 1) * 512)
                            sl2 = slice(tgl * 512, (tgl + 1) * 512)
                            ma, mb_, ya = PQ[0][:, sl2], PQ[1][:, sl2], PQ[2][:, sl2]
                            yb = PR[:, :]
                            for kc in range(8):
                                op("pe", lambda h, kc=kc: h.matmul(ma, lhsT=wmc[k][:, kc, 0:128], rhs=hT[:, kc, tsl], start=(kc == 0), stop=(kc == 7)),
                                   reads=[("wmc", k), ("hT", tg)], writes=[("PQ", 0, tgl)])
                            for kc in range(8):
                                op("pe", lambda h, kc=kc: h.matmul(mb_, lhsT=wmc[k][:, kc, 128:256], rhs=hT[:, kc, tsl], start=(kc == 0), stop=(kc == 7)),
                                   reads=[("wmc", k), ("hT", tg)], writes=[("PQ", 1, tgl)])
                            ak = [("qa_blk", ii, r_) for ii in range(tg * 4, tg * 4 + 4) for r_ in range(2)]
                            bk = [("qb_blk", ii, hg_) for ii in range(tg * 4, tg * 4 + 4) for hg_ in range(2)]
                            for fc in range(4):
                                op("pe", lambda h, fc=fc: h.matmul(ya, lhsT=wbr[k][:, fc, 0:128], rhs=qaT[:, fc, tsl], start=(fc == 0), stop=(fc == 3)),
                                   reads=[("wbr", k)] + ak, writes=[("PQ", 2, tgl)])
                            for fc in range(4):
                                op("pe", lambda h, fc=fc: h.matmul(yb, lhsT=wbr[k][:, fc, 128:256], rhs=qbT[:, fc, tsl], start=(fc == 0), stop=(fc == 3)),
                                   reads=[("wbr", k)] + bk, writes=["PR"])
                            op("act", lambda h: h.activation(out=th[tgl][:], in_=ma, func=AF.Tanh, scale=0.5), reads=[("PQ", 0, tgl)], writes=[("th", tgl)])
                            op("act", lambda h: h.activation(out=th[2 + tgl][:], in_=mb_, func=AF.Tanh, scale=0.5), reads=[("PQ", 1, tgl)], writes=[("th", 2 + tgl)])
                            op("dve", lambda h: h.scalar_tensor_tensor(out=pa[tgl][:], in0=th[tgl][:], scalar=1.0, in1=ya, op0=ALU.add, op1=ALU.mult),
                               reads=[("th", tgl), ("PQ", 2, tgl)], writes=[("pa", tgl)])
                            op("dve", lambda h: h.scalar_tensor_tensor(out=pb[tgl][:], in0=th[2 + tgl][:], scalar=1.0, in1=yb, op0=ALU.add, op1=ALU.mult),
                               reads=[("th", 2 + tgl), "PR"], writes=[("pb", tgl)])
                            op("pool", lambda h: h.tensor_tensor(out=mT[:, dc, sl2], in0=pa[tgl][:], in1=pb[tgl][:], op=ALU.add),
                               reads=[("pa", tgl), ("pb", tgl)], writes=[("mT", tgl)])

                    def b_A(half, tt):
                        t = half * 8 + tt
                        xk = t % 2
                        o_ = PQ[0] if tt % 2 == 0 else PQ[2]
                        oi_ = 0 if tt % 2 == 0 else 2
                        tsl = slice(tt * 128, (tt + 1) * 128)
                        dma_in("sp", x3[xk][:], x_d[b, t * 128:(t + 1) * 128, :], ("x3", xk), "x3_%d" % xk)
                        op("pool", lambda h: h.dma_start(out=pbf[xk][:], in_=p_d[b, t * 128:(t + 1) * 128, :]), writes=[("pbf", xk)], lane="pbf%d" % xk)
                        for hf in range(2):
                            for dc in range(8):
                                op("pe", lambda h, hf=hf, dc=dc: h.matmul(o_[:, hf * 512:(hf + 1) * 512], lhsT=mT[:, dc, tsl], rhs=wo[:, dc, hf * 512:(hf + 1) * 512],
                                                                         start=(dc == 0), stop=(dc == 7)),
                                   reads=[("mT", tt // 4), "wo"], writes=[("PQ", oi_, hf)])

                    def b_B(half, tt):
                        t = half * 8 + tt
                        xk = t % 2
                        o_ = PQ[0] if tt % 2 == 0 else PQ[2]
                        oi_ = 0 if tt % 2 == 0 else 2
                        OK_ = [("PQ", oi_, 0), ("PQ", oi_, 1)]
                        op("act", lambda h: h.activation(out=junk3[:], in_=o_[:, :], func=AF.Square, accum_out=s3[:, 0:1]), reads=OK_, writes=["junk3", "s3a"])
                        op("dve", lambda h: h.tensor_scalar(out=s3[:, 1:2], in0=s3[:, 0:1], scalar1=1.0 / D, scalar2=4.0 * EPS, op0=ALU.mult, op1=ALU.add),
                           reads=["s3a"], writes=["s3b"])
                        op("pool", lambda h: h.tensor_tensor(out=s3[:, 2:3], in0=s3[:, 1:2], in1=negh[:], op=ALU.pow), reads=["s3b", "negh"], writes=["s3c"])
                        op("dve", lambda h: h.scalar_tensor_tensor(out=tA[:], in0=o_[:, :], scalar=s3[:, 2:3], in1=gpost[:], op0=ALU.mult, op1=ALU.mult),
                           reads=OK_ + ["s3c", "gpost"], writes=["tA"])
                        op("pool", lambda h: h.tensor_tensor(out=x1[xk][:], in0=tA[:], in1=x3[xk][:], op=ALU.add), reads=["tA", ("x3", xk)], writes=[("x1", xk)])
                        op("act", lambda h: h.copy(out=x1b[:], in_=x1[xk][:]), reads=[("x1", xk)], writes=["x1b"])
                        for kc in range(8):
                            op("pe", lambda h, kc=kc: h.transpose(out=PT3[:, kc * 128:(kc + 1) * 128], in_=x1b[:, kc * 128:(kc + 1) * 128], identity=ident_bf[:]),
                               reads=["x1b", "ident_bf"], writes=["PT3"])
                        op("act", lambda h: h.copy(out=x1T[xk][:], in_=PT3[:, :].rearrange("p (kc n) -> p kc n", kc=8)), reads=["PT3"], writes=[("x1T", xk)])
                        for pc in range(2):
                            op("pe", lambda h, pc=pc: h.transpose(out=PT3[:, pc * 128:(pc + 1) * 128], in_=pbf[xk][:, pc * 128:(pc + 1) * 128], identity=ident_bf[:]),
                               reads=[("pbf", xk), "ident_bf"], writes=["PT3"])
                        op("dve", lambda h: h.tensor_copy(out=pT[xk][:], in_=PT3[:, 0:256].rearrange("p (kc n) -> p kc n", kc=2)), reads=["PT3"], writes=[("pT", xk)])

                    def b_C(half, tt):
                        xk = (half * 8 + tt) % 2
                        for hf in range(2):
                            for dc in range(8):
                                op("pe", lambda h, hf=hf, dc=dc: h.matmul(PQ[1][:, hf * 512:(hf + 1) * 512], lhsT=x1T[xk][:, dc, :], rhs=wpg[:, dc, hf * 512:(hf + 1) * 512],
                                                                         start=(dc == 0), stop=(dc == 7)),
                                   reads=[("x1T", xk), "wpg"], writes=[("PQ", 1, hf)])

                    def b_D(half, tt):
                        t = half * 8 + tt
                        xk = t % 2
                        GK = [("PQ", 1, 0), ("PQ", 1, 1)]
                        op("act", lambda h: h.activation(out=gth[:], in_=PQ[1][:, :], func=AF.Tanh, scale=0.5), reads=GK, writes=["gth"])
                        for hf in range(2):
                            for pc in range(2):
                                op("pe", lambda h, hf=hf, pc=pc: h.matmul(PR[:, :], lhsT=pT[xk][:, pc, :], rhs=wp[:, pc, hf * 512:(hf + 1) * 512],
                                                                         start=(pc == 0), stop=(pc == 1)),
                                   reads=[("pT", xk), "wp"], writes=["PR"])
                            op("dve", lambda h, hf=hf: h.scalar_tensor_tensor(out=ge2[:, hf * 512:(hf + 1) * 512], in0=gth[:, hf * 512:(hf + 1) * 512], scalar=1.0,
                                                                              in1=PR[:, :], op0=ALU.add, op1=ALU.mult),
                               reads=["gth", "PR"], writes=[("ge2", hf)])
                        op("act", lambda h: h.activation(out=junk3[:], in_=ge2[:], func=AF.Square, accum_out=s3[:, 3:4]), reads=[("ge2", 0), ("ge2", 1)], writes=["junk3", "s3d"])
                        op("dve", lambda h: h.tensor_scalar(out=s3[:, 4:5], in0=s3[:, 3:4], scalar1=1.0 / D, scalar2=4.0 * EPS, op0=ALU.mult, op1=ALU.add),
                           reads=["s3d"], writes=["s3e"])
                        op("pool", lambda h: h.tensor_tensor(out=s3[:, 5:6], in0=s3[:, 4:5], in1=negh[:], op=ALU.pow), reads=["s3e", "negh"], writes=["s3f"])
                        op("dve", lambda h: h.scalar_tensor_tensor(out=fin[xk][:], in0=ge2[:], scalar=s3[:, 5:6], in1=gple[:], op0=ALU.mult, op1=ALU.mult),
                           reads=[("ge2", 0), ("ge2", 1), "s3f", "gple"], writes=[("fin", xk)])
                        op("pool", lambda h: h.tensor_tensor(out=fin[xk][:], in0=fin[xk][:], in1=x1[xk][:], op=ALU.add), reads=[("fin", xk), ("x1", xk)], writes=[("fin", xk)])
                        out_marks.append(op("sp", lambda h: h.dma_start(out=out_d[b, t * 128:(t + 1) * 128, :], in_=fin[xk][:]),
                                            reads=[("fin", xk)], lane="fin%d" % xk))

                    g_load(0)
                    g_load(1)
                    load_cast(wo, "wo", "wo", w_o_d, 8, D, None, kcstep=2)
                    load_cast(wpg, "wpg", "wpg", w_pg_d, 8, D, None, kcstep=2)
                    load_cast(wp, "wp", "wp", w_p_d, 2, D, None, kcstep=2)
                    for gi in range(8):
                        g_compute(gi)
                        if gi + 2 < 8:
                            g_load(gi + 2)
                    a_seq = [(hf_, dc_) for hf_ in range(2) for dc_ in range(8)]
                    a_load(*a_seq[0])
                    a_pos = [1]

                    def a_step(e):
                        if e + 1 < len(a_seq) and a_pos[0] == e + 1:
                            a_load(*a_seq[e + 1])
                            a_pos[0] += 1
                        a_compute(*a_seq[e])

                    for half in range(2):
                        for dc in range(8):
                            a_step(half * 8 + dc)
                        b_A(half, 0)
                        b_B(half, 0)
                        for tt in range(8):
                            if tt + 1 < 8:
                                b_A(half, tt + 1)
                            b_C(half, tt)
                            if tt + 1 < 8:
                                b_B(half, tt + 1)
                            b_D(half, tt)
        last = {}
        for (sk, v) in [m for m in out_marks if m]:
            last[sk] = max(last.get(sk, 0), v)
        T.wait_marks("sp", list(last.items()))
    T.close()
    build_program.stats = (T.nops, T.nwaits)
    return nc


def _consts():
    half = 8
    freqs = 500000.0 ** (-np.arange(half, dtype=np.float64) / half)
    pos = np.arange(S, dtype=np.float64)
    cosT = np.ones((128, S), np.float64)
    sinT = np.zeros((128, S), np.float64)
    perm = np.zeros((128, 128), np.float32)
    for hh in range(2):
        for d in range(16):
            f = hh * 64 + d
            ang = pos * freqs[d % 8]
            cosT[f] = np.cos(ang)
            sinT[f] = np.sin(ang)
            if d < 8:
                perm[f + 8, f] = -1.0
            else:
                perm[f - 8, f] = 1.0
    idx = np.arange(128)
    ident = np.eye(128, dtype=np.float32)
    trineg = np.where(idx[:, None] <= idx[None, :], 0.0, MASKNEG).astype(np.float32)
    causneg = np.where(idx[None, :] <= idx[:, None], 0.0, -1.0e9).astype(np.float32)
    U = (idx[:, None] <= idx[None, :]).astype(np.float32)
    sel0 = np.zeros((128, 128), np.float32)
    sel0[0, :] = 1.0
    ones = np.ones((128, 128), np.float32)
    cst = np.stack([ident, perm, trineg, causneg, U, sel0, ones], axis=1).astype(np.float32)
    pw = list(0.5 ** np.arange(1, NIT + 1, dtype=np.float64))
    pw2 = np.tile(np.array(pw + [pw[-1]])[None, :], (128, 1)).astype(np.float32)
    return cosT.astype(np.float32), sinT.astype(np.float32), np.ascontiguousarray(cst), pw2


def _run(inputs, nseq, seq_ids_per_core):
    x = np.asarray(inputs["x"], np.float32)
    p = np.asarray(inputs["p"], np.float32)[0]
    cosT, sinT, cst, pw2 = _consts()
    rep = lambda v: np.ascontiguousarray(np.broadcast_to(np.asarray(v, np.float32).reshape(1, -1), (128, np.asarray(v).size)))
    common = {
        "w_in": np.ascontiguousarray(np.asarray(inputs["w_in"], np.float32)[0]),
        "w_ba": np.ascontiguousarray(np.asarray(inputs["w_branch_a"], np.float32)[0]),
        "w_bb": np.ascontiguousarray(np.asarray(inputs["w_branch_b"], np.float32)[0]),
        "w_m": np.ascontiguousarray(np.asarray(inputs["w_merge"], np.float32)[0]),
        "w_o": np.ascontiguousarray(np.asarray(inputs["w_out"], np.float32)[0]),
        "w_p": np.ascontiguousarray(np.asarray(inputs["w_ple"], np.float32)[0]),
        "w_pg": np.ascontiguousarray(np.asarray(inputs["w_ple_gate"], np.float32)[0]),
        "gpre": rep(inputs["g_pre"][0]), "gpost": rep(inputs["g_post"][0]), "gple": rep(inputs["g_ple"][0]),
        "bfb": rep(inputs["b_forget"][0]),
        "cosT": cosT, "sinT": sinT, "cst": cst, "pw2": pw2,
    }
    nc = build_program(nseq)
    in_maps = []
    for c in range(NCORES):
        ids = seq_ids_per_core[c]
        m = dict(common)
        m["x"] = np.ascontiguousarray(x[ids])
        m["p"] = np.ascontiguousarray(p[ids])
        in_maps.append(m)
    res = run_bass_kernel_spmd(nc, in_maps, core_ids=list(range(NCORES)))
    return [r["out"] for r in res.results]


def kernel(**inputs):
    B = np.asarray(inputs["x"]).shape[0]
    nseq = B // NCORES
    ids = [list(range(c * nseq, (c + 1) * nseq)) for c in range(NCORES)]
    outs = _run(inputs, nseq, ids)
    return np.concatenate(outs, axis=0).astype(np.float32)
```
